# Optimizing a Trainium2 kernel written in Bass

```python
import jax, jax.numpy as jnp
from jax import lax
import numpy as np


D_MODEL = 2048
BATCH = 8
SEQ = 2048
DEPTH = 2

CTX_LEN = 256
GRID_W = 64
NORM_EPS = 1e-6
RWKV_DIM = D_MODEL // 2
RWKV_HEAD = 64
RWKV_HEADS = RWKV_DIM // RWKV_HEAD
DECAY_LORA = 96
ICLR_LORA = 96
GATE_LORA = 256
RWKV_GN_EPS = 64e-5
GLA_HEADS = 4
GLA_KDIM = D_MODEL // 4
GLA_VDIM = D_MODEL // 2
GLA_DK = GLA_KDIM // GLA_HEADS
GLA_DV = GLA_VDIM // GLA_HEADS
GLA_GATE_LORA = 16
GLA_TAU = 16.0
GLA_CHUNK = 64
GLA_CONV = 3
N_EXPERTS = 16
N_GROUPS = 4
EXPERTS_PER_GROUP = N_EXPERTS // N_GROUPS
TOP_K = 2
D_EXPERT = 1408
RWKV_IN = 3 * RWKV_DIM + DECAY_LORA + ICLR_LORA + GATE_LORA
RWKV_SPLITS = (RWKV_DIM, 2 * RWKV_DIM, 3 * RWKV_DIM, 3 * RWKV_DIM + DECAY_LORA, 3 * RWKV_DIM + DECAY_LORA + ICLR_LORA)
GLA_QKV = 2 * GLA_KDIM + GLA_VDIM
GLA_SEQ_IN = GLA_QKV + GLA_GATE_LORA
GLA_GATE_OFF = RWKV_IN + GLA_SEQ_IN
BR_GATE_OFF = GLA_GATE_OFF + GLA_VDIM
IN_DIM = BR_GATE_OFF + 2 * D_MODEL

kernel_name = 'hybrid_rwkv7_gla_moe_prefix_dit'


def rms_norm(x, g):
    x32 = x.astype(jnp.float32)
    y = x32 * lax.rsqrt(jnp.mean(x32 * x32, -1, keepdims=True) + NORM_EPS)
    return (y * g).astype(x.dtype)


def modulate(h, shift, scale):
    return h * (1.0 + scale) + shift


def centred_shift(p, mu):
    prev = jnp.pad(p, ((0, 0), (1, 0), (0, 0)))[:, :-1]
    nxt = jnp.pad(p, ((0, 0), (0, 1), (0, 0)))[:, 1:]
    return p + mu[0] * (prev - p) + mu[1] * (nxt - p)


def depthwise_conv(u, w):
    return lax.conv_general_dilated(u, w[:, None, :].astype(u.dtype), window_strides=(1,), padding='SAME',
                                    dimension_numbers=('NWC', 'WIO', 'NWC'), feature_group_count=u.shape[-1])


def to_column_major(t, rows):
    b, l, ch = t.shape
    return t.reshape(b, rows, GRID_W, ch).transpose(0, 2, 1, 3).reshape(b, l, ch)


def to_row_major(t, rows):
    b, l, ch = t.shape
    return t.reshape(b, GRID_W, rows, ch).transpose(0, 2, 1, 3).reshape(b, l, ch)


def rwkv_scan(s0, r, w, k, v, kk, a, reverse):
    def step(s, inp):
        r_t, w_t, k_t, v_t, kk_t, a_t = inp
        sa = jnp.einsum('bhvk,bhk->bhv', s, kk_t)
        s = s * w_t[:, :, None, :] - sa[..., None] * (kk_t * a_t)[:, :, None, :] + v_t[..., None] * k_t[:, :, None, :]
        return s, jnp.einsum('bhvk,bhk->bhv', s, r_t)
    xs = tuple(jnp.moveaxis(t, 1, 0) for t in (r, w, k, v, kk, a))
    s_fin, ys = lax.scan(step, s0, xs, reverse=reverse)
    return jnp.moveaxis(ys, 0, 1), s_fin


def rwkv_stream(p, init, mu, w0, w2, a0, a2, g2, k_k, k_a, r_k, ln_g, ln_b):
    b, l, _ = p.shape
    f32 = jnp.float32
    hd = lambda t: t.astype(f32).reshape(t.shape[:-1] + (RWKV_HEADS, RWKV_HEAD))
    s = centred_shift(p, mu)
    r, k, v, xw, xa, xg = jnp.split(s, RWKV_SPLITS, axis=-1)
    r_h, k_h, v_h = hd(r), hd(k), hd(v)
    kk = hd(k * k_k)
    kk = kk / jnp.maximum(jnp.sqrt(jnp.sum(kk * kk, -1, keepdims=True)), 1e-12)
    y = jnp.zeros_like(v_h)
    finals = []
    for d in range(2):
        w_log = -jax.nn.softplus(-(w0[d] + jnp.tanh(xw) @ w2[d]).astype(f32)) - 0.5
        decay = hd(jnp.exp(-jnp.exp(w_log)))
        a = hd(jax.nn.sigmoid((a0[d] + xa @ a2[d]).astype(f32)))
        k_d = k_h * (1.0 + (a - 1.0) * hd(k_a))
        y_d, s_d = rwkv_scan(init[d], r_h, decay, k_d, v_h, kk, a, reverse=(d == 1))
        y = y + y_d
        finals.append(s_d)
    mean = jnp.mean(y, -1, keepdims=True)
    var = jnp.mean(jnp.square(y - mean), -1, keepdims=True)
    yn = ((y - mean) * lax.rsqrt(var + RWKV_GN_EPS)).reshape(b, l, RWKV_DIM) * ln_g + ln_b
    bonus = (jnp.sum(r_h * k_h * r_k, -1, keepdims=True) * v_h).reshape(b, l, RWKV_DIM)
    gate = jax.nn.sigmoid(xg) @ g2
    return (yn + bonus).astype(p.dtype) * gate, finals


def gla_chunked(q, k, v, g, s0):
    b, l, h, _ = q.shape
    dv = v.shape[-1]
    n = l // GLA_CHUNK
    blk = lambda t: t.astype(jnp.float32).reshape(b, n, GLA_CHUNK, h, t.shape[-1])
    q, k, v, g = blk(q), blk(k), blk(v), blk(g)
    gc = jnp.cumsum(g, axis=2)
    g_last = gc[:, :, -1]
    q_g = q * jnp.exp(gc)
    k_g = k * jnp.exp(-gc)
    k_end = k * jnp.exp(g_last[:, :, None] - gc)
    mask = jnp.tril(jnp.ones((GLA_CHUNK, GLA_CHUNK), jnp.float32))
    att = jnp.einsum('bnihd,bnjhd->bnhij', q_g, k_g) * mask
    o_intra = jnp.einsum('bnhij,bnjhv->bnihv', att, v)
    ds = jnp.einsum('bnjhd,bnjhv->bnhdv', k_end, v)

    def step(s, inp):
        ds_c, dec_c = inp
        return s * dec_c[..., None] + ds_c, s
    s_fin, s_prev = lax.scan(step, s0, (jnp.moveaxis(ds, 1, 0), jnp.moveaxis(jnp.exp(g_last), 1, 0)))
    o_inter = jnp.einsum('bnihd,nbhdv->bnihv', q_g, s_prev)
    return (o_intra + o_inter).reshape(b, l, h, dv), s_fin


def gla_stream(u, init, conv_w, alpha_w2, alpha_b, norm_g):
    b, l, _ = u.shape
    f32 = jnp.float32
    qkv = jax.nn.silu(depthwise_conv(u[..., :GLA_QKV], conv_w))
    q = qkv[..., :GLA_KDIM].reshape(b, l, GLA_HEADS, GLA_DK) * (GLA_DK ** -0.5)
    k = qkv[..., GLA_KDIM:2 * GLA_KDIM].reshape(b, l, GLA_HEADS, GLA_DK)
    v = qkv[..., 2 * GLA_KDIM:].reshape(b, l, GLA_HEADS, GLA_DV)
    ad = u[..., GLA_QKV:]
    o = jnp.zeros((b, l, GLA_HEADS, GLA_DV), f32)
    finals = []
    for d in range(2):
        g = (jax.nn.log_sigmoid((ad @ alpha_w2[d] + alpha_b[d]).astype(f32)) / GLA_TAU).reshape(b, l, GLA_HEADS, GLA_DK)
        if d == 0:
            o_d, s_d = gla_chunked(q, k, v, g, init[d])
        else:
            o_d, s_d = gla_chunked(*(jnp.flip(t, 1) for t in (q, k, v, g)), init[d])
            o_d = jnp.flip(o_d, 1)
        o = o + o_d
        finals.append(s_d)
    o = o * lax.rsqrt(jnp.mean(o * o, -1, keepdims=True) + NORM_EPS) * norm_g
    return o.reshape(b, l, GLA_VDIM).astype(u.dtype), finals


def hybrid_mixer(h_c, h_l, rows, need_ctx, w_in, rwkv_mu, rwkv_w0, rwkv_w2, rwkv_a0, rwkv_a2, rwkv_g2,
                 rwkv_k_k, rwkv_k_a, rwkv_r_k, rwkv_ln_g, rwkv_ln_b, gla_conv, gla_alpha_w2, gla_alpha_b,
                 gla_norm_g, w_branch_a, w_branch_b, w_out):
    b = h_l.shape[0]
    f32 = jnp.float32
    rwkv_args = (rwkv_mu, rwkv_w0, rwkv_w2, rwkv_a0, rwkv_a2, rwkv_g2, rwkv_k_k, rwkv_k_a, rwkv_r_k, rwkv_ln_g, rwkv_ln_b)
    gla_args = (gla_conv, gla_alpha_w2, gla_alpha_b, gla_norm_g)
    p_c = h_c @ w_in
    p_l = h_l @ w_in
    zr = jnp.zeros((b, RWKV_HEADS, RWKV_HEAD, RWKV_HEAD), f32)
    zg = jnp.zeros((b, GLA_HEADS, GLA_DK, GLA_DV), f32)
    ya_c, st_r = rwkv_stream(p_c[..., :RWKV_IN], (zr, zr), *rwkv_args)
    ya_l, _ = rwkv_stream(p_l[..., :RWKV_IN], st_r, *rwkv_args)
    yb_c, st_g = gla_stream(p_c[..., RWKV_IN:GLA_GATE_OFF], (zg, zg), *gla_args)
    yb_l_cm, _ = gla_stream(to_column_major(p_l[..., RWKV_IN:GLA_GATE_OFF], rows), st_g, *gla_args)
    yb_l = to_row_major(yb_l_cm, rows)

    def merge(p, ya, yb):
        yb = yb * jax.nn.silu(p[..., GLA_GATE_OFF:BR_GATE_OFF])
        ga = jax.nn.sigmoid(p[..., BR_GATE_OFF:BR_GATE_OFF + D_MODEL])
        gb = jax.nn.sigmoid(p[..., BR_GATE_OFF + D_MODEL:])
        return (ga * (ya @ w_branch_a) + gb * (yb @ w_branch_b)) @ w_out
    y_l = merge(p_l, ya_l, yb_l)
    y_c = merge(p_c, ya_c, yb_c) if need_ctx else None
    return y_c, y_l


def moe_ffn(h, router_w, router_b, w_gate, w_up, w_down):
    shp = h.shape
    t = h.reshape(-1, shp[-1])
    scores = jax.nn.sigmoid((t @ router_w).astype(jnp.float32))
    sel = scores + router_b.astype(jnp.float32)
    grp_score = jnp.sum(lax.top_k(sel.reshape(-1, N_GROUPS, EXPERTS_PER_GROUP), 2)[0], -1)
    best = jnp.argmax(grp_score, -1)
    in_grp = (jnp.arange(N_EXPERTS) // EXPERTS_PER_GROUP)[None, :] == best[:, None]
    _, idx = lax.top_k(jnp.where(in_grp, sel, -jnp.inf), TOP_K)
    wts = jnp.take_along_axis(scores, idx, -1)
    wts = wts / jnp.sum(wts, -1, keepdims=True)
    gates = jnp.sum(jax.nn.one_hot(idx, N_EXPERTS, dtype=jnp.float32) * wts[..., None], 1).astype(t.dtype)
    out = jnp.zeros_like(t)
    for e in range(N_EXPERTS):
        out = out + gates[:, e:e + 1] * ((jax.nn.silu(t @ w_gate[e]) * (t @ w_up[e])) @ w_down[e])
    return out.reshape(shp)


def ffn_sublayer(xs, norm_g, shift, scale, gate, router_w, router_b, w_gate, w_up, w_down):
    return xs + gate * moe_ffn(modulate(rms_norm(xs, norm_g), shift, scale), router_w, router_b, w_gate, w_up, w_down)


def setup_inputs(seed: int = 0) -> dict:
    key = jax.random.key(seed)
    ks = iter(jax.random.split(key, 40))
    f32 = jnp.float32
    normal = lambda shape, s: jax.random.normal(next(ks), shape, f32) * s
    unif = lambda shape, lo, hi: jax.random.uniform(next(ks), shape, f32, lo, hi)
    L = DEPTH
    return {
        'x': normal((BATCH, SEQ, D_MODEL), 1.0),
        'c': normal((BATCH, D_MODEL), 1.0),
        'ctx': normal((BATCH, CTX_LEN, D_MODEL), 1.0),
        'c_ctx': normal((D_MODEL,), 1.0),
        'w_ada': normal((L, D_MODEL, 6 * D_MODEL), 0.5 * D_MODEL ** -0.5),
        'b_ada': normal((L, 6 * D_MODEL), 0.01),
        'norm_mix_g': 1.0 + normal((L, D_MODEL), 0.05),
        'norm_ffn_g': 1.0 + normal((L, D_MODEL), 0.05),
        'w_in': normal((L, D_MODEL, IN_DIM), D_MODEL ** -0.5),
        'rwkv_mu': unif((L, 2, RWKV_IN), 0.0, 0.5),
        'rwkv_w0': unif((L, 2, RWKV_DIM), -5.0, 0.5),
        'rwkv_w2': normal((L, 2, DECAY_LORA, RWKV_DIM), 0.1 * DECAY_LORA ** -0.5),
        'rwkv_a0': normal((L, 2, RWKV_DIM), 0.5),
        'rwkv_a2': normal((L, 2, ICLR_LORA, RWKV_DIM), 0.1 * ICLR_LORA ** -0.5),
        'rwkv_g2': normal((L, GATE_LORA, RWKV_DIM), GATE_LORA ** -0.5),
        'rwkv_k_k': 0.85 + normal((L, RWKV_DIM), 0.05),
        'rwkv_k_a': 1.0 + normal((L, RWKV_DIM), 0.05),
        'rwkv_r_k': normal((L, RWKV_HEADS, RWKV_HEAD), 0.1),
        'rwkv_ln_g': 1.0 + normal((L, RWKV_DIM), 0.05),
        'rwkv_ln_b': normal((L, RWKV_DIM), 0.01),
        'gla_conv': normal((L, GLA_CONV, GLA_QKV), GLA_CONV ** -0.5),
        'gla_alpha_w2': normal((L, 2, GLA_GATE_LORA, GLA_KDIM), GLA_GATE_LORA ** -0.5),
        'gla_alpha_b': unif((L, 2, GLA_KDIM), 0.0, 4.0),
        'gla_norm_g': 1.0 + normal((L, GLA_DV), 0.05),
        'w_branch_a': normal((L, RWKV_DIM, D_MODEL), RWKV_DIM ** -0.5),
        'w_branch_b': normal((L, GLA_VDIM, D_MODEL), GLA_VDIM ** -0.5),
        'w_out': normal((L, D_MODEL, D_MODEL), D_MODEL ** -0.5),
        'router_w': normal((D_MODEL, N_EXPERTS), D_MODEL ** -0.5),
        'router_b': normal((N_EXPERTS,), 0.01),
        'exp_w_gate': normal((L, N_EXPERTS, D_MODEL, D_EXPERT), D_MODEL ** -0.5),
        'exp_w_up': normal((L, N_EXPERTS, D_MODEL, D_EXPERT), D_MODEL ** -0.5),
        'exp_w_down': normal((L, N_EXPERTS, D_EXPERT, D_MODEL), D_EXPERT ** -0.5),
        'final_norm_g': 1.0 + normal((D_MODEL,), 0.05),
    }


def reference(x, c, ctx, c_ctx, w_ada, b_ada, norm_mix_g, norm_ffn_g, w_in, rwkv_mu, rwkv_w0, rwkv_w2,
              rwkv_a0, rwkv_a2, rwkv_g2, rwkv_k_k, rwkv_k_a, rwkv_r_k, rwkv_ln_g, rwkv_ln_b, gla_conv,
              gla_alpha_w2, gla_alpha_b, gla_norm_g, w_branch_a, w_branch_b, w_out, router_w, router_b,
              exp_w_gate, exp_w_up, exp_w_down, final_norm_g):
    rows = x.shape[1] // GRID_W
    silu_c = jax.nn.silu(c)[:, None, :]
    silu_cc = jax.nn.silu(c_ctx)
    x_lat, x_ctx = x, ctx
    for l in range(DEPTH):
        last = l == DEPTH - 1
        mod_l = jnp.split(silu_c @ w_ada[l] + b_ada[l], 6, axis=-1)
        mod_c = jnp.split(silu_cc @ w_ada[l] + b_ada[l], 6, axis=-1)
        h_l = modulate(rms_norm(x_lat, norm_mix_g[l]), mod_l[0], mod_l[1])
        h_c = modulate(rms_norm(x_ctx, norm_mix_g[l]), mod_c[0], mod_c[1])
        y_c, y_l = hybrid_mixer(h_c, h_l, rows, not last, w_in[l], rwkv_mu[l], rwkv_w0[l], rwkv_w2[l],
                                rwkv_a0[l], rwkv_a2[l], rwkv_g2[l], rwkv_k_k[l], rwkv_k_a[l], rwkv_r_k[l],
                                rwkv_ln_g[l], rwkv_ln_b[l], gla_conv[l], gla_alpha_w2[l], gla_alpha_b[l],
                                gla_norm_g[l], w_branch_a[l], w_branch_b[l], w_out[l])
        x_lat = x_lat + mod_l[2] * y_l
        x_lat = ffn_sublayer(x_lat, norm_ffn_g[l], mod_l[3], mod_l[4], mod_l[5], router_w, router_b,
                             exp_w_gate[l], exp_w_up[l], exp_w_down[l])
        if not last:
            x_ctx = x_ctx + mod_c[2] * y_c
            x_ctx = ffn_sublayer(x_ctx, norm_ffn_g[l], mod_c[3], mod_c[4], mod_c[5], router_w, router_b,
                                 exp_w_gate[l], exp_w_up[l], exp_w_down[l])
    return rms_norm(x_lat, final_norm_g)
```

```python
import contextlib
import numpy as np
import concourse.bass as bass
import concourse.mybir as mybir
from concourse.bass_utils import run_bass_kernel_spmd

F32 = mybir.dt.float32
BF16 = mybir.dt.bfloat16
AF = mybir.ActivationFunctionType
ALU = mybir.AluOpType
AX = mybir.AxisListType

ENGS = ("tensor", "vector", "scalar", "gpsimd", "sync")
N_DMA_SEMS = 12

D = 2048
T = 2304
NT = 18
CTX = 256
DEPTH = 2
RWKV_IN = 3520
GLA_OFF = 3520
GLA_GATE_OFF = 5584
BR_GATE_OFF = 6608
IN_DIM = 10704
NE = 16
DE = 1408
EPS = 1e-6


class View:
    def __init__(s, buf, ap, keys):
        s.buf, s.ap, s.keys = buf, ap, keys

    def re(s, pat, **kw):
        return View(s.buf, s.ap.rearrange(pat, **kw), s.keys)

    def __getitem__(s, idx):
        return View(s.buf, s.ap[idx], s.keys)

    def pb(s, n):
        return View(s.buf, s.ap.partition_broadcast(n), s.keys)


class Buf:
    def __init__(s, name, h, nsub=None):
        s.name, s.h, s.nsub = name, h, nsub

    def allkeys(s):
        if s.nsub:
            return [(s.name, i) for i in range(s.nsub)]
        return [s.name]

    def __getitem__(s, idx):
        return View(s, s.h[idx], s.allkeys())

    def sub(s, i, idx):
        return View(s, s.h[idx], [(s.name, i)])


class Prog:
    def __init__(s, nc):
        s.nc = nc
        s.st = contextlib.ExitStack()
        s.ops = {e: [] for e in ENGS}
        s.cnt = {e: 0 for e in ENGS}
        s.seen = {e: {} for e in ENGS}
        s.last_w = {}
        s.readers = {}
        s.dma_rr = {e: 0 for e in ENGS}
        s.dma_tgt = {}
        s.final_tokens = []
        s.nbuf = 0

    def sb(s, name, shape, dt=F32, nsub=None, stack=None):
        s.nbuf += 1
        nm = f"{name}_{s.nbuf}"
        h = (stack or s.st).enter_context(s.nc.sbuf_tensor(nm, list(shape), dt))
        return Buf(nm, h, nsub)

    def ps(s, name, shape, dt=F32, stack=None):
        s.nbuf += 1
        nm = f"{name}_{s.nbuf}"
        h = (stack or s.st).enter_context(s.nc.psum_tensor(nm, list(shape), dt))
        return Buf(nm, h)

    def dram(s, name, shape, dt=F32, kind="Internal", nsub=None):
        h = s.nc.dram_tensor(name, list(shape), dt, kind=kind).ap()
        return Buf(name, h, nsub)

    def _need(s, eng, tok, waits):
        if tok is None:
            return
        sk, v = tok
        if sk == ("e", eng) and eng == "tensor":
            return
        if s.seen[eng].get(sk, 0) >= v:
            return
        s.seen[eng][sk] = v
        waits.append((sk, v))

    def _deps(s, eng, reads, writes):
        waits = []
        for k in reads:
            s._need(eng, s.last_w.get(k), waits)
        for k in writes:
            s._need(eng, s.last_w.get(k), waits)
            for t in s.readers.get(k, ()):
                s._need(eng, t, waits)
        return waits

    def _commit(s, tok, reads, writes):
        for k in reads:
            s.readers.setdefault(k, []).append(tok)
        for k in writes:
            s.last_w[k] = tok
            s.readers[k] = []

    def op(s, eng, fn, reads=(), writes=()):
        waits = s._deps(eng, reads, writes)
        s.cnt[eng] += 1
        tok = (("e", eng), s.cnt[eng])
        s.ops[eng].append((waits, fn, tok))
        s._commit(tok, reads, writes)
        return tok

    def _dma(s, eng, fn, reads=(), writes=()):
        waits = s._deps(eng, reads, writes)
        slot = s.dma_rr[eng] % N_DMA_SEMS
        s.dma_rr[eng] += 1
        sk = ("d", eng, slot)
        prev = s.dma_tgt.get(sk, 0)
        if prev:
            s._need(eng, (sk, prev), waits)
        tgt = prev + 16
        s.dma_tgt[sk] = tgt
        tok = (sk, tgt)
        s.ops[eng].append((waits, fn, tok))
        s._commit(tok, reads, writes)
        return tok

    def barrier(s):
        toks = [(("e", e), s.cnt[e]) for e in ENGS if s.cnt[e]]
        toks += [(sk, v) for sk, v in s.dma_tgt.items()]
        for e in ENGS:
            waits = []
            for t in toks:
                s._need(e, t, waits)
            if waits:
                s.ops[e].append((waits, None, None))
        s.last_w.clear()
        s.readers.clear()

    def emit(s):
        nc = s.nc
        with contextlib.ExitStack() as st:
            semh = {}
            for e in ENGS:
                semh[("e", e)] = st.enter_context(nc.semaphore(f"s_{e}"))
            for e in ENGS:
                for i in range(min(N_DMA_SEMS, s.dma_rr[e])):
                    semh[("d", e, i)] = st.enter_context(nc.semaphore(f"d_{e}_{i}"))
            block = st.enter_context(nc.Block())
            fin = []
            for t in s.final_tokens:
                s._need("sync", t, fin)
            for e in ENGS:
                ops = s.ops[e]
                extra = fin if e == "sync" else []
                if not ops and not extra:
                    continue

                def body(engine, ops=ops, extra=extra):
                    for waits, fn, tok in ops:
                        for sk, v in waits:
                            engine.wait_ge(semh[sk], v)
                        if fn is None:
                            continue
                        ins = fn(engine)
                        sk, v = tok
                        ins.then_inc(semh[sk], 16 if sk[0] == "d" else 1)
                    for sk, v in extra:
                        engine.wait_ge(semh[sk], v)
                getattr(block, e)(body)

    @staticmethod
    def _k(*vs):
        ks = []
        for v in vs:
            if isinstance(v, View):
                ks += v.keys
        return ks

    @staticmethod
    def _a(v):
        return v.ap if isinstance(v, View) else v

    def mm(s, out, lhsT, rhs, start=True, stop=True):
        return s.op("tensor", lambda e: e.matmul(out.ap, lhsT.ap, rhs.ap, start=start, stop=stop),
                    reads=s._k(lhsT, rhs), writes=s._k(out))

    def tr(s, out, in_, ident):
        return s.op("tensor", lambda e: e.transpose(out.ap, in_.ap, ident.ap),
                    reads=s._k(in_, ident), writes=s._k(out))

    def act(s, out, in_, func, bias=None, scale=None, accum=None):
        kw = {}
        if bias is not None:
            kw["bias"] = s._a(bias)
        if scale is not None:
            kw["scale"] = s._a(scale)
        if accum is not None:
            kw["accum_out"] = accum.ap
        return s.op("scalar", lambda e: e.activation(out.ap, in_.ap, func, **kw),
                    reads=s._k(in_, bias, scale), writes=s._k(out, accum))

    def tt(s, out, a, b, op, eng="vector"):
        return s.op(eng, lambda e: e.tensor_tensor(out.ap, a.ap, b.ap, op),
                    reads=s._k(a, b), writes=s._k(out))

    def ts(s, out, a, s1, op0, s2=None, op1=None, eng="vector", accum=None):
        kw = {}
        if op1 is not None:
            kw["op1"] = op1
        if accum is not None:
            kw["accum_out"] = accum.ap
        return s.op(eng, lambda e: e.tensor_scalar(out.ap, a.ap, s._a(s1), s._a(s2), op0, **kw),
                    reads=s._k(a, s1, s2), writes=s._k(out, accum))

    def stt(s, out, a, sc, b, op0, op1):
        return s.op("vector", lambda e: e.scalar_tensor_tensor(out.ap, a.ap, s._a(sc), b.ap, op0, op1),
                    reads=s._k(a, sc, b), writes=s._k(out))

    def cp(s, out, in_, eng="vector"):
        if eng == "scalar":
            return s.op("scalar", lambda e: e.copy(out.ap, in_.ap), reads=s._k(in_), writes=s._k(out))
        return s.op(eng, lambda e: e.tensor_copy(out.ap, in_.ap), reads=s._k(in_), writes=s._k(out))

    def memset(s, out, val, eng="vector"):
        return s.op(eng, lambda e: e.memset(out.ap, val), writes=s._k(out))

    def red(s, out, in_, op, axis=AX.X):
        return s.op("vector", lambda e: e.tensor_reduce(out.ap, in_.ap, axis, op),
                    reads=s._k(in_), writes=s._k(out))

    def recip(s, out, in_):
        return s.op("vector", lambda e: e.reciprocal(out.ap, in_.ap), reads=s._k(in_), writes=s._k(out))

    def scan(s, out, d0, d1, initial, op0, op1):
        return s.op("vector", lambda e: e.tensor_tensor_scan(out.ap, d0.ap, d1.ap, s._a(initial), op0, op1),
                    reads=s._k(d0, d1, initial), writes=s._k(out))

    def dma(s, out, in_, eng="sync"):
        return s._dma(eng, lambda e: e.dma_start(out=out.ap, in_=in_.ap), reads=s._k(in_), writes=s._k(out))


def make_consts():
    c = {}
    c["ident"] = np.eye(128, dtype=np.float32)
    p = np.arange(128)[:, None]
    f = np.arange(128)[None, :]
    c["m_plt"] = (p < f).astype(np.float32)
    c["m_ple"] = (p <= f).astype(np.float32)
    c["m_pgt"] = (p > f).astype(np.float32)
    c["m_pge"] = (p >= f).astype(np.float32)
    bo = np.zeros((128, 128), np.float32)
    bo[:64, :64] = 1
    bo[64:, 64:] = 1
    c["blockones"] = bo
    c["ones"] = np.ones((128, 128), np.float32)
    return np.stack([c[k] for k in ("ident", "m_plt", "m_ple", "m_pgt", "m_pge", "blockones", "ones")], 0)


CONST_NAMES = ("ident", "m_plt", "m_ple", "m_pgt", "m_pge", "blockones", "ones")

W_SPECS = {
    "w_ada": [DEPTH, D, 6 * D], "b_ada": [DEPTH, 6 * D], "norm_mix_g": [DEPTH, D], "norm_ffn_g": [DEPTH, D],
    "w_in": [DEPTH, D, IN_DIM], "rwkv_mu": [DEPTH, 2, RWKV_IN], "rwkv_w0": [DEPTH, 2, 1024],
    "rwkv_w2": [DEPTH, 2, 96, 1024], "rwkv_a0": [DEPTH, 2, 1024], "rwkv_a2": [DEPTH, 2, 96, 1024],
    "rwkv_g2": [DEPTH, 256, 1024], "rwkv_k_k": [DEPTH, 1024], "rwkv_k_a": [DEPTH, 1024],
    "rwkv_r_k": [DEPTH, 16, 64], "rwkv_ln_g": [DEPTH, 1024], "rwkv_ln_b": [DEPTH, 1024],
    "gla_conv": [DEPTH, 3, 2048], "gla_alpha_w2": [DEPTH, 2, 16, 512], "gla_alpha_b": [DEPTH, 2, 512],
    "gla_norm_g": [DEPTH, 256], "w_branch_a": [DEPTH, 1024, D], "w_branch_b": [DEPTH, 1024, D],
    "w_out": [DEPTH, D, D], "router_w": [D, NE], "router_b": [1, NE],
    "exp_w_gate": [DEPTH, NE, D, DE], "exp_w_up": [DEPTH, NE, D, DE], "exp_w_down": [DEPTH, NE, DE, D],
    "final_norm_g": [D],
}


class K:
    def __init__(s, nc, stop_after=None, dbg=False):
        s.nc = nc
        s.P = Prog(nc)
        s.stop_after = stop_after
        s.skip_rwkv = False
        s.dbg = dbg
        P = s.P
        s.inp = {}
        s.inp["x"] = P.dram("x", [2048, D], kind="ExternalInput")
        s.inp["ctx"] = P.dram("ctx", [CTX, D], kind="ExternalInput")
        s.inp["c"] = P.dram("c", [1, D], kind="ExternalInput")
        s.inp["c_ctx"] = P.dram("c_ctx", [1, D], kind="ExternalInput")
        s.inp["consts"] = P.dram("consts", [len(CONST_NAMES), 128, 128], kind="ExternalInput")
        for k, shp in W_SPECS.items():
            s.inp[k] = P.dram(k, shp, kind="ExternalInput")
        s.out = P.dram("out", [2048, D], kind="ExternalOutput")
        kd = "ExternalOutput" if dbg else "Internal"
        s.pT = P.dram("pT", [IN_DIM, T], kind=kd)
        s.xmid = P.dram("xmid", [T, D], kind=kd)
        s.xcur = P.dram("xcur", [T, D], kind=kd)
        s.yaT = P.dram("yaT", [1024, T], BF16, kind=kd)
        s.ybT = P.dram("ybT", [1024, T], BF16, kind=kd)
        s.moddbg = P.dram("moddbg", [128, 256], kind=kd)
        s.modrow = P.dram("modrow", [2, 6 * D], kind=kd)
        s.inp["sel16"] = P.dram("sel16", [16, 16, 128], kind="ExternalInput")
        s.sel16 = P.sb("sel16s", [16, 16, 128])
        P.dma(s.sel16[:], s.inp["sel16"][:])
        s.C = {}
        for i, nm in enumerate(CONST_NAMES):
            b = P.sb("c_" + nm, [128, 128])
            P.dma(b[:], s.inp["consts"][i])
            s.C[nm] = b
        s.psb = [P.ps(f"bank{i}", [128, 512]) for i in range(8)]
        s.psi = 0
        s.zeros = P.sb("zeros", [128, 512])
        P.memset(s.zeros[:], 0.0)

    def bank(s):
        b = s.psb[s.psi % 8]
        s.psi += 1
        return b

    def transpose_pack(s, rows_buf, nrows, out_cols, stack=None):
        P = s.P
        b = s.bank()
        P.tr(b[:, 0:nrows], rows_buf[0:nrows, :], s.C["ident"][0:nrows, 0:nrows])
        P.cp(out_cols[:, 0:nrows], b[:, 0:nrows])

    def stage_prologue(s):
        P = s.P
        crow = P.sb("crow", [32, 128])
        P.dma(crow[0:16, :], s.inp["c"][0].re("(c p) -> c p", p=128))
        P.dma(crow[16:32, :], s.inp["c_ctx"][0].re("(c p) -> c p", p=128))
        P.act(crow[:], crow[:], AF.Silu)
        s.cT = P.sb("cT", [128, 32])
        s.transpose_pack(crow, 32, s.cT)

    def stage_mod(s, l):
        P = s.P
        with contextlib.ExitStack() as st:
            packA = P.sb("packA", [128, 128], stack=st)
            P.dma(packA[0:96, :], s.inp["b_ada"][l].re("(c p) -> c p", p=128))
            P.dma(packA[96:112, :], s.inp["norm_mix_g"][l].re("(c p) -> c p", p=128))
            P.dma(packA[112:128, :], s.inp["norm_ffn_g"][l].re("(c p) -> c p", p=128))
            colA = s.colA
            s.transpose_pack(packA, 128, colA)
            wst = [P.sb(f"wada{i}", [128, 16, 512], stack=st) for i in range(2)]
            acc = s.bank()
            for blk in range(24):
                w = wst[blk % 2]
                src = s.inp["w_ada"][l].re("(kc p) n -> p kc n", p=128)[:, :, blk * 512:(blk + 1) * 512]
                P.dma(w[:], src)
                for j in range(4):
                    dc = blk * 4 + j
                    for kc in range(16):
                        rhs = s.cT[:].re("p (w k) -> p w k", w=2)[:, :, kc]
                        P.mm(acc[:, dc * 2:dc * 2 + 2], w[:, kc, j * 128:(j + 1) * 128], rhs,
                             start=(kc == 0), stop=(kc == 15))
            mod = s.mod
            accv = acc[:, 0:192].re("p (c w) -> p c w", w=2)
            for w_ in range(2):
                P.tt(mod[:, :, w_], accv[:, :, w_], colA[:, 0:96], ALU.add)
            for w_ in range(2):
                P.stt(s.m1[:, :, w_], mod[:, 16:32, w_], 1.0, colA[:, 96:112], ALU.add, ALU.mult)
                P.stt(s.m2[:, :, w_], mod[:, 64:80, w_], 1.0, colA[:, 112:128], ALU.add, ALU.mult)
            if s.dbg:
                P.dma(s.moddbg[:, 0:192], mod[:].re("p c w -> p (c w)"))
            mrow = P.sb("mrow", [128, 128], stack=st)
            mtmp = P.sb("mtmp", [128, 96], stack=st)
            for w_ in range(2):
                P.cp(mtmp[:], mod[:, :, w_])
                bb = s.bank()
                P.tr(bb[0:96, 0:128], mtmp[:, 0:96], s.C["ident"][:])
                P.cp(mrow[0:96, :], bb[0:96, 0:128])
                P.dma(s.modrow[w_].re("(c p) -> c p", p=128), mrow[0:96, :])
        P.barrier()

    def x_src(s, l, tt_):
        if l == 0:
            if tt_ < 2:
                return s.inp["ctx"][tt_ * 128:(tt_ + 1) * 128, :]
            return s.inp["x"][(tt_ - 2) * 128:(tt_ - 1) * 128, :]
        return s.xcur[tt_ * 128:(tt_ + 1) * 128, :]

    def norm_to_hT(s, src_view, hT, tt_, mcol, shcol, w_, xbufs, st_small, hT32=None, sh_off=0):
        P = s.P
        xt = xbufs[tt_ % 2]
        P.dma(xt[:], src_view)
        junk, ss, rstd = st_small
        P.act(junk[:], xt[:], AF.Square, accum=ss[:, 0:1])
        P.ts(ss[:, 0:1], ss[:, 0:1], 1.0 / D, ALU.mult, EPS, ALU.add)
        P.act(ss[:, 0:1], ss[:, 0:1], AF.Sqrt)
        P.recip(rstd[:, 0:1], ss[:, 0:1])
        P.ts(xt[:], xt[:], rstd[:, 0:1], ALU.mult)
        for g in range(4):
            b = s.bank()
            for j in range(4):
                kc = g * 4 + j
                P.tr(b[:, j * 128:(j + 1) * 128], xt[:, kc * 128:(kc + 1) * 128], s.C["ident"][:])
            for j in range(4):
                kc = g * 4 + j
                P.act(hT[:, kc, tt_ * 128:(tt_ + 1) * 128], b[:, j * 128:(j + 1) * 128], AF.Identity,
                      bias=shcol[:, sh_off + kc, w_:w_ + 1], scale=mcol[:, kc, w_:w_ + 1])
                if hT32 is not None:
                    P.act(hT32[:, kc, :], b[:, j * 128:(j + 1) * 128], AF.Identity,
                          bias=shcol[:, sh_off + kc, w_:w_ + 1], scale=mcol[:, kc, w_:w_ + 1])

    def tok_blocks(s, cm):
        blks = [(0, 256, lambda v: v[:, 0:256])]
        for j in range(4):
            if not cm:
                blks.append((256 + 512 * j, 512, lambda v, j=j: v[:, 256 + 512 * j:256 + 512 * (j + 1)]))
            else:
                blks.append((256 + 512 * j, 512,
                             lambda v, j=j: v[:, 256:T].re("p (r c) -> p c r", c=64)[:, 16 * j:16 * (j + 1), :]))
        return blks

    def stage_proj(s, l):
        P = s.P
        with contextlib.ExitStack() as st:
            hT = P.sb("hT", [128, 16, T], BF16, stack=st)
            xbufs = [P.sb(f"xin{i}", [128, D], stack=st) for i in range(2)]
            small = (P.sb("junk", [128, D], stack=st), P.sb("ss", [128, 1], stack=st), P.sb("rstd", [128, 1], stack=st))
            for tt_ in range(NT):
                w_ = 1 if tt_ < 2 else 0
                s.norm_to_hT(s.x_src(l, tt_), hT, tt_, s.m1, s.mod, w_, xbufs, small)
            cbs = [(i * 128, 128) for i in range(24)] + [(3072, 96), (3168, 96), (3264, 128), (3392, 128)]
            c0 = GLA_OFF
            cbs += [(c0 + i * 128, 128) for i in range(16)] + [(c0 + 2048, 16)]
            cbs += [(GLA_GATE_OFF + i * 128, 128) for i in range(8)]
            cbs += [(BR_GATE_OFF + i * 128, 128) for i in range(32)]
            wst = [P.sb(f"wst{i}", [128, 16, 128], stack=st) for i in range(2)]
            wbf = [P.sb(f"wbf{i}", [128, 16, 128], BF16, stack=st) for i in range(2)]
            ob = [P.sb(f"ob{i}", [128, T], stack=st) for i in range(2)]
            win = s.inp["w_in"][l].re("(kc p) n -> p kc n", p=128)
            for bi, (c0, w) in enumerate(cbs):
                cm = GLA_OFF <= c0 < BR_GATE_OFF
                ws, wb, o = wst[bi % 2], wbf[bi % 2], ob[bi % 2]
                P.dma(ws[:, :, 0:w], win[:, :, c0:c0 + w])
                P.cp(wb[:, :, 0:w], ws[:, :, 0:w], eng="gpsimd")
                for ti, (t0, n, fn) in enumerate(s.tok_blocks(cm)):
                    b = s.bank()
                    for kc in range(16):
                        P.mm(b[0:w, 0:n], wb[:, kc, 0:w], fn(hT[:, kc, :]),
                             start=(kc == 0), stop=(kc == 15))
                    if ti % 2 == 0:
                        P.cp(o[0:w, t0:t0 + n], b[0:w, 0:n], eng="scalar")
                    else:
                        P.cp(o[0:w, t0:t0 + n], b[0:w, 0:n], eng="vector")
                P.dma(s.pT[c0:c0 + w, :], o[0:w, :])
        P.barrier()


    def stage_rwkv(s, l):
        P = s.P
        C = s.C
        with contextlib.ExitStack() as st:
            sb = lambda n, shp, dt=F32: P.sb(n, shp, dt, stack=st)
            packB = sb("packB", [128, 128]); colB = sb("colB", [128, 128])
            P.memset(packB[:], 0.0)
            mu = s.inp["rwkv_mu"][l]
            for i in range(2):
                o = 28 * i
                P.dma(packB[o:o + 24, :], mu[i, 0:3072].re("(c p) -> c p", p=128))
                P.dma(packB[o + 24:o + 25, 0:96], mu[i:i + 1, 3072:3168])
                P.dma(packB[o + 25:o + 26, 0:96], mu[i:i + 1, 3168:3264])
                P.dma(packB[o + 26:o + 28, :], mu[i, 3264:3520].re("(c p) -> c p", p=128))
            P.dma(packB[56:72, :], s.inp["rwkv_w0"][l].re("d (c p) -> (d c) p", p=128))
            P.dma(packB[72:88, :], s.inp["rwkv_a0"][l].re("d (c p) -> (d c) p", p=128))
            P.dma(packB[88:96, :], s.inp["rwkv_k_k"][l].re("(c p) -> c p", p=128))
            P.dma(packB[96:104, :], s.inp["rwkv_k_a"][l].re("(c p) -> c p", p=128))
            P.dma(packB[104:112, :], s.inp["rwkv_r_k"][l].re("(c two) k -> c (two k)", two=2))
            P.dma(packB[112:120, :], s.inp["rwkv_ln_g"][l].re("(c p) -> c p", p=128))
            P.dma(packB[120:128, :], s.inp["rwkv_ln_b"][l].re("(c p) -> c p", p=128))
            s.transpose_pack(packB, 128, colB)
            col = lambda i: colB[:, i:i + 1]
            cf = sb("cf", [128, 28])
            P.tt(cf[:], colB[:, 0:28], colB[:, 28:56], ALU.add)
            P.ts(cf[:], cf[:], -1.0, ALU.mult, 1.0, ALU.add)

            def shift_rows(dst, X, blk, n=128):
                P.ts(dst, X, cf[0:n, blk:blk + 1], ALU.mult)
                for (a, b) in ((0, 256), (256, T)):
                    P.stt(dst[:, a + 1:b], X[:, a:b - 1], colB[0:n, blk:blk + 1], dst[:, a + 1:b], ALU.mult, ALU.add)
                    P.stt(dst[:, a:b - 1], X[:, a + 1:b], colB[0:n, 28 + blk:29 + blk], dst[:, a:b - 1], ALU.mult, ALU.add)

            xb = [sb(f"xb{i}", [128, T]) for i in range(2)]
            xbi = [0]

            def load_rows(row0, n):
                X = xb[xbi[0] % 2]
                xbi[0] += 1
                P.dma(X[0:n, :], s.pT[row0:row0 + n, :])
                return X
            txw = sb("txw", [128, T]); xaS = sb("xaS", [128, T]); sxg = sb("sxg", [128, 2, T])
            X = load_rows(3072, 96); shift_rows(txw[0:96, :], X[0:96, :], 24, 96)
            P.act(txw[0:96, :], txw[0:96, :], AF.Tanh)
            X = load_rows(3168, 96); shift_rows(xaS[0:96, :], X[0:96, :], 25, 96)
            for j in range(2):
                X = load_rows(3264 + 128 * j, 128); shift_rows(sxg[:, j, :], X[:, :], 26 + j)
                P.act(sxg[:, j, :], sxg[:, j, :], AF.Sigmoid)
            r_ = sb("r", [128, T]); k_ = sb("k", [128, T]); v_ = sb("v", [128, T]); kk_ = sb("kk", [128, T])
            vT = sb("vT", [128, NT, 128]); yacc = sb("yacc", [128, NT, 128]); yo = sb("yo", [128, T], BF16)
            w2c = sb("w2c", [96, 2, 128]); a2c = sb("a2c", [96, 2, 128]); g2c = sb("g2c", [128, 2, 128])
            RKb = sb("RKb", [128, 128])
            PhiT = sb("PhiT", [128, 128]); P.memset(PhiT[:], 0.0)
            Sb = [sb(f"S{i}", [128, 64]) for i in range(2)]
            NS = 2
            tn = ["lw", "a", "t1", "kd", "b", "cs", "lg", "E1", "E2", "E3", "E4", "d1", "bh", "kh", "bend", "kend"]
            tmp = [{n: sb(f"{n}{i}", [128, 128]) for n in tn} for i in range(NS)]
            KR = [sb(f"KR{i}", [128, 256]) for i in range(NS)]
            TMs = [sb(f"TMs{i}", [128, 384]) for i in range(NS)]
            gC = [sb(f"gC{i}", [128, 1]) for i in range(NS)]
            Php = [sb(f"Php{i}", [128, 128]) for i in range(NS)]
            Wn = [sb(f"Wn{i}", [128, 128]) for i in range(NS)]
            QT = [sb(f"QT{i}", [128, 128]) for i in range(NS)]
            Dl = [sb(f"Dl{i}", [128, 64]) for i in range(NS)]
            hn = ["ArbT", "MkT", "ArkT", "TT0", "TT1", "G"]
            htmp = [[{n: sb(f"{n}{i}{h}", [128, 128]) for n in hn} for h in range(2)] for i in range(NS)]
            XX = [[[sb(f"XX{i}{h}{j}", [128, 256]) for j in range(2)] for h in range(2)] for i in range(NS)]
            ep = {n: sb(n, [128, 128]) for n in ("ysum", "cen", "junk", "yaff", "rk", "bon")}
            mean = sb("mean", [128, 2]); var = sb("var", [128, 2])
            ident = C["ident"]

            for hp in range(8):
                hc = slice(hp * 128, (hp + 1) * 128)
                P.dma(w2c[:], s.inp["rwkv_w2"][l].re("d k n -> k d n")[:, :, hc])
                P.dma(a2c[:], s.inp["rwkv_a2"][l].re("d k n -> k d n")[:, :, hc])
                P.dma(g2c[:], s.inp["rwkv_g2"][l].re("(kc p) n -> p kc n", p=128)[:, :, hc])
                X = load_rows(hp * 128, 128); shift_rows(r_[:, :], X[:, :], hp)
                X = load_rows(1024 + hp * 128, 128); shift_rows(k_[:, :], X[:, :], 8 + hp)
                X = load_rows(2048 + hp * 128, 128); shift_rows(v_[:, :], X[:, :], 16 + hp)
                kkr, sq = xb[0], xb[1]
                P.ts(kkr[:, :], k_[:, :], col(88 + hp), ALU.mult)
                P.tt(sq[:, :], kkr[:, :], kkr[:, :], ALU.mult)
                for t0 in range(0, T, 512):
                    n = min(512, T - t0)
                    b = s.bank()
                    P.mm(b[:, 0:n], C["blockones"][:], sq[:, t0:t0 + n])
                    P.act(sq[:, t0:t0 + n], b[:, 0:n], AF.Sqrt)
                P.ts(sq[:, :], sq[:, :], 1e-12, ALU.max)
                P.recip(sq[:, :], sq[:, :])
                P.tt(kk_[:, :], kkr[:, :], sq[:, :], ALU.mult)
                for c0 in range(0, NT, 4):
                    nn = min(4, NT - c0)
                    b = s.bank()
                    for j in range(nn):
                        P.tr(b[:, j * 128:(j + 1) * 128], v_[:, (c0 + j) * 128:(c0 + j + 1) * 128], ident[:])
                    P.cp(vT[:, c0:c0 + nn, :], b[:, 0:nn * 128].re("p (c f) -> p c f", f=128), eng="scalar")
                P.ts(RKb[:], C["blockones"][:], col(104 + hp), ALU.mult)
                ci = 0
                for d in range(2):
                    order = list(range(NT)) if d == 0 else [1, 0] + list(range(17, 1, -1))
                    mS, mI, mSt = (C["m_plt"], C["m_ple"], C["m_pgt"]) if d == 0 else (C["m_pgt"], C["m_pge"], C["m_plt"])
                    Scur = 0
                    P.memset(Sb[0][:], 0.0)
                    for c in order:
                        i = ci % NS
                        ci += 1
                        t = tmp[i]
                        tok = slice(c * 128, (c + 1) * 128)
                        zb = s.bank()
                        P.mm(zb[:, 0:128], w2c[0:96, d, :], txw[0:96, tok])
                        P.mm(zb[:, 128:256], a2c[0:96, d, :], xaS[0:96, tok])
                        P.act(t["lw"][:], zb[:, 0:128], AF.Sigmoid, bias=col(56 + 8 * d + hp))
                        P.ts(t["lw"][:], t["lw"][:], -0.6065306597, ALU.mult)
                        P.act(t["a"][:], zb[:, 128:256], AF.Sigmoid, bias=col(72 + 8 * d + hp))
                        P.ts(t["t1"][:], t["a"][:], -1.0, ALU.add, col(96 + hp), ALU.mult)
                        P.stt(t["kd"][:], t["t1"][:], 1.0, k_[:, tok], ALU.add, ALU.mult)
                        P.tt(t["b"][:], kk_[:, tok], t["a"][:], ALU.mult)
                        P.scan(t["cs"][:], C["ones"][:], t["lw"][:], 0.0, ALU.mult, ALU.add)
                        tot = t["cs"][:, 127:128]
                        if d == 0:
                            lg = t["cs"]
                        else:
                            lg = t["lg"]
                            P.ts(lg[:], t["cs"][:], -1.0, ALU.mult, tot, ALU.add)
                            P.tt(lg[:], lg[:], t["lw"][:], ALU.add)
                        kr = KR[i]
                        P.act(t["E1"][:], lg[:], AF.Exp)
                        P.tt(kr[:, 128:256], r_[:, tok], t["E1"][:], ALU.mult)
                        P.tt(t["d1"][:], lg[:], t["lw"][:], ALU.subtract)
                        P.act(t["E3"][:], t["d1"][:], AF.Exp)
                        P.tt(kr[:, 0:128], kk_[:, tok], t["E3"][:], ALU.mult)
                        P.act(t["E2"][:], lg[:], AF.Exp, scale=-1.0)
                        P.tt(t["bh"][:], t["b"][:], t["E2"][:], ALU.mult)
                        P.tt(t["kh"][:], t["kd"][:], t["E2"][:], ALU.mult)
                        P.act(t["E4"][:], lg[:], AF.Exp, bias=tot, scale=-1.0)
                        P.tt(t["bend"][:], t["b"][:], t["E4"][:], ALU.mult)
                        P.tt(t["kend"][:], t["kd"][:], t["E4"][:], ALU.mult)
                        P.act(gC[i][:], tot, AF.Exp)
                        bt = s.bank()
                        P.tr(bt[:, 0:128], kr[:, 0:128], ident[:])
                        P.tr(bt[:, 128:256], t["bend"][:], ident[:])
                        P.tr(bt[:, 256:384], t["kend"][:], ident[:])
                        tm = TMs[i]
                        P.cp(tm[:], bt[:, 0:384], eng="scalar")
                        KKT, BendT, KendT = tm[:, 0:128], tm[:, 128:256], tm[:, 256:384]
                        for h in range(2):
                            hs = slice(h * 64, h * 64 + 64)
                            ht = htmp[i][h]
                            xx = XX[i][h]
                            b1 = s.bank(); P.mm(b1[:, 0:256], t["bh"][hs, :], kr[hs, :])
                            b2 = s.bank(); P.mm(b2[:, 0:256], t["kh"][hs, :], kr[hs, :])
                            b3 = s.bank(); P.mm(b3[:, 0:128], kr[hs, 0:128], t["bh"][hs, :])
                            P.tt(xx[0][:, 128:256], b1[:, 0:128], mS[:], ALU.mult)
                            P.tt(ht["ArbT"][:], b1[:, 128:256], mI[:], ALU.mult)
                            P.tt(ht["MkT"][:], b2[:, 0:128], mS[:], ALU.mult)
                            P.tt(ht["ArkT"][:], b2[:, 128:256], mI[:], ALU.mult)
                            P.tt(xx[0][:, 0:128], b3[:, 0:128], mSt[:], ALU.mult)
                            P.tt(ht["TT0"][:], ident[:], xx[0][:, 128:256], ALU.subtract, eng="gpsimd")
                            TTc, TTn = ht["TT0"], ht["TT1"]
                            for lv in range(6):
                                xc, xn = xx[lv % 2], xx[(lv + 1) % 2]
                                bx = s.bank()
                                P.mm(bx[:, 0:128], xc[:, 128:256], xc[:, 0:128])
                                if lv < 5:
                                    P.mm(bx[:, 128:256], xc[:, 0:128], xc[:, 128:256])
                                    P.cp(xn[:, 0:256], bx[:, 0:256], eng="scalar")
                                else:
                                    P.cp(xn[:, 0:128], bx[:, 0:128], eng="scalar")
                                bq = s.bank()
                                P.mm(bq[:, 0:128], xn[:, 0:128], TTc[:])
                                P.tt(TTn[:], bq[:, 0:128], TTc[:], ALU.add)
                                TTc, TTn = TTn, TTc
                            bg = s.bank(); P.mm(bg[:, 0:64], ht["MkT"][:], vT[:, c, hs])
                            P.cp(ht["G"][:, 0:64], bg[:, 0:64], eng="scalar")
                            bp = s.bank()
                            P.mm(bp[:, 0:64], TTc[:], KKT[:, hs])
                            P.mm(bp[:, 64:128], TTc[:], ht["G"][:, 0:64])
                            P.cp(Php[i][:, hs], bp[:, 0:64], eng="scalar")
                            P.act(Wn[i][:, hs], bp[:, 64:128], AF.Copy, scale=-1.0)
                        Sc = Sb[Scur]; Sn = Sb[1 - Scur]
                        for h in range(2):
                            hs = slice(h * 64, h * 64 + 64)
                            bq = s.bank()
                            P.mm(bq[:, 0:128], Php[i][:], htmp[i][h]["ArbT"][:])
                            P.tt(QT[i][hs, :], kr[hs, 128:256], bq[hs, 0:128], ALU.subtract)
                        by = s.bank()
                        for h in range(2):
                            hs = slice(h * 64, h * 64 + 64)
                            P.mm(by[:, hs], htmp[i][h]["ArkT"][:], vT[:, c, hs], start=True, stop=False)
                            P.mm(by[:, hs], htmp[i][h]["ArbT"][:], Wn[i][:, hs], start=False, stop=False)
                            P.mm(by[:, hs], QT[i][hs, :], Sc[hs, :], start=False, stop=True)
                        bf = s.bank(); P.mm(bf[:, 0:128], Php[i][:], BendT)
                        bd = s.bank()
                        P.mm(bd[:, 0:128], KendT, vT[:, c, :], start=True, stop=False)
                        P.mm(bd[:, 0:128], BendT, Wn[i][:], start=False, stop=True)
                        for h in range(2):
                            hs = slice(h * 64, h * 64 + 64)
                            P.stt(PhiT[hs, hs], ident[hs, hs], gC[i][hs, 0:1], bf[hs, hs], ALU.mult, ALU.subtract)
                            P.cp(Dl[i][hs, :], bd[hs, hs], eng="scalar")
                        bs = s.bank(); P.mm(bs[:, 0:64], PhiT[:], Sc[:])
                        P.tt(Sn[:], bs[:, 0:64], Dl[i][:], ALU.add)
                        Scur = 1 - Scur
                        if d == 0:
                            P.cp(yacc[:, c, :], by[:, 0:128], eng="scalar")
                            continue
                        ys = ep["ysum"]
                        P.tt(ys[:], by[:, 0:128], yacc[:, c, :], ALU.add)
                        P.red(mean[:], ys[:].re("p (h v) -> p h v", h=2), ALU.add)
                        P.ts(mean[:], mean[:], 1.0 / 64, ALU.mult)
                        for h in range(2):
                            hs = slice(h * 64, h * 64 + 64)
                            P.ts(ep["cen"][:, hs], ys[:, hs], mean[:, h:h + 1], ALU.subtract)
                            P.act(ep["junk"][:, hs], ep["cen"][:, hs], AF.Square, accum=var[:, h:h + 1])
                        P.ts(var[:], var[:], 1.0 / 64, ALU.mult, 64e-5, ALU.add)
                        P.act(var[:], var[:], AF.Sqrt)
                        P.recip(var[:], var[:])
                        for h in range(2):
                            hs = slice(h * 64, h * 64 + 64)
                            P.ts(ep["cen"][:, hs], ep["cen"][:, hs], var[:, h:h + 1], ALU.mult)
                        be = s.bank()
                        P.tr(be[:, 0:128], ep["cen"][:], ident[:])
                        P.act(ep["yaff"][:], be[:, 0:128], AF.Identity, bias=col(120 + hp), scale=col(112 + hp))
                        P.tt(ep["rk"][:], r_[:, tok], k_[:, tok], ALU.mult)
                        bb = s.bank()
                        P.mm(bb[:, 0:128], RKb[:], ep["rk"][:])
                        P.mm(bb[:, 128:256], g2c[:, 0, :], sxg[:, 0, tok], start=True, stop=False)
                        P.mm(bb[:, 128:256], g2c[:, 1, :], sxg[:, 1, tok], start=False, stop=True)
                        P.tt(ep["bon"][:], bb[:, 0:128], v_[:, tok], ALU.mult)
                        P.tt(ep["yaff"][:], ep["yaff"][:], ep["bon"][:], ALU.add)
                        P.tt(yo[:, tok], ep["yaff"][:], bb[:, 128:256], ALU.mult)
                P.dma(s.yaT[hc, :], yo[:, :])
        P.barrier()

    def stage_gla(s, l):
        P = s.P
        C = s.C
        ident = C["ident"]
        with contextlib.ExitStack() as st:
            sb = lambda n, shp, dt=F32: P.sb(n, shp, dt, stack=st)
            packC = sb("packC", [128, 128]); colC = sb("colC", [128, 128])
            P.memset(packC[:], 0.0)
            P.dma(packC[0:48, :], s.inp["gla_conv"][l].re("j (c p) -> (j c) p", p=128))
            P.dma(packC[48:56, :], s.inp["gla_alpha_b"][l].re("d (c p) -> (d c) p", p=128))
            P.dma(packC[56:58, :], s.inp["gla_norm_g"][l].re("(c p) -> c p", p=128))
            s.transpose_pack(packC, 64, colC)
            col = lambda i: colC[:, i:i + 1]

            def conv_rows(dst, X, blk):
                P.ts(dst, X, col(16 + blk), ALU.mult)
                for (a, b) in ((0, 256), (256, T)):
                    P.stt(dst[:, a + 1:b], X[:, a:b - 1], col(blk), dst[:, a + 1:b], ALU.mult, ALU.add)
                    P.stt(dst[:, a:b - 1], X[:, a + 1:b], col(32 + blk), dst[:, a:b - 1], ALU.mult, ALU.add)
                P.act(dst, dst, AF.Silu)
            xb = [sb(f"gxb{i}", [128, T]) for i in range(2)]
            xbi = [0]

            def load_rows(row0, n):
                X = xb[xbi[0] % 2]
                xbi[0] += 1
                P.dma(X[0:n, :], s.pT[row0:row0 + n, :])
                return X
            adS = sb("adS", [16, T])
            P.dma(adS[:], s.pT[GLA_OFF + 2048:GLA_OFF + 2064, :])
            q_ = sb("q", [128, T]); k_ = sb("gk", [128, T]); v_ = sb("gv", [128, 2, T]); gt = sb("gt", [128, 2, T])
            vT = sb("gvT", [128, NT, 256]); oacc = sb("oacc", [128, NT, 256]); yo = sb("gyo", [128, 2, T], BF16)
            aw = sb("aw", [16, 2, 128])
            Sb = [sb(f"gS{i}", [128, 256]) for i in range(2)]
            NS = 2
            tn = ["g", "cs", "gc", "E1", "E2", "E4", "qg", "kg", "kend", "attT", "kendT"]
            tmp = [{n: sb(f"g{n}{i}", [128, 128]) for n in tn} for i in range(NS)]
            gCe = [sb(f"gCe{i}", [128, 1]) for i in range(NS)]
            osum = sb("osum", [128, 256]); junk = sb("gjunk", [128, 256]); ssq = sb("ssq", [128, 1])
            for hd in range(4):
                P.dma(aw[:], s.inp["gla_alpha_w2"][l].re("d k n -> k d n")[:, :, hd * 128:(hd + 1) * 128])
                X = load_rows(GLA_OFF + hd * 128, 128); conv_rows(q_[:, :], X[:, :], hd)
                X = load_rows(GLA_OFF + 512 + hd * 128, 128); conv_rows(k_[:, :], X[:, :], 4 + hd)
                for vc in range(2):
                    X = load_rows(GLA_OFF + 1024 + hd * 256 + vc * 128, 128)
                    conv_rows(v_[:, vc, :], X[:, :], 8 + hd * 2 + vc)
                    P.dma(gt[:, vc, :], s.pT[GLA_GATE_OFF + hd * 256 + vc * 128:GLA_GATE_OFF + hd * 256 + (vc + 1) * 128, :])
                    P.act(gt[:, vc, :], gt[:, vc, :], AF.Silu)
                for c in range(NT):
                    b = s.bank()
                    for vc in range(2):
                        P.tr(b[:, vc * 128:(vc + 1) * 128], v_[:, vc, c * 128:(c + 1) * 128], ident[:])
                    P.cp(vT[:, c, :], b[:, 0:256], eng="scalar")
                ci = 0
                for d in range(2):
                    order = list(range(NT)) if d == 0 else [1, 0] + list(range(17, 1, -1))
                    mI = C["m_ple"] if d == 0 else C["m_pge"]
                    Scur = 0
                    P.memset(Sb[0][:], 0.0)
                    for c in order:
                        i = ci % NS
                        ci += 1
                        t = tmp[i]
                        tok = slice(c * 128, (c + 1) * 128)
                        zb = s.bank()
                        P.mm(zb[:, 0:128], aw[0:16, d, :], adS[0:16, tok])
                        P.act(t["g"][:], zb[:, 0:128], AF.Sigmoid, bias=col(48 + 4 * d + hd))
                        P.act(t["g"][:], t["g"][:], AF.Ln)
                        P.ts(t["g"][:], t["g"][:], 1.0 / 16, ALU.mult)
                        P.scan(t["cs"][:], C["ones"][:], t["g"][:], 0.0, ALU.mult, ALU.add)
                        tot = t["cs"][:, 127:128]
                        if d == 0:
                            gc = t["cs"]
                        else:
                            gc = t["gc"]
                            P.ts(gc[:], t["cs"][:], -1.0, ALU.mult, tot, ALU.add)
                            P.tt(gc[:], gc[:], t["g"][:], ALU.add)
                        P.act(t["E1"][:], gc[:], AF.Exp)
                        P.stt(t["qg"][:], q_[:, tok], 128 ** -0.5, t["E1"][:], ALU.mult, ALU.mult)
                        P.act(t["E2"][:], gc[:], AF.Exp, scale=-1.0)
                        P.tt(t["kg"][:], k_[:, tok], t["E2"][:], ALU.mult)
                        P.act(t["E4"][:], gc[:], AF.Exp, bias=tot, scale=-1.0)
                        P.tt(t["kend"][:], k_[:, tok], t["E4"][:], ALU.mult)
                        P.act(gCe[i][:], tot, AF.Exp)
                        ba = s.bank(); P.mm(ba[:, 0:128], t["kg"][:], t["qg"][:])
                        P.tt(t["attT"][:], ba[:, 0:128], mI[:], ALU.mult)
                        bt = s.bank(); P.tr(bt[:, 0:128], t["kend"][:], ident[:])
                        P.cp(t["kendT"][:], bt[:, 0:128], eng="scalar")
                        Sc = Sb[Scur]; Sn = Sb[1 - Scur]
                        bo = s.bank()
                        P.mm(bo[:, 0:256], t["attT"][:], vT[:, c, :], start=True, stop=False)
                        P.mm(bo[:, 0:256], t["qg"][:], Sc[:], start=False, stop=True)
                        bs = s.bank(); P.mm(bs[:, 0:256], t["kendT"][:], vT[:, c, :])
                        P.stt(Sn[:], Sc[:], gCe[i][:, 0:1], bs[:, 0:256], ALU.mult, ALU.add)
                        Scur = 1 - Scur
                        if d == 0:
                            P.cp(oacc[:, c, :], bo[:, 0:256], eng="scalar")
                            continue
                        P.tt(osum[:], bo[:, 0:256], oacc[:, c, :], ALU.add)
                        P.act(junk[:], osum[:], AF.Square, accum=ssq[:, 0:1])
                        P.ts(ssq[:], ssq[:], 1.0 / 256, ALU.mult, EPS, ALU.add)
                        P.act(ssq[:], ssq[:], AF.Sqrt)
                        P.recip(ssq[:], ssq[:])
                        P.ts(osum[:], osum[:], ssq[:, 0:1], ALU.mult)
                        be = s.bank()
                        for vc in range(2):
                            P.tr(be[:, vc * 128:(vc + 1) * 128], osum[:, vc * 128:(vc + 1) * 128], ident[:])
                        for vc in range(2):
                            P.stt(yo[:, vc, tok], be[:, vc * 128:(vc + 1) * 128], col(56 + vc), gt[:, vc, tok], ALU.mult, ALU.mult)
                for vc in range(2):
                    P.dma(s.ybT[hd * 256 + vc * 128:hd * 256 + (vc + 1) * 128, :], yo[:, vc, :])
        P.barrier()


    def stage_merge(s, l, last):
        P = s.P
        with contextlib.ExitStack() as st:
            sb = lambda n, shp, dt=F32: P.sb(n, shp, dt, stack=st)
            yaS = sb("yaS", [128, 8, T], BF16); ybS = sb("ybS", [128, 8, T], BF16)
            for kc in range(8):
                P.dma(yaS[:, kc, :], s.yaT[kc * 128:(kc + 1) * 128, :])
                P.dma(ybS[:, kc, :], s.ybT[kc * 128:(kc + 1) * 128, :])
            gbc = sb("gbc", [128, 2, D])
            for w_ in range(2):
                P.dma(gbc[:, w_, :], s.modrow[w_, 2 * D:3 * D].pb(128))
            wa_st = sb("wa_st", [128, 8, 128]); wb_st = sb("wb_st", [128, 8, 128])
            wa = sb("wa", [128, 8, 128], BF16); wb = sb("wb", [128, 8, 128], BF16)
            gaS = sb("gaS", [128, 512]); gbS = sb("gbS", [128, 512])
            mix = sb("mix", [128, 16, 512], BF16)
            m1 = sb("mm1", [128, 512]); m2 = sb("mm2", [128, 512])
            wo_st = sb("wo_st", [128, 16, 512]); wo = sb("wo", [128, 16, 512], BF16)
            xt = sb("mxt", [128, 512]); yt = sb("myt", [128, 512])
            blocks = [(256 + 512 * j, 512, 0, j) for j in range(4)]
            if not last:
                blocks = [(0, 256, 1, -1)] + blocks
            wav = s.inp["w_branch_a"][l].re("(kc p) n -> p kc n", p=128)
            wbv = s.inp["w_branch_b"][l].re("(kc p) n -> p kc n", p=128)
            wov = s.inp["w_out"][l].re("(kc p) n -> p kc n", p=128)
            for (t0, n, w_, j) in blocks:
                for dc in range(16):
                    dcs = slice(dc * 128, (dc + 1) * 128)
                    P.dma(wa_st[:], wav[:, :, dcs]); P.cp(wa[:], wa_st[:], eng="gpsimd")
                    P.dma(wb_st[:], wbv[:, :, dcs]); P.cp(wb[:], wb_st[:], eng="gpsimd")
                    P.dma(gaS[:, 0:n], s.pT[BR_GATE_OFF + dc * 128:BR_GATE_OFF + (dc + 1) * 128, t0:t0 + n])
                    P.dma(gbS[:, 0:n], s.pT[BR_GATE_OFF + D + dc * 128:BR_GATE_OFF + D + (dc + 1) * 128, t0:t0 + n])
                    bA = s.bank()
                    for kc in range(8):
                        P.mm(bA[:, 0:n], wa[:, kc, :], yaS[:, kc, t0:t0 + n], start=(kc == 0), stop=(kc == 7))
                    bB = s.bank()
                    for kc in range(8):
                        if j < 0:
                            rhs = ybS[:, kc, 0:256]
                        else:
                            rhs = ybS[:, kc, 256:T].re("p (c r) -> p r c", r=32)[:, 8 * j:8 * j + 8, :]
                        P.mm(bB[:, 0:n], wb[:, kc, :], rhs, start=(kc == 0), stop=(kc == 7))
                    P.act(gaS[:, 0:n], gaS[:, 0:n], AF.Sigmoid)
                    P.act(gbS[:, 0:n], gbS[:, 0:n], AF.Sigmoid)
                    P.tt(m1[:, 0:n], bA[:, 0:n], gaS[:, 0:n], ALU.mult)
                    P.tt(m2[:, 0:n], bB[:, 0:n], gbS[:, 0:n], ALU.mult)
                    P.tt(mix[:, dc, 0:n], m1[:, 0:n], m2[:, 0:n], ALU.add, eng="gpsimd")
                for db in range(4):
                    dbs = slice(db * 512, (db + 1) * 512)
                    P.dma(wo_st[:], wov[:, :, dbs]); P.cp(wo[:], wo_st[:], eng="gpsimd")
                    for ti in range(n // 128):
                        tile = t0 // 128 + ti
                        rows = slice(tile * 128, (tile + 1) * 128)
                        bo = s.bank()
                        for kc in range(16):
                            P.mm(bo[:, 0:512], mix[:, kc, ti * 128:(ti + 1) * 128], wo[:, kc, :], start=(kc == 0), stop=(kc == 15))
                        P.dma(xt[:], s.x_src(l, tile)[:, dbs])
                        P.tt(yt[:], bo[:, 0:512], gbc[:, w_, dbs], ALU.mult)
                        P.tt(yt[:], yt[:], xt[:], ALU.add)
                        P.dma(s.xmid[rows, dbs], yt[:])
        P.barrier()

    def stage_ffn(s, l, last):
        P = s.P
        C = s.C
        tiles = list(range(2, NT)) if last else list(range(NT))
        groups = [tiles[i:i + 5] for i in range(0, len(tiles), 5)] if not last else [tiles[i:i + 4] for i in range(0, 16, 4)]
        with contextlib.ExitStack() as st0:
            sb0 = lambda n, shp, dt=F32: P.sb(n, shp, dt, stack=st0)
            rw = sb0("rw", [128, 16, NE]); P.dma(rw[:], s.inp["router_w"][:].re("(kc p) e -> p kc e", p=128))
            rb = sb0("rb", [128, NE]); P.dma(rb[:], s.inp["router_b"][0].pb(128))
            hT2 = sb0("hT2", [128, 16, 640], BF16); acc = sb0("acc", [128, 5, D]); gatesT = sb0("gatesT", [16, 640])
            for grp in groups:
                ng = len(grp); ntok = ng * 128
                tbl = [(a, min(512, ntok - a)) for a in range(0, ntok, 512)]
                with contextlib.ExitStack() as stA:
                    sbA = lambda n, shp, dt=F32: P.sb(n, shp, dt, stack=stA)
                    xb1 = sbA("fx", [128, D])
                    small = (sbA("fjunk", [128, D]), sbA("fss", [128, 1]), sbA("frstd", [128, 1]))
                    hT32 = sbA("hT32", [128, 16, 128])
                    q = {n: sbA("r_" + n, [128, 16]) for n in ("sc", "sel", "msk", "eq1", "msk2", "eq2", "wts", "gates")}
                    g4 = {n: sbA("r4_" + n, [128, 4]) for n in ("gs", "t4", "gmask", "pen")}
                    r1 = {n: sbA("r1_" + n, [128, 1]) for n in ("gmax", "m1", "m2", "ssum")}
                    for ti, tile in enumerate(grp):
                        w_ = 1 if tile < 2 else 0
                        s.norm_to_hT(s.xmid[tile * 128:(tile + 1) * 128, :], hT2, ti, s.m2, s.mod, w_, [xb1, xb1], small,
                                     hT32=hT32, sh_off=48)
                        br = s.bank()
                        for kc in range(16):
                            P.mm(br[:, 0:NE], hT32[:, kc, :], rw[:, kc, :], start=(kc == 0), stop=(kc == 15))
                        P.act(q["sc"][:], br[:, 0:NE], AF.Sigmoid)
                        P.tt(q["sel"][:], q["sc"][:], rb[:], ALU.add)
                        s4 = q["sel"][:].re("p (g j) -> p g j", j=4)
                        first = True
                        for a_ in range(4):
                            for b_ in range(a_ + 1, 4):
                                if first:
                                    P.tt(g4["gs"][:], s4[:, :, a_], s4[:, :, b_], ALU.add)
                                    first = False
                                else:
                                    P.tt(g4["t4"][:], s4[:, :, a_], s4[:, :, b_], ALU.add)
                                    P.tt(g4["gs"][:], g4["gs"][:], g4["t4"][:], ALU.max)
                        P.red(r1["gmax"][:], g4["gs"][:], ALU.max)
                        P.ts(g4["gmask"][:], g4["gs"][:], r1["gmax"][:, 0:1], ALU.is_ge)
                        P.ts(g4["pen"][:], g4["gmask"][:], -1.0, ALU.add, 1e9, ALU.mult)
                        m4 = q["msk"][:].re("p (g j) -> p g j", j=4)
                        for j_ in range(4):
                            P.tt(m4[:, :, j_], s4[:, :, j_], g4["pen"][:], ALU.add)
                        P.red(r1["m1"][:], q["msk"][:], ALU.max)
                        P.ts(q["eq1"][:], q["msk"][:], r1["m1"][:, 0:1], ALU.is_equal)
                        P.stt(q["msk2"][:], q["eq1"][:], -1e9, q["msk"][:], ALU.mult, ALU.add)
                        P.red(r1["m2"][:], q["msk2"][:], ALU.max)
                        P.ts(q["eq2"][:], q["msk2"][:], r1["m2"][:, 0:1], ALU.is_equal)
                        P.tt(q["eq1"][:], q["eq1"][:], q["eq2"][:], ALU.add)
                        P.tt(q["wts"][:], q["sc"][:], q["eq1"][:], ALU.mult)
                        P.red(r1["ssum"][:], q["wts"][:], ALU.add)
                        P.recip(r1["ssum"][:], r1["ssum"][:])
                        P.ts(q["gates"][:], q["wts"][:], r1["ssum"][:, 0:1], ALU.mult)
                        bt = s.bank()
                        P.tr(bt[0:NE, 0:128], q["gates"][:, 0:NE], C["ident"][:])
                        P.cp(gatesT[0:NE, ti * 128:(ti + 1) * 128], bt[0:NE, 0:128])
                P.barrier()
                with contextlib.ExitStack() as stB:
                    sbB = lambda n, shp, dt=F32: P.sb(n, shp, dt, stack=stB)
                    wd = sbB("wd", [128, 11, D], BF16)
                    stg = [sbB(f"stg{i}", [128, D]) for i in range(3)]
                    wgb = sbB("wgb", [128, 16, 128], BF16); wub = sbB("wub", [128, 16, 128], BF16)
                    actE = sbB("actE", [128, 11, 640], BF16); gb_ = sbB("gb_", [128, 640])
                    sg = sbB("sg", [128, 512]); tq = sbB("tq", [128, 512])
                    for e in range(NE):
                        for (a, n) in tbl:
                            bk = s.bank()
                            P.mm(bk[:, 0:n], s.sel16[0:16, e, :], gatesT[0:16, a:a + n])
                            P.cp(gb_[:, a:a + n], bk[:, 0:n], eng="scalar")
                        wgv = s.inp["exp_w_gate"][l, e].re("(kc p) f -> p kc f", p=128)
                        wuv = s.inp["exp_w_up"][l, e].re("(kc p) f -> p kc f", p=128)
                        for fc in range(11):
                            fs = slice(fc * 128, (fc + 1) * 128)
                            s0 = stg[0][:].re("p (kc f) -> p kc f", f=128)
                            s1 = stg[1][:].re("p (kc f) -> p kc f", f=128)
                            P.dma(s0, wgv[:, :, fs]); P.cp(wgb[:], s0, eng="gpsimd")
                            P.dma(s1, wuv[:, :, fs]); P.cp(wub[:], s1, eng="gpsimd")
                            P.dma(stg[2][:], s.inp["exp_w_down"][l, e, fs, :]); P.cp(wd[:, fc, :], stg[2][:], eng="gpsimd")
                            for (a, n) in tbl:
                                bg = s.bank()
                                for kc in range(16):
                                    P.mm(bg[:, 0:n], wgb[:, kc, :], hT2[:, kc, a:a + n], start=(kc == 0), stop=(kc == 15))
                                bu = s.bank()
                                for kc in range(16):
                                    P.mm(bu[:, 0:n], wub[:, kc, :], hT2[:, kc, a:a + n], start=(kc == 0), stop=(kc == 15))
                                P.act(sg[:, 0:n], bg[:, 0:n], AF.Silu)
                                P.tt(tq[:, 0:n], bu[:, 0:n], gb_[:, a:a + n], ALU.mult)
                                P.tt(actE[:, fc, a:a + n], sg[:, 0:n], tq[:, 0:n], ALU.mult, eng="gpsimd")
                        for ti in range(ng):
                            for db in range(4):
                                dbs = slice(db * 512, (db + 1) * 512)
                                bo = s.bank()
                                for fc in range(11):
                                    P.mm(bo[:, 0:512], actE[:, fc, ti * 128:(ti + 1) * 128], wd[:, fc, dbs], start=(fc == 0), stop=(fc == 10))
                                if e == 0:
                                    P.cp(acc[:, ti, dbs], bo[:, 0:512], eng="scalar")
                                else:
                                    P.tt(acc[:, ti, dbs], bo[:, 0:512], acc[:, ti, dbs], ALU.add)
                P.barrier()
                with contextlib.ExitStack() as stC:
                    sbC = lambda n, shp, dt=F32: P.sb(n, shp, dt, stack=stC)
                    gbc = sbC("fgbc", [128, 2, D])
                    for w_ in range(2):
                        P.dma(gbc[:, w_, :], s.modrow[w_, 5 * D:6 * D].pb(128))
                    fng = sbC("fng", [128, D])
                    if last:
                        P.dma(fng[:], s.inp["final_norm_g"][:].pb(128))
                    xt = sbC("cxt", [128, D]); yt = sbC("cyt", [128, D]); junk = sbC("cjunk", [128, D])
                    ss = sbC("css", [128, 1])
                    for ti, tile in enumerate(grp):
                        w_ = 1 if tile < 2 else 0
                        rows = slice(tile * 128, (tile + 1) * 128)
                        P.dma(xt[:], s.xmid[rows, :])
                        P.tt(yt[:], acc[:, ti, :], gbc[:, w_, :], ALU.mult)
                        P.tt(yt[:], yt[:], xt[:], ALU.add)
                        if not last:
                            P.dma(s.xcur[rows, :], yt[:])
                        else:
                            P.act(junk[:], yt[:], AF.Square, accum=ss[:, 0:1])
                            P.ts(ss[:], ss[:], 1.0 / D, ALU.mult, EPS, ALU.add)
                            P.act(ss[:], ss[:], AF.Sqrt)
                            P.recip(ss[:], ss[:])
                            P.stt(yt[:], yt[:], ss[:, 0:1], fng[:], ALU.mult, ALU.mult)
                            P.dma(s.out[(tile - 2) * 128:(tile - 1) * 128, :], yt[:])
                P.barrier()

    def build(s):
        P = s.P
        s.colA = P.sb("colA", [128, 128])
        s.mod = P.sb("mod", [128, 96, 2])
        s.m1 = P.sb("m1", [128, 16, 2])
        s.m2 = P.sb("m2", [128, 16, 2])
        s.stage_prologue()
        fin = []
        for l in range(DEPTH):
            s.stage_mod(l)
            if s.stop_after == ("mod", l):
                break
            s.stage_proj(l)
            if s.stop_after == ("proj", l):
                break
            if not s.skip_rwkv:
                s.stage_rwkv(l)
            if s.stop_after == ("rwkv", l):
                break
            s.stage_gla(l)
            if s.stop_after == ("gla", l):
                break
            last = (l == DEPTH - 1)
            s.stage_merge(l, last)
            if s.stop_after == ("merge", l):
                break
            s.stage_ffn(l, last)
            if s.stop_after == ("ffn", l):
                break
        P.barrier()
        P.final_tokens = [(sk, v) for sk, v in P.dma_tgt.items()]
        P.emit()
        P.st.close()


def build_nc(stop_after=None, dbg=False):
    nc = bass.Bass("TRN2", target_bir_lowering=False)
    k = K(nc, stop_after=stop_after, dbg=dbg)
    k.build()
    return nc


def make_in_maps(inputs, cores):
    consts = make_consts()
    shared = {}
    for k, shp in W_SPECS.items():
        shared[k] = np.ascontiguousarray(np.asarray(inputs[k], dtype=np.float32).reshape(shp))
    maps = []
    for b in cores:
        m = dict(shared)
        m["x"] = np.ascontiguousarray(inputs["x"][b])
        m["ctx"] = np.ascontiguousarray(inputs["ctx"][b])
        m["c"] = np.ascontiguousarray(inputs["c"][b:b + 1])
        m["c_ctx"] = np.ascontiguousarray(np.asarray(inputs["c_ctx"]).reshape(1, D))
        m["consts"] = consts
        sel = np.zeros((16, 16, 128), np.float32)
        for e in range(16):
            sel[e, e, :] = 1.0
        m["sel16"] = sel
        maps.append(m)
    return maps


def kernel(**inputs):
    nc = build_nc()
    maps = make_in_maps(inputs, list(range(8)))
    res = run_bass_kernel_spmd(nc, maps, core_ids=list(range(8)))
    return np.stack([r["out"] for r in res.results], 0).astype(np.float32)
```

```python
import contextlib
import numpy as np
import concourse.bass as bass
import concourse.mybir as mybir
from concourse.bass_utils import run_bass_kernel_spmd

F32 = mybir.dt.float32
BF16 = mybir.dt.bfloat16
AF = mybir.ActivationFunctionType
ALU = mybir.AluOpType
AX = mybir.AxisListType

ENGS = ("tensor", "vector", "scalar", "gpsimd", "sync")
N_DMA_SEMS = 12

D = 2048
T = 2304
NT = 18
CTX = 256
DEPTH = 2
RWKV_IN = 3520
GLA_OFF = 3520
GLA_GATE_OFF = 5584
BR_GATE_OFF = 6608
IN_DIM = 10704
NE = 16
DE = 1408
EPS = 1e-6


class View:
    def __init__(s, buf, ap, keys):
        s.buf, s.ap, s.keys = buf, ap, keys

    def re(s, pat, **kw):
        return View(s.buf, s.ap.rearrange(pat, **kw), s.keys)

    def __getitem__(s, idx):
        return View(s.buf, s.ap[idx], s.keys)

    def pb(s, n):
        return View(s.buf, s.ap.partition_broadcast(n), s.keys)


class Buf:
    def __init__(s, name, h, nsub=None):
        s.name, s.h, s.nsub = name, h, nsub

    def allkeys(s):
        if s.nsub:
            return [(s.name, i) for i in range(s.nsub)]
        return [s.name]

    def __getitem__(s, idx):
        return View(s, s.h[idx], s.allkeys())

    def sub(s, i, idx):
        return View(s, s.h[idx], [(s.name, i)])


class Prog:
    def __init__(s, nc):
        s.nc = nc
        s.st = contextlib.ExitStack()
        s.ops = {e: [] for e in ENGS}
        s.cnt = {e: 0 for e in ENGS}
        s.seen = {e: {} for e in ENGS}
        s.last_w = {}
        s.readers = {}
        s.dma_rr = {e: 0 for e in ENGS}
        s.dma_tgt = {}
        s.final_tokens = []
        s.nbuf = 0

    def sb(s, name, shape, dt=F32, nsub=None, stack=None):
        s.nbuf += 1
        nm = f"{name}_{s.nbuf}"
        h = (stack or s.st).enter_context(s.nc.sbuf_tensor(nm, list(shape), dt))
        return Buf(nm, h, nsub)

    def ps(s, name, shape, dt=F32, stack=None):
        s.nbuf += 1
        nm = f"{name}_{s.nbuf}"
        h = (stack or s.st).enter_context(s.nc.psum_tensor(nm, list(shape), dt))
        return Buf(nm, h)

    def dram(s, name, shape, dt=F32, kind="Internal", nsub=None):
        h = s.nc.dram_tensor(name, list(shape), dt, kind=kind).ap()
        return Buf(name, h, nsub)

    def _need(s, eng, tok, waits):
        if tok is None:
            return
        sk, v = tok
        if sk == ("e", eng) and eng == "tensor":
            return
        if s.seen[eng].get(sk, 0) >= v:
            return
        s.seen[eng][sk] = v
        waits.append((sk, v))

    def _deps(s, eng, reads, writes):
        waits = []
        for k in reads:
            s._need(eng, s.last_w.get(k), waits)
        for k in writes:
            s._need(eng, s.last_w.get(k), waits)
            for t in s.readers.get(k, ()):
                s._need(eng, t, waits)
        return waits

    def _commit(s, tok, reads, writes):
        for k in reads:
            s.readers.setdefault(k, []).append(tok)
        for k in writes:
            s.last_w[k] = tok
            s.readers[k] = []

    def op(s, eng, fn, reads=(), writes=()):
        waits = s._deps(eng, reads, writes)
        s.cnt[eng] += 1
        tok = (("e", eng), s.cnt[eng])
        s.ops[eng].append((waits, fn, tok))
        s._commit(tok, reads, writes)
        return tok

    def _dma(s, eng, fn, reads=(), writes=()):
        waits = s._deps(eng, reads, writes)
        slot = s.dma_rr[eng] % N_DMA_SEMS
        s.dma_rr[eng] += 1
        sk = ("d", eng, slot)
        prev = s.dma_tgt.get(sk, 0)
        if prev:
            s._need(eng, (sk, prev), waits)
        tgt = prev + 16
        s.dma_tgt[sk] = tgt
        tok = (sk, tgt)
        s.ops[eng].append((waits, fn, tok))
        s._commit(tok, reads, writes)
        return tok

    def barrier(s):
        toks = [(("e", e), s.cnt[e]) for e in ENGS if s.cnt[e]]
        toks += [(sk, v) for sk, v in s.dma_tgt.items()]
        for e in ENGS:
            waits = []
            for t in toks:
                s._need(e, t, waits)
            if waits:
                s.ops[e].append((waits, None, None))
        s.last_w.clear()
        s.readers.clear()

    def emit(s):
        nc = s.nc
        with contextlib.ExitStack() as st:
            semh = {}
            for e in ENGS:
                semh[("e", e)] = st.enter_context(nc.semaphore(f"s_{e}"))
            for e in ENGS:
                for i in range(min(N_DMA_SEMS, s.dma_rr[e])):
                    semh[("d", e, i)] = st.enter_context(nc.semaphore(f"d_{e}_{i}"))
            block = st.enter_context(nc.Block())
            fin = []
            for t in s.final_tokens:
                s._need("sync", t, fin)
            for e in ENGS:
                ops = s.ops[e]
                extra = fin if e == "sync" else []
                if not ops and not extra:
                    continue

                def body(engine, ops=ops, extra=extra):
                    for waits, fn, tok in ops:
                        for sk, v in waits:
                            engine.wait_ge(semh[sk], v)
                        if fn is None:
                            continue
                        ins = fn(engine)
                        sk, v = tok
                        ins.then_inc(semh[sk], 16 if sk[0] == "d" else 1)
                    for sk, v in extra:
                        engine.wait_ge(semh[sk], v)
                getattr(block, e)(body)

    @staticmethod
    def _k(*vs):
        ks = []
        for v in vs:
            if isinstance(v, View):
                ks += v.keys
        return ks

    @staticmethod
    def _a(v):
        return v.ap if isinstance(v, View) else v

    def mm(s, out, lhsT, rhs, start=True, stop=True):
        return s.op("tensor", lambda e: e.matmul(out.ap, lhsT.ap, rhs.ap, start=start, stop=stop),
                    reads=s._k(lhsT, rhs), writes=s._k(out))

    def tr(s, out, in_, ident):
        return s.op("tensor", lambda e: e.transpose(out.ap, in_.ap, ident.ap),
                    reads=s._k(in_, ident), writes=s._k(out))

    def act(s, out, in_, func, bias=None, scale=None, accum=None):
        kw = {}
        if bias is not None:
            kw["bias"] = s._a(bias)
        if scale is not None:
            kw["scale"] = s._a(scale)
        if accum is not None:
            kw["accum_out"] = accum.ap
        return s.op("scalar", lambda e: e.activation(out.ap, in_.ap, func, **kw),
                    reads=s._k(in_, bias, scale), writes=s._k(out, accum))

    def tt(s, out, a, b, op, eng="vector"):
        return s.op(eng, lambda e: e.tensor_tensor(out.ap, a.ap, b.ap, op),
                    reads=s._k(a, b), writes=s._k(out))

    def ts(s, out, a, s1, op0, s2=None, op1=None, eng="vector", accum=None):
        kw = {}
        if op1 is not None:
            kw["op1"] = op1
        if accum is not None:
            kw["accum_out"] = accum.ap
        return s.op(eng, lambda e: e.tensor_scalar(out.ap, a.ap, s._a(s1), s._a(s2), op0, **kw),
                    reads=s._k(a, s1, s2), writes=s._k(out, accum))

    def stt(s, out, a, sc, b, op0, op1):
        return s.op("vector", lambda e: e.scalar_tensor_tensor(out.ap, a.ap, s._a(sc), b.ap, op0, op1),
                    reads=s._k(a, sc, b), writes=s._k(out))

    def cp(s, out, in_, eng="vector"):
        if eng == "scalar":
            return s.op("scalar", lambda e: e.copy(out.ap, in_.ap), reads=s._k(in_), writes=s._k(out))
        return s.op(eng, lambda e: e.tensor_copy(out.ap, in_.ap), reads=s._k(in_), writes=s._k(out))

    def memset(s, out, val, eng="vector"):
        return s.op(eng, lambda e: e.memset(out.ap, val), writes=s._k(out))

    def red(s, out, in_, op, axis=AX.X):
        return s.op("vector", lambda e: e.tensor_reduce(out.ap, in_.ap, axis, op),
                    reads=s._k(in_), writes=s._k(out))

    def recip(s, out, in_):
        return s.op("vector", lambda e: e.reciprocal(out.ap, in_.ap), reads=s._k(in_), writes=s._k(out))

    def scan(s, out, d0, d1, initial, op0, op1):
        return s.op("vector", lambda e: e.tensor_tensor_scan(out.ap, d0.ap, d1.ap, s._a(initial), op0, op1),
                    reads=s._k(d0, d1, initial), writes=s._k(out))

    def dma(s, out, in_, eng="sync"):
        return s._dma(eng, lambda e: e.dma_start(out=out.ap, in_=in_.ap), reads=s._k(in_), writes=s._k(out))


def make_consts():
    c = {}
    c["ident"] = np.eye(128, dtype=np.float32)
    p = np.arange(128)[:, None]
    f = np.arange(128)[None, :]
    c["m_plt"] = (p < f).astype(np.float32)
    c["m_ple"] = (p <= f).astype(np.float32)
    c["m_pgt"] = (p > f).astype(np.float32)
    c["m_pge"] = (p >= f).astype(np.float32)
    bo = np.zeros((128, 128), np.float32)
    bo[:64, :64] = 1
    bo[64:, 64:] = 1
    c["blockones"] = bo
    c["ones"] = np.ones((128, 128), np.float32)
    return np.stack([c[k] for k in ("ident", "m_plt", "m_ple", "m_pgt", "m_pge", "blockones", "ones")], 0)


CONST_NAMES = ("ident", "m_plt", "m_ple", "m_pgt", "m_pge", "blockones", "ones")

W_SPECS = {
    "w_ada": [DEPTH, D, 6 * D], "b_ada": [DEPTH, 6 * D], "norm_mix_g": [DEPTH, D], "norm_ffn_g": [DEPTH, D],
    "w_in": [DEPTH, D, IN_DIM], "rwkv_mu": [DEPTH, 2, RWKV_IN], "rwkv_w0": [DEPTH, 2, 1024],
    "rwkv_w2": [DEPTH, 2, 96, 1024], "rwkv_a0": [DEPTH, 2, 1024], "rwkv_a2": [DEPTH, 2, 96, 1024],
    "rwkv_g2": [DEPTH, 256, 1024], "rwkv_k_k": [DEPTH, 1024], "rwkv_k_a": [DEPTH, 1024],
    "rwkv_r_k": [DEPTH, 16, 64], "rwkv_ln_g": [DEPTH, 1024], "rwkv_ln_b": [DEPTH, 1024],
    "gla_conv": [DEPTH, 3, 2048], "gla_alpha_w2": [DEPTH, 2, 16, 512], "gla_alpha_b": [DEPTH, 2, 512],
    "gla_norm_g": [DEPTH, 256], "w_branch_a": [DEPTH, 1024, D], "w_branch_b": [DEPTH, 1024, D],
    "w_out": [DEPTH, D, D], "router_w": [D, NE], "router_b": [1, NE],
    "exp_w_gate": [DEPTH, NE, D, DE], "exp_w_up": [DEPTH, NE, D, DE], "exp_w_down": [DEPTH, NE, DE, D],
    "final_norm_g": [D],
}


class K:
    def __init__(s, nc, stop_after=None, dbg=False):
        s.nc = nc
        s.P = Prog(nc)
        s.stop_after = stop_after
        s.skip_rwkv = False
        s.dbg = dbg
        P = s.P
        s.inp = {}
        s.inp["x"] = P.dram("x", [2048, D], kind="ExternalInput")
        s.inp["ctx"] = P.dram("ctx", [CTX, D], kind="ExternalInput")
        s.inp["c"] = P.dram("c", [1, D], kind="ExternalInput")
        s.inp["c_ctx"] = P.dram("c_ctx", [1, D], kind="ExternalInput")
        s.inp["consts"] = P.dram("consts", [len(CONST_NAMES), 128, 128], kind="ExternalInput")
        for k, shp in W_SPECS.items():
            s.inp[k] = P.dram(k, shp, kind="ExternalInput")
        s.out = P.dram("out", [2048, D], kind="ExternalOutput")
        kd = "ExternalOutput" if dbg else "Internal"
        s.pT = P.dram("pT", [IN_DIM, T], kind=kd)
        s.xmid = P.dram("xmid", [T, D], kind=kd)
        s.xcur = P.dram("xcur", [T, D], kind=kd)
        s.yaT = P.dram("yaT", [1024, T], BF16, kind=kd)
        s.ybT = P.dram("ybT", [1024, T], BF16, kind=kd)
        s.moddbg = P.dram("moddbg", [128, 256], kind=kd)
        s.modrow = P.dram("modrow", [2, 6 * D], kind=kd)
        s.inp["sel16"] = P.dram("sel16", [16, 16, 128], kind="ExternalInput")

        s.C = {}
        for i, nm in enumerate(CONST_NAMES):
            b = P.sb("c_" + nm, [128, 128])
            P.dma(b[:], s.inp["consts"][i])
            s.C[nm] = b
        s.psb = [P.ps(f"bank{i}", [128, 512]) for i in range(8)]
        s.psi = 0

    def bank(s):
        b = s.psb[s.psi % 8]
        s.psi += 1
        return b

    def transpose_pack(s, rows_buf, nrows, out_cols, stack=None):
        P = s.P
        b = s.bank()
        P.tr(b[:, 0:nrows], rows_buf[0:nrows, :], s.C["ident"][0:nrows, 0:nrows])
        P.cp(out_cols[:, 0:nrows], b[:, 0:nrows])

    def stage_prologue(s):
        P = s.P
        crow = P.sb("crow", [32, 128])
        P.dma(crow[0:16, :], s.inp["c"][0].re("(c p) -> c p", p=128))
        P.dma(crow[16:32, :], s.inp["c_ctx"][0].re("(c p) -> c p", p=128))
        P.act(crow[:], crow[:], AF.Silu)
        s.cT = P.sb("cT", [128, 32])
        s.transpose_pack(crow, 32, s.cT)

    def stage_mod(s, l):
        P = s.P
        with contextlib.ExitStack() as st:
            packA = P.sb("packA", [128, 128], stack=st)
            P.dma(packA[0:96, :], s.inp["b_ada"][l].re("(c p) -> c p", p=128))
            P.dma(packA[96:112, :], s.inp["norm_mix_g"][l].re("(c p) -> c p", p=128))
            P.dma(packA[112:128, :], s.inp["norm_ffn_g"][l].re("(c p) -> c p", p=128))
            colA = s.colA
            s.transpose_pack(packA, 128, colA)
            wst = [P.sb(f"wada{i}", [128, 16, 512], stack=st) for i in range(2)]
            acc = s.bank()
            for blk in range(24):
                w = wst[blk % 2]
                src = s.inp["w_ada"][l].re("(kc p) n -> p kc n", p=128)[:, :, blk * 512:(blk + 1) * 512]
                P.dma(w[:], src)
                for j in range(4):
                    dc = blk * 4 + j
                    for kc in range(16):
                        rhs = s.cT[:].re("p (w k) -> p w k", w=2)[:, :, kc]
                        P.mm(acc[:, dc * 2:dc * 2 + 2], w[:, kc, j * 128:(j + 1) * 128], rhs,
                             start=(kc == 0), stop=(kc == 15))
            mod = s.mod
            accv = acc[:, 0:192].re("p (c w) -> p c w", w=2)
            for w_ in range(2):
                P.tt(mod[:, :, w_], accv[:, :, w_], colA[:, 0:96], ALU.add)
            for w_ in range(2):
                P.stt(s.m1[:, :, w_], mod[:, 16:32, w_], 1.0, colA[:, 96:112], ALU.add, ALU.mult)
                P.stt(s.m2[:, :, w_], mod[:, 64:80, w_], 1.0, colA[:, 112:128], ALU.add, ALU.mult)
            if s.dbg:
                P.dma(s.moddbg[:, 0:192], mod[:].re("p c w -> p (c w)"))
            mrow = P.sb("mrow", [128, 128], stack=st)
            mtmp = P.sb("mtmp", [128, 96], stack=st)
            for w_ in range(2):
                P.cp(mtmp[:], mod[:, :, w_])
                bb = s.bank()
                P.tr(bb[0:96, 0:128], mtmp[:, 0:96], s.C["ident"][:])
                P.cp(mrow[0:96, :], bb[0:96, 0:128])
                P.dma(s.modrow[w_].re("(c p) -> c p", p=128), mrow[0:96, :])
        P.barrier()

    def x_src(s, l, tt_):
        if l == 0:
            if tt_ < 2:
                return s.inp["ctx"][tt_ * 128:(tt_ + 1) * 128, :]
            return s.inp["x"][(tt_ - 2) * 128:(tt_ - 1) * 128, :]
        return s.xcur[tt_ * 128:(tt_ + 1) * 128, :]

    def norm_to_hT(s, src_view, hT, tt_, mcol, shcol, w_, xbufs, st_small, hT32=None, sh_off=0):
        P = s.P
        xt = xbufs[tt_ % 2]
        P.dma(xt[:], src_view)
        junk, ss, rstd = st_small
        P.act(junk[:], xt[:], AF.Square, accum=ss[:, 0:1])
        P.ts(ss[:, 0:1], ss[:, 0:1], 1.0 / D, ALU.mult, EPS, ALU.add)
        P.act(ss[:, 0:1], ss[:, 0:1], AF.Sqrt)
        P.recip(rstd[:, 0:1], ss[:, 0:1])
        P.ts(xt[:], xt[:], rstd[:, 0:1], ALU.mult)
        for g in range(4):
            b = s.bank()
            for j in range(4):
                kc = g * 4 + j
                P.tr(b[:, j * 128:(j + 1) * 128], xt[:, kc * 128:(kc + 1) * 128], s.C["ident"][:])
            for j in range(4):
                kc = g * 4 + j
                P.act(hT[:, kc, tt_ * 128:(tt_ + 1) * 128], b[:, j * 128:(j + 1) * 128], AF.Identity,
                      bias=shcol[:, sh_off + kc, w_:w_ + 1], scale=mcol[:, kc, w_:w_ + 1])
                if hT32 is not None:
                    P.act(hT32[:, kc, :], b[:, j * 128:(j + 1) * 128], AF.Identity,
                          bias=shcol[:, sh_off + kc, w_:w_ + 1], scale=mcol[:, kc, w_:w_ + 1])

    def tok_blocks(s, cm):
        blks = [(0, 256, lambda v: v[:, 0:256])]
        for j in range(4):
            if not cm:
                blks.append((256 + 512 * j, 512, lambda v, j=j: v[:, 256 + 512 * j:256 + 512 * (j + 1)]))
            else:
                blks.append((256 + 512 * j, 512,
                             lambda v, j=j: v[:, 256:T].re("p (r c) -> p c r", c=64)[:, 16 * j:16 * (j + 1), :]))
        return blks

    def stage_proj(s, l):
        P = s.P
        with contextlib.ExitStack() as st:
            hT = P.sb("hT", [128, 16, T], BF16, stack=st)
            xbufs = [P.sb(f"xin{i}", [128, D], stack=st) for i in range(2)]
            small = (P.sb("junk", [128, D], stack=st), P.sb("ss", [128, 1], stack=st), P.sb("rstd", [128, 1], stack=st))
            for tt_ in range(NT):
                w_ = 1 if tt_ < 2 else 0
                s.norm_to_hT(s.x_src(l, tt_), hT, tt_, s.m1, s.mod, w_, xbufs, small)
            cbs = [(i * 128, 128) for i in range(24)] + [(3072, 96), (3168, 96), (3264, 128), (3392, 128)]
            c0 = GLA_OFF
            cbs += [(c0 + i * 128, 128) for i in range(16)] + [(c0 + 2048, 16)]
            cbs += [(GLA_GATE_OFF + i * 128, 128) for i in range(8)]
            cbs += [(BR_GATE_OFF + i * 128, 128) for i in range(32)]
            wst = [P.sb(f"wst{i}", [128, 16, 128], stack=st) for i in range(2)]
            wbf = [P.sb(f"wbf{i}", [128, 16, 128], BF16, stack=st) for i in range(2)]
            ob = [P.sb(f"ob{i}", [128, T], stack=st) for i in range(2)]
            win = s.inp["w_in"][l].re("(kc p) n -> p kc n", p=128)
            for bi, (c0, w) in enumerate(cbs):
                cm = GLA_OFF <= c0 < BR_GATE_OFF
                ws, wb, o = wst[bi % 2], wbf[bi % 2], ob[bi % 2]
                P.dma(ws[:, :, 0:w], win[:, :, c0:c0 + w])
                P.cp(wb[:, :, 0:w], ws[:, :, 0:w], eng="gpsimd")
                for ti, (t0, n, fn) in enumerate(s.tok_blocks(cm)):
                    b = s.bank()
                    for kc in range(16):
                        P.mm(b[0:w, 0:n], wb[:, kc, 0:w], fn(hT[:, kc, :]),
                             start=(kc == 0), stop=(kc == 15))
                    if ti % 2 == 0:
                        P.cp(o[0:w, t0:t0 + n], b[0:w, 0:n], eng="scalar")
                    else:
                        P.cp(o[0:w, t0:t0 + n], b[0:w, 0:n], eng="vector")
                P.dma(s.pT[c0:c0 + w, :], o[0:w, :])
        P.barrier()


    def stage_rwkv(s, l):
        P = s.P
        C = s.C
        with contextlib.ExitStack() as st:
            sb = lambda n, shp, dt=F32: P.sb(n, shp, dt, stack=st)
            packB = sb("packB", [128, 128]); colB = sb("colB", [128, 128])
            P.memset(packB[:], 0.0)
            mu = s.inp["rwkv_mu"][l]
            for i in range(2):
                o = 28 * i
                P.dma(packB[o:o + 24, :], mu[i, 0:3072].re("(c p) -> c p", p=128))
                P.dma(packB[o + 24:o + 25, 0:96], mu[i:i + 1, 3072:3168])
                P.dma(packB[o + 25:o + 26, 0:96], mu[i:i + 1, 3168:3264])
                P.dma(packB[o + 26:o + 28, :], mu[i, 3264:3520].re("(c p) -> c p", p=128))
            P.dma(packB[56:72, :], s.inp["rwkv_w0"][l].re("d (c p) -> (d c) p", p=128))
            P.dma(packB[72:88, :], s.inp["rwkv_a0"][l].re("d (c p) -> (d c) p", p=128))
            P.dma(packB[88:96, :], s.inp["rwkv_k_k"][l].re("(c p) -> c p", p=128))
            P.dma(packB[96:104, :], s.inp["rwkv_k_a"][l].re("(c p) -> c p", p=128))
            P.dma(packB[104:112, :], s.inp["rwkv_r_k"][l].re("(c two) k -> c (two k)", two=2))
            P.dma(packB[112:120, :], s.inp["rwkv_ln_g"][l].re("(c p) -> c p", p=128))
            P.dma(packB[120:128, :], s.inp["rwkv_ln_b"][l].re("(c p) -> c p", p=128))
            s.transpose_pack(packB, 128, colB)
            col = lambda i: colB[:, i:i + 1]
            cf = sb("cf", [128, 28])
            P.tt(cf[:], colB[:, 0:28], colB[:, 28:56], ALU.add)
            P.ts(cf[:], cf[:], -1.0, ALU.mult, 1.0, ALU.add)

            def shift_rows(dst, X, blk, n=128):
                P.ts(dst, X, cf[0:n, blk:blk + 1], ALU.mult)
                for (a, b) in ((0, 256), (256, T)):
                    P.stt(dst[:, a + 1:b], X[:, a:b - 1], colB[0:n, blk:blk + 1], dst[:, a + 1:b], ALU.mult, ALU.add)
                    P.stt(dst[:, a:b - 1], X[:, a + 1:b], colB[0:n, 28 + blk:29 + blk], dst[:, a:b - 1], ALU.mult, ALU.add)

            xb = [sb(f"xb{i}", [128, T]) for i in range(2)]
            xbi = [0]

            def load_rows(row0, n):
                X = xb[xbi[0] % 2]
                xbi[0] += 1
                P.dma(X[0:n, :], s.pT[row0:row0 + n, :])
                return X
            txw = sb("txw", [128, T]); xaS = sb("xaS", [128, T]); sxg = sb("sxg", [128, 2, T], BF16)
            for j in range(2):
                X = load_rows(3264 + 128 * j, 128); shift_rows(txw[:, :], X[:, :], 26 + j)
                P.act(sxg[:, j, :], txw[:, :], AF.Sigmoid)
            X = load_rows(3072, 96); shift_rows(txw[0:96, :], X[0:96, :], 24, 96)
            P.act(txw[0:96, :], txw[0:96, :], AF.Tanh)
            X = load_rows(3168, 96); shift_rows(xaS[0:96, :], X[0:96, :], 25, 96)
            r_ = sb("r", [128, T]); k_ = sb("k", [128, T]); v_ = sb("v", [128, T]); kk_ = sb("kk", [128, T])
            vT = sb("vT", [128, NT, 128]); yacc = sb("yacc", [128, NT, 128]); yo = sb("yo", [128, T], BF16)
            w2c = sb("w2c", [96, 2, 128]); a2c = sb("a2c", [96, 2, 128]); g2c = sb("g2c", [128, 2, 128], BF16); g2f = sb("g2f", [128, 2, 128])
            RKb = sb("RKb", [128, 128])
            PhiT = sb("PhiT", [128, 128]); P.memset(PhiT[:], 0.0)
            Sb = [sb(f"S{i}", [128, 64]) for i in range(2)]
            G_ = 2
            NS = 3
            tn = ["lw", "a", "t1", "kd", "b", "cs", "lg", "EA", "EB"]
            tmp = [{n: sb(f"{n}{i}", [128, 128]) for n in tn} for i in range(NS)]
            tb = [{n: sb(f"{n}{i}", [128, 128]) for n in ("bh", "kh", "bend", "kend")} for i in range(NS)]
            KR = [sb(f"KR{i}", [128, 256]) for i in range(NS)]
            TMs = [sb(f"TMs{i}", [128, 384]) for i in range(NS)]
            gC = [sb(f"gC{i}", [128, 1]) for i in range(NS)]
            Php = [sb(f"Php{i}", [128, 128]) for i in range(NS)]
            Wn = [sb(f"Wn{i}", [128, 128]) for i in range(NS)]
            QT = [sb(f"QT{i}", [128, 128]) for i in range(NS)]
            Dl = [sb(f"Dl{i}", [128, 64]) for i in range(NS)]
            hn = ["ArbT", "MkT", "ArkT", "TT0", "TT1", "G"]
            htmp = [[{n: sb(f"{n}{i}{h}", [128, 128] if n != "G" else [128, 64]) for n in hn} for h in range(2)] for i in range(NS)]
            XX = [[[sb(f"XX{i}{h}{j}", [128, 256]) for j in range(2)] for h in range(2)] for i in range(NS)]
            ep = {n: sb(n, [128, 128]) for n in ("ysum", "cen", "junk", "yaff", "rk", "bon")}
            mean = sb("mean", [128, 2]); var = sb("var", [128, 2])
            ident = C["ident"]

            import os as _os
            for hp in range(int(_os.environ.get('RW_HP', 8))):
                hc = slice(hp * 128, (hp + 1) * 128)
                P.dma(w2c[:], s.inp["rwkv_w2"][l].re("d k n -> k d n")[:, :, hc])
                P.dma(a2c[:], s.inp["rwkv_a2"][l].re("d k n -> k d n")[:, :, hc])
                P.dma(g2f[:], s.inp["rwkv_g2"][l].re("(kc p) n -> p kc n", p=128)[:, :, hc])
                P.cp(g2c[:], g2f[:], eng="gpsimd")
                X = load_rows(hp * 128, 128); shift_rows(r_[:, :], X[:, :], hp)
                X = load_rows(1024 + hp * 128, 128); shift_rows(k_[:, :], X[:, :], 8 + hp)
                X = load_rows(2048 + hp * 128, 128); shift_rows(v_[:, :], X[:, :], 16 + hp)
                kkr, sq = xb[0], xb[1]
                P.ts(kkr[:, :], k_[:, :], col(88 + hp), ALU.mult)
                P.tt(sq[:, :], kkr[:, :], kkr[:, :], ALU.mult)
                for t0 in range(0, T, 512):
                    n = min(512, T - t0)
                    b = s.bank()
                    P.mm(b[:, 0:n], C["blockones"][:], sq[:, t0:t0 + n])
                    P.act(sq[:, t0:t0 + n], b[:, 0:n], AF.Sqrt)
                P.ts(sq[:, :], sq[:, :], 1e-12, ALU.max)
                P.recip(sq[:, :], sq[:, :])
                P.tt(kk_[:, :], kkr[:, :], sq[:, :], ALU.mult)
                for c0 in range(0, NT, 4):
                    nn = min(4, NT - c0)
                    b = s.bank()
                    for j in range(nn):
                        P.tr(b[:, j * 128:(j + 1) * 128], v_[:, (c0 + j) * 128:(c0 + j + 1) * 128], ident[:])
                    P.cp(vT[:, c0:c0 + nn, :], b[:, 0:nn * 128].re("p (c f) -> p c f", f=128), eng="scalar")
                P.ts(RKb[:], C["blockones"][:], col(104 + hp), ALU.mult)
                ci = 0
                for d in range(2):
                    order = list(range(NT)) if d == 0 else [1, 0] + list(range(17, 1, -1))
                    mS, mI, mSt = (C["m_plt"], C["m_ple"], C["m_pgt"]) if d == 0 else (C["m_pgt"], C["m_pge"], C["m_plt"])
                    Scur = 0
                    P.memset(Sb[0][:], 0.0)
                    for gi in range(0, int(_os.environ.get('RW_NT', NT)), G_):
                        grp = [(c, (ci + j) % NS) for j, c in enumerate(order[gi:gi + G_])]
                        ci += len(grp)
                        for (c, i) in grp:
                            t = tmp[i]; u = tb[i]; kr = KR[i]
                            tok = slice(c * 128, (c + 1) * 128)
                            zb = s.bank()
                            P.mm(zb[:, 0:128], w2c[0:96, d, :], txw[0:96, tok])
                            P.mm(zb[:, 128:256], a2c[0:96, d, :], xaS[0:96, tok])
                            P.act(t["lw"][:], zb[:, 0:128], AF.Sigmoid, bias=col(56 + 8 * d + hp))
                            P.ts(t["lw"][:], t["lw"][:], -0.6065306597, ALU.mult)
                            P.act(t["a"][:], zb[:, 128:256], AF.Sigmoid, bias=col(72 + 8 * d + hp))
                            P.ts(t["t1"][:], t["a"][:], -1.0, ALU.add, col(96 + hp), ALU.mult)
                            P.stt(t["kd"][:], t["t1"][:], 1.0, k_[:, tok], ALU.add, ALU.mult)
                            P.tt(t["b"][:], kk_[:, tok], t["a"][:], ALU.mult, eng="gpsimd")
                            P.scan(t["cs"][:], C["ones"][:], t["lw"][:], 0.0, ALU.mult, ALU.add)
                            tot = t["cs"][:, 127:128]
                            if d == 0:
                                lg = t["cs"]
                            else:
                                lg = t["lg"]
                                P.ts(lg[:], t["cs"][:], -1.0, ALU.mult, tot, ALU.add)
                                P.tt(lg[:], lg[:], t["lw"][:], ALU.add)
                            P.act(t["EA"][:], lg[:], AF.Exp)
                            P.tt(kr[:, 128:256], r_[:, tok], t["EA"][:], ALU.mult)
                            P.tt(t["t1"][:], lg[:], t["lw"][:], ALU.subtract, eng="gpsimd")
                            P.act(t["EA"][:], t["t1"][:], AF.Exp)
                            P.tt(kr[:, 0:128], kk_[:, tok], t["EA"][:], ALU.mult)
                            P.act(t["EB"][:], lg[:], AF.Exp, scale=-1.0)
                            P.tt(u["bh"][:], t["b"][:], t["EB"][:], ALU.mult)
                            P.tt(u["kh"][:], t["kd"][:], t["EB"][:], ALU.mult, eng="gpsimd")
                            P.act(t["EB"][:], lg[:], AF.Exp, bias=tot, scale=-1.0)
                            P.tt(u["bend"][:], t["b"][:], t["EB"][:], ALU.mult, eng="gpsimd")
                            P.tt(u["kend"][:], t["kd"][:], t["EB"][:], ALU.mult)
                            P.act(gC[i][:], tot, AF.Exp)
                            bt = s.bank()
                            P.tr(bt[:, 0:128], kr[:, 0:128], ident[:])
                            P.tr(bt[:, 128:256], u["bend"][:], ident[:])
                            P.tr(bt[:, 256:384], u["kend"][:], ident[:])
                            P.cp(TMs[i][:], bt[:, 0:384], eng="scalar")
                        chains = [(c, i, h) for (c, i) in grp for h in range(2)]
                        if _os.environ.get('RW_SKIP') == 'p1':
                            continue
                        for (c, i, h) in chains:
                            hs = slice(h * 64, h * 64 + 64)
                            u = tb[i]; kr = KR[i]; ht = htmp[i][h]; xx = XX[i][h]
                            b1 = s.bank(); P.mm(b1[:, 0:256], u["bh"][hs, :], kr[hs, :])
                            b2 = s.bank(); P.mm(b2[:, 0:256], u["kh"][hs, :], kr[hs, :])
                            P.mm(b2[:, 256:384], kr[hs, 0:128], u["bh"][hs, :])
                            P.tt(xx[0][:, 128:256], b1[:, 0:128], mS[:], ALU.mult)
                            P.tt(ht["ArbT"][:], b1[:, 128:256], mI[:], ALU.mult)
                            P.tt(ht["MkT"][:], b2[:, 0:128], mS[:], ALU.mult)
                            P.tt(ht["ArkT"][:], b2[:, 128:256], mI[:], ALU.mult)
                            P.tt(xx[0][:, 0:128], b2[:, 256:384], mSt[:], ALU.mult)
                            P.tt(ht["TT0"][:], ident[:], xx[0][:, 128:256], ALU.subtract, eng="gpsimd")
                        if _os.environ.get('RW_SKIP') == 'p2':
                            continue
                        TTc = {(c, h): "TT0" for (c, i, h) in chains}
                        for lv in range(6):
                            bxs = {}
                            for (c, i, h) in chains:
                                xc = XX[i][h][lv % 2]
                                bx = s.bank(); bxs[(c, h)] = bx
                                P.mm(bx[:, 0:128], xc[:, 128:256], xc[:, 0:128])
                                if lv < 5:
                                    P.mm(bx[:, 128:256], xc[:, 0:128], xc[:, 128:256])
                            for (c, i, h) in chains:
                                xn = XX[i][h][(lv + 1) % 2]
                                bx = bxs[(c, h)]
                                if lv < 5:
                                    P.cp(xn[:, 0:256], bx[:, 0:256], eng="scalar")
                                else:
                                    P.cp(xn[:, 0:128], bx[:, 0:128], eng="scalar")
                            bqs = {}
                            for (c, i, h) in chains:
                                xn = XX[i][h][(lv + 1) % 2]
                                bq = s.bank(); bqs[(c, h)] = bq
                                P.mm(bq[:, 0:128], xn[:, 0:128], htmp[i][h][TTc[(c, h)]][:])
                            for (c, i, h) in chains:
                                cur = TTc[(c, h)]
                                nxt = "TT1" if cur == "TT0" else "TT0"
                                P.tt(htmp[i][h][nxt][:], bqs[(c, h)][:, 0:128], htmp[i][h][cur][:], ALU.add)
                                TTc[(c, h)] = nxt
                        if _os.environ.get('RW_SKIP') == 'p3':
                            continue
                        for (c, i, h) in chains:
                            hs = slice(h * 64, h * 64 + 64)
                            ht = htmp[i][h]
                            TTf = ht[TTc[(c, h)]]
                            msk = int(_os.environ.get('RW_P4', 15))
                            bg = s.bank()
                            if msk & 1:
                                P.mm(bg[:, 0:64], ht["MkT"][:], vT[:, c, hs])
                                P.cp(ht["G"][:, 0:64], bg[:, 0:64], eng="scalar")
                            bp = s.bank()
                            if msk & 2:
                                P.mm(bp[:, 0:64], TTf[:], TMs[i][:, h * 64:h * 64 + 64])
                                P.mm(bp[:, 64:128], TTf[:], ht["G"][:, 0:64])
                            if msk & 4:
                                P.cp(Php[i][:, hs], bp[:, 0:64], eng="scalar")
                            if msk & 8:
                                P.act(Wn[i][:, hs], bp[:, 64:128], AF.Copy, scale=-1.0)
                        if _os.environ.get('RW_SKIP') == 'p4':
                            continue
                        for (c, i) in grp:
                            t = tmp[i]; tm = TMs[i]
                            tok = slice(c * 128, (c + 1) * 128)
                            BendT, KendT = tm[:, 128:256], tm[:, 256:384]
                            Sc = Sb[Scur]; Sn = Sb[1 - Scur]
                            for h in range(2):
                                hs = slice(h * 64, h * 64 + 64)
                                bq = s.bank()
                                P.mm(bq[:, 0:128], Php[i][:], htmp[i][h]["ArbT"][:])
                                P.tt(QT[i][hs, :], KR[i][hs, 128:256], bq[hs, 0:128], ALU.subtract)
                            by = s.bank()
                            for h in range(2):
                                hs = slice(h * 64, h * 64 + 64)
                                P.mm(by[:, hs], htmp[i][h]["ArkT"][:], vT[:, c, hs], start=True, stop=False)
                                if _os.environ.get('RW_SKIP') == 'qts':
                                    P.mm(by[:, hs], htmp[i][h]["ArbT"][:], Wn[i][:, hs], start=False, stop=True)
                                else:
                                    P.mm(by[:, hs], htmp[i][h]["ArbT"][:], Wn[i][:, hs], start=False, stop=False)
                                    P.mm(by[:, hs], QT[i][hs, :], Sc[hs, :], start=False, stop=True)
                            bf = s.bank(); P.mm(bf[:, 0:128], Php[i][:], BendT)
                            bd = s.bank()
                            P.mm(bd[:, 0:128], KendT, vT[:, c, :], start=True, stop=False)
                            P.mm(bd[:, 0:128], BendT, Wn[i][:], start=False, stop=True)
                            for h in range(2):
                                hs = slice(h * 64, h * 64 + 64)
                                P.stt(PhiT[hs, hs], ident[hs, hs], gC[i][hs, 0:1], bf[hs, hs], ALU.mult, ALU.subtract)
                                P.cp(Dl[i][hs, :], bd[hs, h * 64:h * 64 + 64], eng="scalar")
                            bs = s.bank(); P.mm(bs[:, 0:64], PhiT[:], Sc[:])
                            P.tt(Sn[:], bs[:, 0:64], Dl[i][:], ALU.add)
                            Scur = 1 - Scur
                            if d == 0:
                                P.cp(yacc[:, c, :], by[:, 0:128], eng="scalar")
                                continue
                            if _os.environ.get('RW_NOEP'):
                                continue
                            ys = ep["ysum"]
                            P.tt(ys[:], by[:, 0:128], yacc[:, c, :], ALU.add)
                            P.red(mean[:], ys[:].re("p (h v) -> p h v", h=2), ALU.add)
                            P.ts(mean[:], mean[:], 1.0 / 64, ALU.mult)
                            for h in range(2):
                                hs = slice(h * 64, h * 64 + 64)
                                P.ts(ep["cen"][:, hs], ys[:, hs], mean[:, h:h + 1], ALU.subtract)
                                P.act(ep["junk"][:, hs], ep["cen"][:, hs], AF.Square, accum=var[:, h:h + 1])
                            P.ts(var[:], var[:], 1.0 / 64, ALU.mult, 64e-5, ALU.add)
                            P.act(var[:], var[:], AF.Sqrt)
                            P.recip(var[:], var[:])
                            for h in range(2):
                                hs = slice(h * 64, h * 64 + 64)
                                P.ts(ep["cen"][:, hs], ep["cen"][:, hs], var[:, h:h + 1], ALU.mult)
                            be = s.bank()
                            P.tr(be[:, 0:128], ep["cen"][:], ident[:])
                            P.act(ep["yaff"][:], be[:, 0:128], AF.Identity, bias=col(120 + hp), scale=col(112 + hp))
                            P.tt(ep["rk"][:], r_[:, tok], k_[:, tok], ALU.mult, eng="gpsimd")
                            bb = s.bank()
                            P.mm(bb[:, 0:128], RKb[:], ep["rk"][:])
                            P.mm(bb[:, 128:256], g2c[:, 0, :], sxg[:, 0, tok], start=True, stop=False)
                            P.mm(bb[:, 128:256], g2c[:, 1, :], sxg[:, 1, tok], start=False, stop=True)
                            P.tt(ep["bon"][:], bb[:, 0:128], v_[:, tok], ALU.mult)
                            P.tt(ep["yaff"][:], ep["yaff"][:], ep["bon"][:], ALU.add)
                            P.tt(yo[:, tok], ep["yaff"][:], bb[:, 128:256], ALU.mult)
                P.dma(s.yaT[hc, :], yo[:, :])
        P.barrier()

    def stage_gla(s, l):
        P = s.P
        C = s.C
        ident = C["ident"]
        with contextlib.ExitStack() as st:
            sb = lambda n, shp, dt=F32: P.sb(n, shp, dt, stack=st)
            packC = sb("packC", [128, 128]); colC = sb("colC", [128, 128])
            P.memset(packC[:], 0.0)
            P.dma(packC[0:48, :], s.inp["gla_conv"][l].re("j (c p) -> (j c) p", p=128))
            P.dma(packC[48:56, :], s.inp["gla_alpha_b"][l].re("d (c p) -> (d c) p", p=128))
            P.dma(packC[56:58, :], s.inp["gla_norm_g"][l].re("(c p) -> c p", p=128))
            s.transpose_pack(packC, 64, colC)
            col = lambda i: colC[:, i:i + 1]

            def conv_rows(dst, X, blk):
                P.ts(dst, X, col(16 + blk), ALU.mult)
                for (a, b) in ((0, 256), (256, T)):
                    P.stt(dst[:, a + 1:b], X[:, a:b - 1], col(blk), dst[:, a + 1:b], ALU.mult, ALU.add)
                    P.stt(dst[:, a:b - 1], X[:, a + 1:b], col(32 + blk), dst[:, a:b - 1], ALU.mult, ALU.add)
                P.act(dst, dst, AF.Silu)
            xb = [sb(f"gxb{i}", [128, T]) for i in range(2)]
            xbi = [0]

            def load_rows(row0, n):
                X = xb[xbi[0] % 2]
                xbi[0] += 1
                P.dma(X[0:n, :], s.pT[row0:row0 + n, :])
                return X
            adS = sb("adS", [16, T])
            P.dma(adS[:], s.pT[GLA_OFF + 2048:GLA_OFF + 2064, :])
            q_ = sb("q", [128, T]); k_ = sb("gk", [128, T]); v_ = sb("gv", [128, 2, T]); gt = sb("gt", [128, 2, T])
            vT = sb("gvT", [128, NT, 256]); oacc = sb("oacc", [128, NT, 256]); yo = sb("gyo", [128, 2, T], BF16)
            aw = sb("aw", [16, 2, 128])
            Sb = [sb(f"gS{i}", [128, 256]) for i in range(2)]
            NS = 2
            tn = ["g", "cs", "gc", "E1", "E2", "E4", "qg", "kg", "kend", "attT", "kendT"]
            tmp = [{n: sb(f"g{n}{i}", [128, 128]) for n in tn} for i in range(NS)]
            gCe = [sb(f"gCe{i}", [128, 1]) for i in range(NS)]
            osum = sb("osum", [128, 256]); junk = sb("gjunk", [128, 256]); ssq = sb("ssq", [128, 1])
            for hd in range(4):
                P.dma(aw[:], s.inp["gla_alpha_w2"][l].re("d k n -> k d n")[:, :, hd * 128:(hd + 1) * 128])
                X = load_rows(GLA_OFF + hd * 128, 128); conv_rows(q_[:, :], X[:, :], hd)
                X = load_rows(GLA_OFF + 512 + hd * 128, 128); conv_rows(k_[:, :], X[:, :], 4 + hd)
                for vc in range(2):
                    X = load_rows(GLA_OFF + 1024 + hd * 256 + vc * 128, 128)
                    conv_rows(v_[:, vc, :], X[:, :], 8 + hd * 2 + vc)
                    P.dma(gt[:, vc, :], s.pT[GLA_GATE_OFF + hd * 256 + vc * 128:GLA_GATE_OFF + hd * 256 + (vc + 1) * 128, :])
                    P.act(gt[:, vc, :], gt[:, vc, :], AF.Silu)
                for c in range(NT):
                    b = s.bank()
                    for vc in range(2):
                        P.tr(b[:, vc * 128:(vc + 1) * 128], v_[:, vc, c * 128:(c + 1) * 128], ident[:])
                    P.cp(vT[:, c, :], b[:, 0:256], eng="scalar")
                ci = 0
                for d in range(2):
                    order = list(range(NT)) if d == 0 else [1, 0] + list(range(17, 1, -1))
                    mI = C["m_ple"] if d == 0 else C["m_pge"]
                    Scur = 0
                    P.memset(Sb[0][:], 0.0)
                    for c in order:
                        i = ci % NS
                        ci += 1
                        t = tmp[i]
                        tok = slice(c * 128, (c + 1) * 128)
                        zb = s.bank()
                        P.mm(zb[:, 0:128], aw[0:16, d, :], adS[0:16, tok])
                        P.act(t["g"][:], zb[:, 0:128], AF.Sigmoid, bias=col(48 + 4 * d + hd))
                        P.act(t["g"][:], t["g"][:], AF.Ln)
                        P.ts(t["g"][:], t["g"][:], 1.0 / 16, ALU.mult)
                        P.scan(t["cs"][:], C["ones"][:], t["g"][:], 0.0, ALU.mult, ALU.add)
                        tot = t["cs"][:, 127:128]
                        if d == 0:
                            gc = t["cs"]
                        else:
                            gc = t["gc"]
                            P.ts(gc[:], t["cs"][:], -1.0, ALU.mult, tot, ALU.add)
                            P.tt(gc[:], gc[:], t["g"][:], ALU.add)
                        P.act(t["E1"][:], gc[:], AF.Exp)
                        P.stt(t["qg"][:], q_[:, tok], 128 ** -0.5, t["E1"][:], ALU.mult, ALU.mult)
                        P.act(t["E2"][:], gc[:], AF.Exp, scale=-1.0)
                        P.tt(t["kg"][:], k_[:, tok], t["E2"][:], ALU.mult)
                        P.act(t["E4"][:], gc[:], AF.Exp, bias=tot, scale=-1.0)
                        P.tt(t["kend"][:], k_[:, tok], t["E4"][:], ALU.mult)
                        P.act(gCe[i][:], tot, AF.Exp)
                        ba = s.bank(); P.mm(ba[:, 0:128], t["kg"][:], t["qg"][:])
                        P.tt(t["attT"][:], ba[:, 0:128], mI[:], ALU.mult)
                        bt = s.bank(); P.tr(bt[:, 0:128], t["kend"][:], ident[:])
                        P.cp(t["kendT"][:], bt[:, 0:128], eng="scalar")
                        Sc = Sb[Scur]; Sn = Sb[1 - Scur]
                        bo = s.bank()
                        P.mm(bo[:, 0:256], t["attT"][:], vT[:, c, :], start=True, stop=False)
                        P.mm(bo[:, 0:256], t["qg"][:], Sc[:], start=False, stop=True)
                        bs = s.bank(); P.mm(bs[:, 0:256], t["kendT"][:], vT[:, c, :])
                        P.stt(Sn[:], Sc[:], gCe[i][:, 0:1], bs[:, 0:256], ALU.mult, ALU.add)
                        Scur = 1 - Scur
                        if d == 0:
                            P.cp(oacc[:, c, :], bo[:, 0:256], eng="scalar")
                            continue
                        P.tt(osum[:], bo[:, 0:256], oacc[:, c, :], ALU.add)
                        P.act(junk[:], osum[:], AF.Square, accum=ssq[:, 0:1])
                        P.ts(ssq[:], ssq[:], 1.0 / 256, ALU.mult, EPS, ALU.add)
                        P.act(ssq[:], ssq[:], AF.Sqrt)
                        P.recip(ssq[:], ssq[:])
                        P.ts(osum[:], osum[:], ssq[:, 0:1], ALU.mult)
                        be = s.bank()
                        for vc in range(2):
                            P.tr(be[:, vc * 128:(vc + 1) * 128], osum[:, vc * 128:(vc + 1) * 128], ident[:])
                        for vc in range(2):
                            P.stt(yo[:, vc, tok], be[:, vc * 128:(vc + 1) * 128], col(56 + vc), gt[:, vc, tok], ALU.mult, ALU.mult)
                for vc in range(2):
                    P.dma(s.ybT[hd * 256 + vc * 128:hd * 256 + (vc + 1) * 128, :], yo[:, vc, :])
        P.barrier()


    def stage_merge(s, l, last):
        P = s.P
        with contextlib.ExitStack() as st:
            sb = lambda n, shp, dt=F32: P.sb(n, shp, dt, stack=st)
            yaS = sb("yaS", [128, 8, T], BF16); ybS = sb("ybS", [128, 8, T], BF16)
            for kc in range(8):
                P.dma(yaS[:, kc, :], s.yaT[kc * 128:(kc + 1) * 128, :])
                P.dma(ybS[:, kc, :], s.ybT[kc * 128:(kc + 1) * 128, :])
            gbc = sb("gbc", [128, 2, D])
            for w_ in range(2):
                P.dma(gbc[:, w_, :], s.modrow[w_, 2 * D:3 * D].pb(128))
            wa_st = sb("wa_st", [128, 8, 128]); wb_st = sb("wb_st", [128, 8, 128])
            wa = sb("wa", [128, 8, 128], BF16); wb = sb("wb", [128, 8, 128], BF16)
            gaS = sb("gaS", [128, 512]); gbS = sb("gbS", [128, 512])
            mix = sb("mix", [128, 16, 512], BF16)
            m1 = sb("mm1", [128, 512]); m2 = sb("mm2", [128, 512])
            wo_st = sb("wo_st", [128, 16, 512]); wo = sb("wo", [128, 16, 512], BF16)
            xt = sb("mxt", [128, 512]); yt = sb("myt", [128, 512])
            blocks = [(256 + 512 * j, 512, 0, j) for j in range(4)]
            if not last:
                blocks = [(0, 256, 1, -1)] + blocks
            wav = s.inp["w_branch_a"][l].re("(kc p) n -> p kc n", p=128)
            wbv = s.inp["w_branch_b"][l].re("(kc p) n -> p kc n", p=128)
            wov = s.inp["w_out"][l].re("(kc p) n -> p kc n", p=128)
            for (t0, n, w_, j) in blocks:
                for dc in range(16):
                    dcs = slice(dc * 128, (dc + 1) * 128)
                    P.dma(wa_st[:], wav[:, :, dcs]); P.cp(wa[:], wa_st[:], eng="gpsimd")
                    P.dma(wb_st[:], wbv[:, :, dcs]); P.cp(wb[:], wb_st[:], eng="gpsimd")
                    P.dma(gaS[:, 0:n], s.pT[BR_GATE_OFF + dc * 128:BR_GATE_OFF + (dc + 1) * 128, t0:t0 + n])
                    P.dma(gbS[:, 0:n], s.pT[BR_GATE_OFF + D + dc * 128:BR_GATE_OFF + D + (dc + 1) * 128, t0:t0 + n])
                    bA = s.bank()
                    for kc in range(8):
                        P.mm(bA[:, 0:n], wa[:, kc, :], yaS[:, kc, t0:t0 + n], start=(kc == 0), stop=(kc == 7))
                    bB = s.bank()
                    for kc in range(8):
                        if j < 0:
                            rhs = ybS[:, kc, 0:256]
                        else:
                            rhs = ybS[:, kc, 256:T].re("p (c r) -> p r c", r=32)[:, 8 * j:8 * j + 8, :]
                        P.mm(bB[:, 0:n], wb[:, kc, :], rhs, start=(kc == 0), stop=(kc == 7))
                    P.act(gaS[:, 0:n], gaS[:, 0:n], AF.Sigmoid)
                    P.act(gbS[:, 0:n], gbS[:, 0:n], AF.Sigmoid)
                    P.tt(m1[:, 0:n], bA[:, 0:n], gaS[:, 0:n], ALU.mult)
                    P.tt(m2[:, 0:n], bB[:, 0:n], gbS[:, 0:n], ALU.mult)
                    P.tt(mix[:, dc, 0:n], m1[:, 0:n], m2[:, 0:n], ALU.add, eng="gpsimd")
                for db in range(4):
                    dbs = slice(db * 512, (db + 1) * 512)
                    P.dma(wo_st[:], wov[:, :, dbs]); P.cp(wo[:], wo_st[:], eng="gpsimd")
                    for ti in range(n // 128):
                        tile = t0 // 128 + ti
                        rows = slice(tile * 128, (tile + 1) * 128)
                        bo = s.bank()
                        for kc in range(16):
                            P.mm(bo[:, 0:512], mix[:, kc, ti * 128:(ti + 1) * 128], wo[:, kc, :], start=(kc == 0), stop=(kc == 15))
                        P.dma(xt[:], s.x_src(l, tile)[:, dbs])
                        P.tt(yt[:], bo[:, 0:512], gbc[:, w_, dbs], ALU.mult)
                        P.tt(yt[:], yt[:], xt[:], ALU.add)
                        P.dma(s.xmid[rows, dbs], yt[:])
        P.barrier()

    def stage_ffn(s, l, last):
        P = s.P
        C = s.C
        tiles = list(range(2, NT)) if last else list(range(NT))
        groups = [tiles[0:6], tiles[6:12], tiles[12:18]] if not last else [tiles[0:6], tiles[6:11], tiles[11:16]]
        with contextlib.ExitStack() as st0:
            sb0 = lambda n, shp, dt=F32: P.sb(n, shp, dt, stack=st0)
            rw = sb0("rw", [128, 16, NE]); P.dma(rw[:], s.inp["router_w"][:].re("(kc p) e -> p kc e", p=128))
            rb = sb0("rb", [128, NE]); P.dma(rb[:], s.inp["router_b"][0].pb(128))
            s.sel16 = sb0("sel16s", [16, 16, 128])
            P.dma(s.sel16[:], s.inp["sel16"][:])
            hT2 = sb0("hT2", [128, 16, 768], BF16); acc = sb0("acc", [128, 6, D]); gatesT = sb0("gatesT", [16, 768])
            for grp in groups:
                ng = len(grp); ntok = ng * 128
                tbl = [(a, min(512, ntok - a)) for a in range(0, ntok, 512)]
                with contextlib.ExitStack() as stA:
                    sbA = lambda n, shp, dt=F32: P.sb(n, shp, dt, stack=stA)
                    xb1 = sbA("fx", [128, D])
                    small = (sbA("fjunk", [128, D]), sbA("fss", [128, 1]), sbA("frstd", [128, 1]))
                    hT32 = sbA("hT32", [128, 16, 128])
                    q = {n: sbA("r_" + n, [128, 16]) for n in ("sc", "sel", "msk", "eq1", "msk2", "eq2", "wts", "gates")}
                    g4 = {n: sbA("r4_" + n, [128, 4]) for n in ("gs", "t4", "gmask", "pen")}
                    r1 = {n: sbA("r1_" + n, [128, 1]) for n in ("gmax", "m1", "m2", "ssum")}
                    for ti, tile in enumerate(grp):
                        w_ = 1 if tile < 2 else 0
                        s.norm_to_hT(s.xmid[tile * 128:(tile + 1) * 128, :], hT2, ti, s.m2, s.mod, w_, [xb1, xb1], small,
                                     hT32=hT32, sh_off=48)
                        br = s.bank()
                        for kc in range(16):
                            P.mm(br[:, 0:NE], hT32[:, kc, :], rw[:, kc, :], start=(kc == 0), stop=(kc == 15))
                        P.act(q["sc"][:], br[:, 0:NE], AF.Sigmoid)
                        P.tt(q["sel"][:], q["sc"][:], rb[:], ALU.add)
                        s4 = q["sel"][:].re("p (g j) -> p g j", j=4)
                        first = True
                        for a_ in range(4):
                            for b_ in range(a_ + 1, 4):
                                if first:
                                    P.tt(g4["gs"][:], s4[:, :, a_], s4[:, :, b_], ALU.add)
                                    first = False
                                else:
                                    P.tt(g4["t4"][:], s4[:, :, a_], s4[:, :, b_], ALU.add)
                                    P.tt(g4["gs"][:], g4["gs"][:], g4["t4"][:], ALU.max)
                        P.red(r1["gmax"][:], g4["gs"][:], ALU.max)
                        P.ts(g4["gmask"][:], g4["gs"][:], r1["gmax"][:, 0:1], ALU.is_ge)
                        P.ts(g4["pen"][:], g4["gmask"][:], -1.0, ALU.add, 1e9, ALU.mult)
                        m4 = q["msk"][:].re("p (g j) -> p g j", j=4)
                        for j_ in range(4):
                            P.tt(m4[:, :, j_], s4[:, :, j_], g4["pen"][:], ALU.add)
                        P.red(r1["m1"][:], q["msk"][:], ALU.max)
                        P.ts(q["eq1"][:], q["msk"][:], r1["m1"][:, 0:1], ALU.is_equal)
                        P.stt(q["msk2"][:], q["eq1"][:], -1e9, q["msk"][:], ALU.mult, ALU.add)
                        P.red(r1["m2"][:], q["msk2"][:], ALU.max)
                        P.ts(q["eq2"][:], q["msk2"][:], r1["m2"][:, 0:1], ALU.is_equal)
                        P.tt(q["eq1"][:], q["eq1"][:], q["eq2"][:], ALU.add)
                        P.tt(q["wts"][:], q["sc"][:], q["eq1"][:], ALU.mult)
                        P.red(r1["ssum"][:], q["wts"][:], ALU.add)
                        P.recip(r1["ssum"][:], r1["ssum"][:])
                        P.ts(q["gates"][:], q["wts"][:], r1["ssum"][:, 0:1], ALU.mult)
                        bt = s.bank()
                        P.tr(bt[0:NE, 0:128], q["gates"][:, 0:NE], C["ident"][:])
                        P.cp(gatesT[0:NE, ti * 128:(ti + 1) * 128], bt[0:NE, 0:128])
                P.barrier()
                with contextlib.ExitStack() as stB:
                    sbB = lambda n, shp, dt=F32: P.sb(n, shp, dt, stack=stB)
                    wd = sbB("wd", [128, 11, D], BF16)
                    stg = [sbB(f"stg{i}", [128, D]) for i in range(3)]
                    wgb = sbB("wgb", [128, 16, 128], BF16); wub = sbB("wub", [128, 16, 128], BF16)
                    actE = sbB("actE", [128, 11, 768], BF16); gb_ = sbB("gb_", [128, 768])
                    sg = sbB("sg", [128, 512]); tq = sbB("tq", [128, 512])
                    for e in range(NE):
                        for (a, n) in tbl:
                            bk = s.bank()
                            P.mm(bk[:, 0:n], s.sel16[0:16, e, :], gatesT[0:16, a:a + n])
                            P.cp(gb_[:, a:a + n], bk[:, 0:n], eng="scalar")
                        wgv = s.inp["exp_w_gate"][l, e].re("(kc p) f -> p kc f", p=128)
                        wuv = s.inp["exp_w_up"][l, e].re("(kc p) f -> p kc f", p=128)
                        for fc in range(11):
                            fs = slice(fc * 128, (fc + 1) * 128)
                            s0 = stg[0][:].re("p (kc f) -> p kc f", f=128)
                            s1 = stg[1][:].re("p (kc f) -> p kc f", f=128)
                            P.dma(s0, wgv[:, :, fs]); P.cp(wgb[:], s0, eng="vector")
                            P.dma(s1, wuv[:, :, fs]); P.cp(wub[:], s1, eng="scalar")
                            P.dma(stg[2][:], s.inp["exp_w_down"][l, e, fs, :]); P.cp(wd[:, fc, :], stg[2][:], eng="gpsimd")
                            for (a, n) in tbl:
                                bg = s.bank()
                                for kc in range(16):
                                    P.mm(bg[:, 0:n], wgb[:, kc, :], hT2[:, kc, a:a + n], start=(kc == 0), stop=(kc == 15))
                                bu = s.bank()
                                for kc in range(16):
                                    P.mm(bu[:, 0:n], wub[:, kc, :], hT2[:, kc, a:a + n], start=(kc == 0), stop=(kc == 15))
                                P.act(sg[:, 0:n], bg[:, 0:n], AF.Silu)
                                P.tt(tq[:, 0:n], bu[:, 0:n], gb_[:, a:a + n], ALU.mult)
                                P.tt(actE[:, fc, a:a + n], sg[:, 0:n], tq[:, 0:n], ALU.mult, eng="gpsimd")
                        for ti in range(ng):
                            for db in range(4):
                                dbs = slice(db * 512, (db + 1) * 512)
                                bo = s.bank()
                                for fc in range(11):
                                    P.mm(bo[:, 0:512], actE[:, fc, ti * 128:(ti + 1) * 128], wd[:, fc, dbs], start=(fc == 0), stop=(fc == 10))
                                if e == 0:
                                    P.cp(acc[:, ti, dbs], bo[:, 0:512], eng="scalar")
                                else:
                                    P.tt(acc[:, ti, dbs], bo[:, 0:512], acc[:, ti, dbs], ALU.add)
                P.barrier()
                with contextlib.ExitStack() as stC:
                    sbC = lambda n, shp, dt=F32: P.sb(n, shp, dt, stack=stC)
                    gbc = sbC("fgbc", [128, 2, D])
                    for w_ in range(2):
                        P.dma(gbc[:, w_, :], s.modrow[w_, 5 * D:6 * D].pb(128))
                    fng = sbC("fng", [128, D])
                    if last:
                        P.dma(fng[:], s.inp["final_norm_g"][:].pb(128))
                    xt = sbC("cxt", [128, D]); yt = sbC("cyt", [128, D]); junk = sbC("cjunk", [128, D])
                    ss = sbC("css", [128, 1])
                    for ti, tile in enumerate(grp):
                        w_ = 1 if tile < 2 else 0
                        rows = slice(tile * 128, (tile + 1) * 128)
                        P.dma(xt[:], s.xmid[rows, :])
                        P.tt(yt[:], acc[:, ti, :], gbc[:, w_, :], ALU.mult)
                        P.tt(yt[:], yt[:], xt[:], ALU.add)
                        if not last:
                            P.dma(s.xcur[rows, :], yt[:])
                        else:
                            P.act(junk[:], yt[:], AF.Square, accum=ss[:, 0:1])
                            P.ts(ss[:], ss[:], 1.0 / D, ALU.mult, EPS, ALU.add)
                            P.act(ss[:], ss[:], AF.Sqrt)
                            P.recip(ss[:], ss[:])
                            P.stt(yt[:], yt[:], ss[:, 0:1], fng[:], ALU.mult, ALU.mult)
                            P.dma(s.out[(tile - 2) * 128:(tile - 1) * 128, :], yt[:])
                P.barrier()

    def build(s):
        P = s.P
        s.colA = P.sb("colA", [128, 128])
        s.mod = P.sb("mod", [128, 96, 2])
        s.m1 = P.sb("m1", [128, 16, 2])
        s.m2 = P.sb("m2", [128, 16, 2])
        s.stage_prologue()
        fin = []
        for l in range(DEPTH):
            s.stage_mod(l)
            if s.stop_after == ("mod", l):
                break
            s.stage_proj(l)
            if s.stop_after == ("proj", l):
                break
            if not s.skip_rwkv:
                s.stage_rwkv(l)
            if s.stop_after == ("rwkv", l):
                break
            s.stage_gla(l)
            if s.stop_after == ("gla", l):
                break
            last = (l == DEPTH - 1)
            s.stage_merge(l, last)
            if s.stop_after == ("merge", l):
                break
            s.stage_ffn(l, last)
            if s.stop_after == ("ffn", l):
                break
        P.barrier()
        P.final_tokens = [(sk, v) for sk, v in P.dma_tgt.items()]
        P.emit()
        P.st.close()


def build_nc(stop_after=None, dbg=False):
    nc = bass.Bass("TRN2", target_bir_lowering=False)
    k = K(nc, stop_after=stop_after, dbg=dbg)
    k.build()
    return nc


def make_in_maps(inputs, cores):
    consts = make_consts()
    shared = {}
    for k, shp in W_SPECS.items():
        shared[k] = np.ascontiguousarray(np.asarray(inputs[k], dtype=np.float32).reshape(shp))
    maps = []
    for b in cores:
        m = dict(shared)
        m["x"] = np.ascontiguousarray(inputs["x"][b])
        m["ctx"] = np.ascontiguousarray(inputs["ctx"][b])
        m["c"] = np.ascontiguousarray(inputs["c"][b:b + 1])
        m["c_ctx"] = np.ascontiguousarray(np.asarray(inputs["c_ctx"]).reshape(1, D))
        m["consts"] = consts
        sel = np.zeros((16, 16, 128), np.float32)
        for e in range(16):
            sel[e, e, :] = 1.0
        m["sel16"] = sel
        maps.append(m)
    return maps


def kernel(**inputs):
    nc = build_nc()
    maps = make_in_maps(inputs, list(range(8)))
    res = run_bass_kernel_spmd(nc, maps, core_ids=list(range(8)))
    return np.stack([r["out"] for r in res.results], 0).astype(np.float32)
```

```python
import contextlib
import numpy as np
import concourse.bass as bass
import concourse.mybir as mybir
from concourse.bass_utils import run_bass_kernel_spmd

F32 = mybir.dt.float32
BF16 = mybir.dt.bfloat16
AF = mybir.ActivationFunctionType
ALU = mybir.AluOpType
AX = mybir.AxisListType

ENGS = ("tensor", "vector", "scalar", "gpsimd", "sync")
N_DMA_SEMS = 12
import os as _os0
NO_SAME_ENGINE_SYNC = False

D = 2048
T = 2304
NT = 18
CTX = 256
DEPTH = 2
RWKV_IN = 3520
GLA_OFF = 3520
GLA_GATE_OFF = 5584
BR_GATE_OFF = 6608
IN_DIM = 10704
NE = 16
DE = 1408
EPS = 1e-6


class View:
    def __init__(s, buf, ap, keys):
        s.buf, s.ap, s.keys = buf, ap, keys

    def re(s, pat, **kw):
        return View(s.buf, s.ap.rearrange(pat, **kw), s.keys)

    def __getitem__(s, idx):
        return View(s.buf, s.ap[idx], s.keys)

    def pb(s, n):
        return View(s.buf, s.ap.partition_broadcast(n), s.keys)


class Buf:
    def __init__(s, name, h, nsub=None):
        s.name, s.h, s.nsub = name, h, nsub

    def allkeys(s):
        if s.nsub:
            return [(s.name, i) for i in range(s.nsub)]
        return [s.name]

    def __getitem__(s, idx):
        return View(s, s.h[idx], s.allkeys())

    def sub(s, i, idx):
        return View(s, s.h[idx], [(s.name, i)])


class Prog:
    def __init__(s, nc):
        s.nc = nc
        s.st = contextlib.ExitStack()
        s.ops = {e: [] for e in ENGS}
        s.cnt = {e: 0 for e in ENGS}
        s.seen = {e: {} for e in ENGS}
        s.last_w = {}
        s.readers = {}
        s.dma_rr = {e: 0 for e in ENGS}
        s.dma_tgt = {}
        s.final_tokens = []
        s.nbuf = 0

    def sb(s, name, shape, dt=F32, nsub=None, stack=None):
        s.nbuf += 1
        nm = f"{name}_{s.nbuf}"
        h = (stack or s.st).enter_context(s.nc.sbuf_tensor(nm, list(shape), dt))
        return Buf(nm, h, nsub)

    def ps(s, name, shape, dt=F32, stack=None):
        s.nbuf += 1
        nm = f"{name}_{s.nbuf}"
        h = (stack or s.st).enter_context(s.nc.psum_tensor(nm, list(shape), dt))
        return Buf(nm, h)

    def dram(s, name, shape, dt=F32, kind="Internal", nsub=None):
        h = s.nc.dram_tensor(name, list(shape), dt, kind=kind).ap()
        return Buf(name, h, nsub)

    def _need(s, eng, tok, waits):
        if tok is None:
            return
        sk, v = tok
        if sk == ("e", eng) and (eng == "tensor" or NO_SAME_ENGINE_SYNC):
            return
        if s.seen[eng].get(sk, 0) >= v:
            return
        s.seen[eng][sk] = v
        waits.append((sk, v))

    def _deps(s, eng, reads, writes):
        waits = []
        for k in reads:
            s._need(eng, s.last_w.get(k), waits)
        for k in writes:
            s._need(eng, s.last_w.get(k), waits)
            for t in s.readers.get(k, ()):
                s._need(eng, t, waits)
        return waits

    def _commit(s, tok, reads, writes):
        for k in reads:
            s.readers.setdefault(k, []).append(tok)
        for k in writes:
            s.last_w[k] = tok
            s.readers[k] = []

    def op(s, eng, fn, reads=(), writes=()):
        waits = s._deps(eng, reads, writes)
        s.cnt[eng] += 1
        tok = (("e", eng), s.cnt[eng])
        s.ops[eng].append((waits, fn, tok))
        s._commit(tok, reads, writes)
        return tok

    def _dma(s, eng, fn, reads=(), writes=()):
        waits = s._deps(eng, reads, writes)
        slot = s.dma_rr[eng] % N_DMA_SEMS
        s.dma_rr[eng] += 1
        sk = ("d", eng, slot)
        prev = s.dma_tgt.get(sk, 0)
        if prev:
            s._need(eng, (sk, prev), waits)
        tgt = prev + 16
        s.dma_tgt[sk] = tgt
        tok = (sk, tgt)
        s.ops[eng].append((waits, fn, tok))
        s._commit(tok, reads, writes)
        return tok

    def barrier(s):
        toks = [(("e", e), s.cnt[e]) for e in ENGS if s.cnt[e]]
        toks += [(sk, v) for sk, v in s.dma_tgt.items()]
        for e in ENGS:
            waits = []
            for t in toks:
                s._need(e, t, waits)
            if waits:
                s.ops[e].append((waits, None, None))
        s.last_w.clear()
        s.readers.clear()

    def emit(s):
        nc = s.nc
        with contextlib.ExitStack() as st:
            semh = {}
            for e in ENGS:
                semh[("e", e)] = st.enter_context(nc.semaphore(f"s_{e}"))
            for e in ENGS:
                for i in range(min(N_DMA_SEMS, s.dma_rr[e])):
                    semh[("d", e, i)] = st.enter_context(nc.semaphore(f"d_{e}_{i}"))
            block = st.enter_context(nc.Block())
            fin = []
            for t in s.final_tokens:
                s._need("sync", t, fin)
            for e in ENGS:
                ops = s.ops[e]
                extra = fin if e == "sync" else []
                if not ops and not extra:
                    continue

                def body(engine, ops=ops, extra=extra):
                    for waits, fn, tok in ops:
                        for sk, v in waits:
                            engine.wait_ge(semh[sk], v)
                        if fn is None:
                            continue
                        ins = fn(engine)
                        sk, v = tok
                        ins.then_inc(semh[sk], 16 if sk[0] == "d" else 1)
                    for sk, v in extra:
                        engine.wait_ge(semh[sk], v)
                getattr(block, e)(body)

    @staticmethod
    def _k(*vs):
        ks = []
        for v in vs:
            if isinstance(v, View):
                ks += v.keys
        return ks

    @staticmethod
    def _a(v):
        return v.ap if isinstance(v, View) else v

    def mm(s, out, lhsT, rhs, start=True, stop=True):
        return s.op("tensor", lambda e: e.matmul(out.ap, lhsT.ap, rhs.ap, start=start, stop=stop),
                    reads=s._k(lhsT, rhs), writes=s._k(out))

    def tr(s, out, in_, ident):
        return s.op("tensor", lambda e: e.transpose(out.ap, in_.ap, ident.ap),
                    reads=s._k(in_, ident), writes=s._k(out))

    def act(s, out, in_, func, bias=None, scale=None, accum=None):
        kw = {}
        if bias is not None:
            kw["bias"] = s._a(bias)
        if scale is not None:
            kw["scale"] = s._a(scale)
        if accum is not None:
            kw["accum_out"] = accum.ap
        return s.op("scalar", lambda e: e.activation(out.ap, in_.ap, func, **kw),
                    reads=s._k(in_, bias, scale), writes=s._k(out, accum))

    def tt(s, out, a, b, op, eng="vector"):
        return s.op(eng, lambda e: e.tensor_tensor(out.ap, a.ap, b.ap, op),
                    reads=s._k(a, b), writes=s._k(out))

    def ts(s, out, a, s1, op0, s2=None, op1=None, eng="vector", accum=None):
        kw = {}
        if op1 is not None:
            kw["op1"] = op1
        if accum is not None:
            kw["accum_out"] = accum.ap
        return s.op(eng, lambda e: e.tensor_scalar(out.ap, a.ap, s._a(s1), s._a(s2), op0, **kw),
                    reads=s._k(a, s1, s2), writes=s._k(out, accum))

    def stt(s, out, a, sc, b, op0, op1):
        return s.op("vector", lambda e: e.scalar_tensor_tensor(out.ap, a.ap, s._a(sc), b.ap, op0, op1),
                    reads=s._k(a, sc, b), writes=s._k(out))

    def cp(s, out, in_, eng="vector"):
        if eng == "scalar":
            return s.op("scalar", lambda e: e.copy(out.ap, in_.ap), reads=s._k(in_), writes=s._k(out))
        return s.op(eng, lambda e: e.tensor_copy(out.ap, in_.ap), reads=s._k(in_), writes=s._k(out))

    def memset(s, out, val, eng="vector"):
        return s.op(eng, lambda e: e.memset(out.ap, val), writes=s._k(out))

    def red(s, out, in_, op, axis=AX.X):
        return s.op("vector", lambda e: e.tensor_reduce(out.ap, in_.ap, axis, op),
                    reads=s._k(in_), writes=s._k(out))

    def recip(s, out, in_):
        return s.op("vector", lambda e: e.reciprocal(out.ap, in_.ap), reads=s._k(in_), writes=s._k(out))

    def scan(s, out, d0, d1, initial, op0, op1):
        return s.op("vector", lambda e: e.tensor_tensor_scan(out.ap, d0.ap, d1.ap, s._a(initial), op0, op1),
                    reads=s._k(d0, d1, initial), writes=s._k(out))

    def dma(s, out, in_, eng="sync"):
        return s._dma(eng, lambda e: e.dma_start(out=out.ap, in_=in_.ap), reads=s._k(in_), writes=s._k(out))


def make_consts():
    c = {}
    c["ident"] = np.eye(128, dtype=np.float32)
    p = np.arange(128)[:, None]
    f = np.arange(128)[None, :]
    c["m_plt"] = (p < f).astype(np.float32)
    c["m_ple"] = (p <= f).astype(np.float32)
    c["m_pgt"] = (p > f).astype(np.float32)
    c["m_pge"] = (p >= f).astype(np.float32)
    bo = np.zeros((128, 128), np.float32)
    bo[:64, :64] = 1
    bo[64:, 64:] = 1
    c["blockones"] = bo
    c["ones"] = np.ones((128, 128), np.float32)
    return np.stack([c[k] for k in ("ident", "m_plt", "m_ple", "m_pgt", "m_pge", "blockones", "ones")], 0)


CONST_NAMES = ("ident", "m_plt", "m_ple", "m_pgt", "m_pge", "blockones", "ones")

W_SPECS = {
    "w_ada": [DEPTH, D, 6 * D], "b_ada": [DEPTH, 6 * D], "norm_mix_g": [DEPTH, D], "norm_ffn_g": [DEPTH, D],
    "w_in": [DEPTH, D, IN_DIM], "rwkv_mu": [DEPTH, 2, RWKV_IN], "rwkv_w0": [DEPTH, 2, 1024],
    "rwkv_w2": [DEPTH, 2, 96, 1024], "rwkv_a0": [DEPTH, 2, 1024], "rwkv_a2": [DEPTH, 2, 96, 1024],
    "rwkv_g2": [DEPTH, 256, 1024], "rwkv_k_k": [DEPTH, 1024], "rwkv_k_a": [DEPTH, 1024],
    "rwkv_r_k": [DEPTH, 16, 64], "rwkv_ln_g": [DEPTH, 1024], "rwkv_ln_b": [DEPTH, 1024],
    "gla_conv": [DEPTH, 3, 2048], "gla_alpha_w2": [DEPTH, 2, 16, 512], "gla_alpha_b": [DEPTH, 2, 512],
    "gla_norm_g": [DEPTH, 256], "w_branch_a": [DEPTH, 1024, D], "w_branch_b": [DEPTH, 1024, D],
    "w_out": [DEPTH, D, D], "router_w": [D, NE], "router_b": [1, NE],
    "exp_w_gate": [DEPTH, NE, D, DE], "exp_w_up": [DEPTH, NE, D, DE], "exp_w_down": [DEPTH, NE, DE, D],
    "final_norm_g": [D],
}


class K:
    def __init__(s, nc, stop_after=None, dbg=False):
        s.nc = nc
        s.P = Prog(nc)
        s.stop_after = stop_after
        s.skip_rwkv = False
        s.dbg = dbg
        P = s.P
        s.inp = {}
        s.inp["x"] = P.dram("x", [2048, D], kind="ExternalInput")
        s.inp["ctx"] = P.dram("ctx", [CTX, D], kind="ExternalInput")
        s.inp["c"] = P.dram("c", [1, D], kind="ExternalInput")
        s.inp["c_ctx"] = P.dram("c_ctx", [1, D], kind="ExternalInput")
        s.inp["consts"] = P.dram("consts", [len(CONST_NAMES), 128, 128], kind="ExternalInput")
        for k, shp in W_SPECS.items():
            s.inp[k] = P.dram(k, shp, kind="ExternalInput")
        s.out = P.dram("out", [2048, D], kind="ExternalOutput")
        kd = "ExternalOutput" if dbg else "Internal"
        s.pT = P.dram("pT", [IN_DIM, T], kind=kd)
        s.xmid = P.dram("xmid", [T, D], kind=kd)
        s.xcur = P.dram("xcur", [T, D], kind=kd)
        s.yaT = P.dram("yaT", [1024, T], BF16, kind=kd)
        s.ybT = P.dram("ybT", [1024, T], BF16, kind=kd)
        s.moddbg = P.dram("moddbg", [128, 256], kind=kd)
        s.modrow = P.dram("modrow", [2, 6 * D], kind=kd)
        s.inp["sel16"] = P.dram("sel16", [16, 16, 128], kind="ExternalInput")

        s.C = {}
        for i, nm in enumerate(CONST_NAMES):
            b = P.sb("c_" + nm, [128, 128])
            P.dma(b[:], s.inp["consts"][i])
            s.C[nm] = b
        s.psb = [P.ps(f"bank{i}", [128, 512]) for i in range(8)]
        s.psi = 0

    def bank(s):
        b = s.psb[s.psi % 8]
        s.psi += 1
        return b

    def transpose_pack(s, rows_buf, nrows, out_cols, stack=None):
        P = s.P
        b = s.bank()
        P.tr(b[:, 0:nrows], rows_buf[0:nrows, :], s.C["ident"][0:nrows, 0:nrows])
        P.cp(out_cols[:, 0:nrows], b[:, 0:nrows])

    def stage_prologue(s):
        P = s.P
        crow = P.sb("crow", [32, 128])
        P.dma(crow[0:16, :], s.inp["c"][0].re("(c p) -> c p", p=128))
        P.dma(crow[16:32, :], s.inp["c_ctx"][0].re("(c p) -> c p", p=128))
        P.act(crow[:], crow[:], AF.Silu)
        s.cT = P.sb("cT", [128, 32])
        s.transpose_pack(crow, 32, s.cT)

    def stage_mod(s, l):
        P = s.P
        with contextlib.ExitStack() as st:
            packA = P.sb("packA", [128, 128], stack=st)
            P.dma(packA[0:96, :], s.inp["b_ada"][l].re("(c p) -> c p", p=128))
            P.dma(packA[96:112, :], s.inp["norm_mix_g"][l].re("(c p) -> c p", p=128))
            P.dma(packA[112:128, :], s.inp["norm_ffn_g"][l].re("(c p) -> c p", p=128))
            colA = s.colA
            s.transpose_pack(packA, 128, colA)
            wst = [P.sb(f"wada{i}", [128, 16, 512], stack=st) for i in range(2)]
            acc = s.bank()
            for blk in range(24):
                w = wst[blk % 2]
                src = s.inp["w_ada"][l].re("(kc p) n -> p kc n", p=128)[:, :, blk * 512:(blk + 1) * 512]
                P.dma(w[:], src)
                for j in range(4):
                    dc = blk * 4 + j
                    for kc in range(16):
                        rhs = s.cT[:].re("p (w k) -> p w k", w=2)[:, :, kc]
                        P.mm(acc[:, dc * 2:dc * 2 + 2], w[:, kc, j * 128:(j + 1) * 128], rhs,
                             start=(kc == 0), stop=(kc == 15))
            mod = s.mod
            accv = acc[:, 0:192].re("p (c w) -> p c w", w=2)
            for w_ in range(2):
                P.tt(mod[:, :, w_], accv[:, :, w_], colA[:, 0:96], ALU.add)
            for w_ in range(2):
                P.stt(s.m1[:, :, w_], mod[:, 16:32, w_], 1.0, colA[:, 96:112], ALU.add, ALU.mult)
                P.stt(s.m2[:, :, w_], mod[:, 64:80, w_], 1.0, colA[:, 112:128], ALU.add, ALU.mult)
            if s.dbg:
                P.dma(s.moddbg[:, 0:192], mod[:].re("p c w -> p (c w)"))
            mrow = P.sb("mrow", [128, 128], stack=st)
            mtmp = P.sb("mtmp", [128, 96], stack=st)
            for w_ in range(2):
                P.cp(mtmp[:], mod[:, :, w_])
                bb = s.bank()
                P.tr(bb[0:96, 0:128], mtmp[:, 0:96], s.C["ident"][:])
                P.cp(mrow[0:96, :], bb[0:96, 0:128])
                P.dma(s.modrow[w_].re("(c p) -> c p", p=128), mrow[0:96, :])
        P.barrier()

    def x_src(s, l, tt_):
        if l == 0:
            if tt_ < 2:
                return s.inp["ctx"][tt_ * 128:(tt_ + 1) * 128, :]
            return s.inp["x"][(tt_ - 2) * 128:(tt_ - 1) * 128, :]
        return s.xcur[tt_ * 128:(tt_ + 1) * 128, :]

    def norm_to_hT(s, src_view, hT, tt_, mcol, shcol, w_, xbufs, st_small, hT32=None, sh_off=0):
        P = s.P
        xt = xbufs[tt_ % 2]
        P.dma(xt[:], src_view)
        junk, ss, rstd = st_small
        P.act(junk[:], xt[:], AF.Square, accum=ss[:, 0:1])
        P.ts(ss[:, 0:1], ss[:, 0:1], 1.0 / D, ALU.mult, EPS, ALU.add)
        P.act(ss[:, 0:1], ss[:, 0:1], AF.Sqrt)
        P.recip(rstd[:, 0:1], ss[:, 0:1])
        P.ts(xt[:], xt[:], rstd[:, 0:1], ALU.mult)
        for g in range(4):
            b = s.bank()
            for j in range(4):
                kc = g * 4 + j
                P.tr(b[:, j * 128:(j + 1) * 128], xt[:, kc * 128:(kc + 1) * 128], s.C["ident"][:])
            for j in range(4):
                kc = g * 4 + j
                P.act(hT[:, kc, tt_ * 128:(tt_ + 1) * 128], b[:, j * 128:(j + 1) * 128], AF.Identity,
                      bias=shcol[:, sh_off + kc, w_:w_ + 1], scale=mcol[:, kc, w_:w_ + 1])
                if hT32 is not None:
                    P.act(hT32[:, kc, :], b[:, j * 128:(j + 1) * 128], AF.Identity,
                          bias=shcol[:, sh_off + kc, w_:w_ + 1], scale=mcol[:, kc, w_:w_ + 1])

    def tok_blocks(s, cm):
        blks = [(0, 256, lambda v: v[:, 0:256])]
        for j in range(4):
            if not cm:
                blks.append((256 + 512 * j, 512, lambda v, j=j: v[:, 256 + 512 * j:256 + 512 * (j + 1)]))
            else:
                blks.append((256 + 512 * j, 512,
                             lambda v, j=j: v[:, 256:T].re("p (r c) -> p c r", c=64)[:, 16 * j:16 * (j + 1), :]))
        return blks

    def stage_proj(s, l):
        P = s.P
        with contextlib.ExitStack() as st:
            hT = P.sb("hT", [128, 16, T], BF16, stack=st)
            xbufs = [P.sb(f"xin{i}", [128, D], stack=st) for i in range(2)]
            small = (P.sb("junk", [128, D], stack=st), P.sb("ss", [128, 1], stack=st), P.sb("rstd", [128, 1], stack=st))
            for tt_ in range(NT):
                w_ = 1 if tt_ < 2 else 0
                s.norm_to_hT(s.x_src(l, tt_), hT, tt_, s.m1, s.mod, w_, xbufs, small)
            cbs = [(i * 128, 128) for i in range(24)] + [(3072, 96), (3168, 96), (3264, 128), (3392, 128)]
            c0 = GLA_OFF
            cbs += [(c0 + i * 128, 128) for i in range(16)] + [(c0 + 2048, 16)]
            cbs += [(GLA_GATE_OFF + i * 128, 128) for i in range(8)]
            cbs += [(BR_GATE_OFF + i * 128, 128) for i in range(32)]
            wst = [P.sb(f"wst{i}", [128, 16, 128], stack=st) for i in range(3)]
            wbf = [P.sb(f"wbf{i}", [128, 16, 128], BF16, stack=st) for i in range(3)]
            ob = [P.sb(f"ob{i}", [128, T], stack=st) for i in range(3)]
            win = s.inp["w_in"][l].re("(kc p) n -> p kc n", p=128)
            for bi, (c0, w) in enumerate(cbs):
                cm = GLA_OFF <= c0 < BR_GATE_OFF
                ws, wb, o = wst[bi % 3], wbf[bi % 3], ob[bi % 3]
                P.dma(ws[:, :, 0:w], win[:, :, c0:c0 + w])
                P.cp(wb[:, :, 0:w], ws[:, :, 0:w], eng="gpsimd")
                for ti, (t0, n, fn) in enumerate(s.tok_blocks(cm)):
                    b = s.bank()
                    for kc in range(16):
                        P.mm(b[0:w, 0:n], wb[:, kc, 0:w], fn(hT[:, kc, :]),
                             start=(kc == 0), stop=(kc == 15))
                    if ti % 2 == 0:
                        P.cp(o[0:w, t0:t0 + n], b[0:w, 0:n], eng="scalar")
                    else:
                        P.cp(o[0:w, t0:t0 + n], b[0:w, 0:n], eng="vector")
                P.dma(s.pT[c0:c0 + w, :], o[0:w, :])
        P.barrier()


    def stage_rwkv(s, l):
        P = s.P
        C = s.C
        with contextlib.ExitStack() as st:
            sb = lambda n, shp, dt=F32: P.sb(n, shp, dt, stack=st)
            packB = sb("packB", [128, 128]); colB = sb("colB", [128, 128])
            P.memset(packB[:], 0.0)
            mu = s.inp["rwkv_mu"][l]
            for i in range(2):
                o = 28 * i
                P.dma(packB[o:o + 24, :], mu[i, 0:3072].re("(c p) -> c p", p=128))
                P.dma(packB[o + 24:o + 25, 0:96], mu[i:i + 1, 3072:3168])
                P.dma(packB[o + 25:o + 26, 0:96], mu[i:i + 1, 3168:3264])
                P.dma(packB[o + 26:o + 28, :], mu[i, 3264:3520].re("(c p) -> c p", p=128))
            P.dma(packB[56:72, :], s.inp["rwkv_w0"][l].re("d (c p) -> (d c) p", p=128))
            P.dma(packB[72:88, :], s.inp["rwkv_a0"][l].re("d (c p) -> (d c) p", p=128))
            P.dma(packB[88:96, :], s.inp["rwkv_k_k"][l].re("(c p) -> c p", p=128))
            P.dma(packB[96:104, :], s.inp["rwkv_k_a"][l].re("(c p) -> c p", p=128))
            P.dma(packB[104:112, :], s.inp["rwkv_r_k"][l].re("(c two) k -> c (two k)", two=2))
            P.dma(packB[112:120, :], s.inp["rwkv_ln_g"][l].re("(c p) -> c p", p=128))
            P.dma(packB[120:128, :], s.inp["rwkv_ln_b"][l].re("(c p) -> c p", p=128))
            s.transpose_pack(packB, 128, colB)
            col = lambda i: colB[:, i:i + 1]
            cf = sb("cf", [128, 28])
            P.tt(cf[:], colB[:, 0:28], colB[:, 28:56], ALU.add)
            P.ts(cf[:], cf[:], -1.0, ALU.mult, 1.0, ALU.add)

            def shift_rows(dst, X, blk, n=128):
                P.ts(dst, X, cf[0:n, blk:blk + 1], ALU.mult)
                for (a, b) in ((0, 256), (256, T)):
                    P.stt(dst[:, a + 1:b], X[:, a:b - 1], colB[0:n, blk:blk + 1], dst[:, a + 1:b], ALU.mult, ALU.add)
                    P.stt(dst[:, a:b - 1], X[:, a + 1:b], colB[0:n, 28 + blk:29 + blk], dst[:, a:b - 1], ALU.mult, ALU.add)

            xb = [sb(f"xb{i}", [128, T]) for i in range(2)]
            xbi = [0]

            def load_rows(row0, n):
                X = xb[xbi[0] % 2]
                xbi[0] += 1
                P.dma(X[0:n, :], s.pT[row0:row0 + n, :])
                return X
            txw = sb("txw", [128, T]); xaS = sb("xaS", [128, T]); sxg = sb("sxg", [128, 2, T], BF16)
            for j in range(2):
                X = load_rows(3264 + 128 * j, 128); shift_rows(txw[:, :], X[:, :], 26 + j)
                P.act(sxg[:, j, :], txw[:, :], AF.Sigmoid)
            X = load_rows(3072, 96); shift_rows(txw[0:96, :], X[0:96, :], 24, 96)
            P.act(txw[0:96, :], txw[0:96, :], AF.Tanh)
            X = load_rows(3168, 96); shift_rows(xaS[0:96, :], X[0:96, :], 25, 96)
            r_ = sb("r", [128, T]); k_ = sb("k", [128, T]); v_ = sb("v", [128, T]); kk_ = sb("kk", [128, T])
            vT = sb("vT", [128, NT, 128]); yacc = sb("yacc", [128, NT, 128]); yo = sb("yo", [128, T], BF16)
            w2c = sb("w2c", [96, 2, 128]); a2c = sb("a2c", [96, 2, 128]); g2c = sb("g2c", [128, 2, 128], BF16); g2f = sb("g2f", [128, 2, 128])
            RKb = sb("RKb", [128, 128])
            PhiT = sb("PhiT", [128, 128]); P.memset(PhiT[:], 0.0)
            Sb = [sb(f"S{i}", [128, 64]) for i in range(2)]
            G_ = 2
            NS = 3
            tn = ["lw", "a", "t1", "kd", "b", "cs", "lg", "EA", "EB"]
            tmp = [{n: sb(f"{n}{i}", [128, 128]) for n in tn} for i in range(NS)]
            tb = [{n: sb(f"{n}{i}", [128, 128]) for n in ("bh", "kh", "bend", "kend")} for i in range(NS)]
            KR = [sb(f"KR{i}", [128, 256]) for i in range(NS)]
            TMs = [sb(f"TMs{i}", [128, 384]) for i in range(NS)]
            gC = [sb(f"gC{i}", [128, 1]) for i in range(NS)]
            Php = [sb(f"Php{i}", [128, 128]) for i in range(NS)]
            Wn = [sb(f"Wn{i}", [128, 128]) for i in range(NS)]
            QT = [sb(f"QT{i}", [128, 128]) for i in range(NS)]
            Dl = [sb(f"Dl{i}", [128, 64]) for i in range(NS)]
            hn = ["ArbT", "MkT", "ArkT", "TT0", "TT1", "G"]
            htmp = [[{n: sb(f"{n}{i}{h}", [128, 128] if n != "G" else [128, 64]) for n in hn} for h in range(2)] for i in range(NS)]
            XX = [[[sb(f"XX{i}{h}{j}", [128, 256]) for j in range(2)] for h in range(2)] for i in range(NS)]
            ep = {n: sb(n, [128, 128]) for n in ("ysum", "cen", "junk", "yaff", "rk", "bon")}
            mean = sb("mean", [128, 2]); var = sb("var", [128, 2])
            ident = C["ident"]

            import os as _os
            for hp in range(int(_os.environ.get('RW_HP', 8))):
                hc = slice(hp * 128, (hp + 1) * 128)
                P.dma(w2c[:], s.inp["rwkv_w2"][l].re("d k n -> k d n")[:, :, hc])
                P.dma(a2c[:], s.inp["rwkv_a2"][l].re("d k n -> k d n")[:, :, hc])
                P.dma(g2f[:], s.inp["rwkv_g2"][l].re("(kc p) n -> p kc n", p=128)[:, :, hc])
                P.cp(g2c[:], g2f[:], eng="gpsimd")
                X = load_rows(hp * 128, 128); shift_rows(r_[:, :], X[:, :], hp)
                X = load_rows(1024 + hp * 128, 128); shift_rows(k_[:, :], X[:, :], 8 + hp)
                X = load_rows(2048 + hp * 128, 128); shift_rows(v_[:, :], X[:, :], 16 + hp)
                kkr, sq = xb[0], xb[1]
                P.ts(kkr[:, :], k_[:, :], col(88 + hp), ALU.mult)
                P.tt(sq[:, :], kkr[:, :], kkr[:, :], ALU.mult)
                for t0 in range(0, T, 512):
                    n = min(512, T - t0)
                    b = s.bank()
                    P.mm(b[:, 0:n], C["blockones"][:], sq[:, t0:t0 + n])
                    P.act(sq[:, t0:t0 + n], b[:, 0:n], AF.Sqrt)
                P.ts(sq[:, :], sq[:, :], 1e-12, ALU.max)
                P.recip(sq[:, :], sq[:, :])
                P.tt(kk_[:, :], kkr[:, :], sq[:, :], ALU.mult)
                for c0 in range(0, NT, 4):
                    nn = min(4, NT - c0)
                    b = s.bank()
                    for j in range(nn):
                        P.tr(b[:, j * 128:(j + 1) * 128], v_[:, (c0 + j) * 128:(c0 + j + 1) * 128], ident[:])
                    P.cp(vT[:, c0:c0 + nn, :], b[:, 0:nn * 128].re("p (c f) -> p c f", f=128), eng="scalar")
                P.ts(RKb[:], C["blockones"][:], col(104 + hp), ALU.mult)
                ci = 0
                for d in range(2):
                    order = list(range(NT)) if d == 0 else [1, 0] + list(range(17, 1, -1))
                    mS, mI, mSt = (C["m_plt"], C["m_ple"], C["m_pgt"]) if d == 0 else (C["m_pgt"], C["m_pge"], C["m_plt"])
                    Scur = 0
                    P.memset(Sb[0][:], 0.0)
                    for gi in range(0, int(_os.environ.get('RW_NT', NT)), G_):
                        grp = [(c, (ci + j) % NS) for j, c in enumerate(order[gi:gi + G_])]
                        ci += len(grp)
                        for (c, i) in grp:
                            t = tmp[i]; u = tb[i]; kr = KR[i]
                            tok = slice(c * 128, (c + 1) * 128)
                            zb = s.bank()
                            P.mm(zb[:, 0:128], w2c[0:96, d, :], txw[0:96, tok])
                            P.mm(zb[:, 128:256], a2c[0:96, d, :], xaS[0:96, tok])
                            P.act(t["lw"][:], zb[:, 0:128], AF.Sigmoid, bias=col(56 + 8 * d + hp))
                            P.ts(t["lw"][:], t["lw"][:], -0.6065306597, ALU.mult)
                            P.act(t["a"][:], zb[:, 128:256], AF.Sigmoid, bias=col(72 + 8 * d + hp))
                            P.ts(t["t1"][:], t["a"][:], -1.0, ALU.add, col(96 + hp), ALU.mult)
                            P.stt(t["kd"][:], t["t1"][:], 1.0, k_[:, tok], ALU.add, ALU.mult)
                            P.tt(t["b"][:], kk_[:, tok], t["a"][:], ALU.mult, eng="gpsimd")
                            P.scan(t["cs"][:], C["ones"][:], t["lw"][:], 0.0, ALU.mult, ALU.add)
                            tot = t["cs"][:, 127:128]
                            if d == 0:
                                lg = t["cs"]
                            else:
                                lg = t["lg"]
                                P.ts(lg[:], t["cs"][:], -1.0, ALU.mult, tot, ALU.add)
                                P.tt(lg[:], lg[:], t["lw"][:], ALU.add)
                            P.act(t["EA"][:], lg[:], AF.Exp)
                            P.tt(kr[:, 128:256], r_[:, tok], t["EA"][:], ALU.mult)
                            P.tt(t["t1"][:], lg[:], t["lw"][:], ALU.subtract, eng="gpsimd")
                            P.act(t["EA"][:], t["t1"][:], AF.Exp)
                            P.tt(kr[:, 0:128], kk_[:, tok], t["EA"][:], ALU.mult)
                            P.act(t["EB"][:], lg[:], AF.Exp, scale=-1.0)
                            P.tt(u["bh"][:], t["b"][:], t["EB"][:], ALU.mult)
                            P.tt(u["kh"][:], t["kd"][:], t["EB"][:], ALU.mult, eng="gpsimd")
                            P.act(t["EB"][:], lg[:], AF.Exp, bias=tot, scale=-1.0)
                            P.tt(u["bend"][:], t["b"][:], t["EB"][:], ALU.mult, eng="gpsimd")
                            P.tt(u["kend"][:], t["kd"][:], t["EB"][:], ALU.mult)
                            P.act(gC[i][:], tot, AF.Exp)
                            bt = s.bank()
                            P.tr(bt[:, 0:128], kr[:, 0:128], ident[:])
                            P.tr(bt[:, 128:256], u["bend"][:], ident[:])
                            P.tr(bt[:, 256:384], u["kend"][:], ident[:])
                            P.cp(TMs[i][:], bt[:, 0:384], eng="scalar")
                        chains = [(c, i, h) for (c, i) in grp for h in range(2)]
                        if _os.environ.get('RW_SKIP') == 'p1':
                            continue
                        for (c, i, h) in chains:
                            hs = slice(h * 64, h * 64 + 64)
                            u = tb[i]; kr = KR[i]; ht = htmp[i][h]; xx = XX[i][h]
                            b1 = s.bank(); P.mm(b1[:, 0:256], u["bh"][hs, :], kr[hs, :])
                            b2 = s.bank(); P.mm(b2[:, 0:256], u["kh"][hs, :], kr[hs, :])
                            P.mm(b2[:, 256:384], kr[hs, 0:128], u["bh"][hs, :])
                            P.tt(xx[0][:, 128:256], b1[:, 0:128], mS[:], ALU.mult)
                            P.tt(ht["ArbT"][:], b1[:, 128:256], mI[:], ALU.mult)
                            P.tt(ht["MkT"][:], b2[:, 0:128], mS[:], ALU.mult)
                            P.tt(ht["ArkT"][:], b2[:, 128:256], mI[:], ALU.mult)
                            P.tt(xx[0][:, 0:128], b2[:, 256:384], mSt[:], ALU.mult)
                            P.tt(ht["TT0"][:], ident[:], xx[0][:, 128:256], ALU.subtract, eng="gpsimd")
                        if _os.environ.get('RW_SKIP') == 'p2':
                            continue
                        TTc = {(c, h): "TT0" for (c, i, h) in chains}
                        for lv in range(6):
                            bxs = {}
                            for (c, i, h) in chains:
                                xc = XX[i][h][lv % 2]
                                bx = s.bank(); bxs[(c, h)] = bx
                                P.mm(bx[:, 0:128], xc[:, 128:256], xc[:, 0:128])
                            for (c, i, h) in chains:
                                xn = XX[i][h][(lv + 1) % 2]
                                P.cp(xn[:, 0:128], bxs[(c, h)][:, 0:128], eng="scalar")
                            bqs = {}
                            for (c, i, h) in chains:
                                xn = XX[i][h][(lv + 1) % 2]
                                bq = s.bank(); bqs[(c, h)] = bq
                                P.mm(bq[:, 0:128], xn[:, 0:128], htmp[i][h][TTc[(c, h)]][:])
                                if lv < 5:
                                    P.tr(bq[:, 128:256], xn[:, 0:128], ident[:])
                            for (c, i, h) in chains:
                                cur = TTc[(c, h)]
                                nxt = "TT1" if cur == "TT0" else "TT0"
                                P.tt(htmp[i][h][nxt][:], bqs[(c, h)][:, 0:128], htmp[i][h][cur][:], ALU.add)
                                if lv < 5:
                                    P.cp(XX[i][h][(lv + 1) % 2][:, 128:256], bqs[(c, h)][:, 128:256])
                                TTc[(c, h)] = nxt
                        if _os.environ.get('RW_SKIP') == 'p3':
                            continue
                        for (c, i, h) in chains:
                            hs = slice(h * 64, h * 64 + 64)
                            ht = htmp[i][h]
                            TTf = ht[TTc[(c, h)]]
                            msk = int(_os.environ.get('RW_P4', 15))
                            bg = s.bank()
                            if msk & 1:
                                P.mm(bg[:, 0:64], ht["MkT"][:], vT[:, c, hs])
                                P.cp(ht["G"][:, 0:64], bg[:, 0:64], eng="scalar")
                            bp = s.bank()
                            if msk & 2:
                                P.mm(bp[:, 0:64], TTf[:], TMs[i][:, h * 64:h * 64 + 64])
                                P.mm(bp[:, 64:128], TTf[:], ht["G"][:, 0:64])
                            if msk & 4:
                                P.cp(Php[i][:, hs], bp[:, 0:64], eng="scalar")
                            if msk & 8:
                                P.act(Wn[i][:, hs], bp[:, 64:128], AF.Copy, scale=-1.0)
                        if _os.environ.get('RW_SKIP') == 'p4':
                            continue
                        for (c, i) in grp:
                            t = tmp[i]; tm = TMs[i]
                            tok = slice(c * 128, (c + 1) * 128)
                            BendT, KendT = tm[:, 128:256], tm[:, 256:384]
                            Sc = Sb[Scur]; Sn = Sb[1 - Scur]
                            for h in range(2):
                                hs = slice(h * 64, h * 64 + 64)
                                bq = s.bank()
                                P.mm(bq[:, 0:128], Php[i][:], htmp[i][h]["ArbT"][:])
                                P.tt(QT[i][hs, :], KR[i][hs, 128:256], bq[hs, 0:128], ALU.subtract)
                            by = s.bank()
                            for h in range(2):
                                hs = slice(h * 64, h * 64 + 64)
                                P.mm(by[:, hs], htmp[i][h]["ArkT"][:], vT[:, c, hs], start=True, stop=False)
                                if _os.environ.get('RW_SKIP') == 'qts':
                                    P.mm(by[:, hs], htmp[i][h]["ArbT"][:], Wn[i][:, hs], start=False, stop=True)
                                else:
                                    P.mm(by[:, hs], htmp[i][h]["ArbT"][:], Wn[i][:, hs], start=False, stop=False)
                                    P.mm(by[:, hs], QT[i][hs, :], Sc[hs, :], start=False, stop=True)
                            bf = s.bank(); P.mm(bf[:, 0:128], Php[i][:], BendT)
                            bd = s.bank()
                            P.mm(bd[:, 0:128], KendT, vT[:, c, :], start=True, stop=False)
                            P.mm(bd[:, 0:128], BendT, Wn[i][:], start=False, stop=True)
                            for h in range(2):
                                hs = slice(h * 64, h * 64 + 64)
                                P.stt(PhiT[hs, hs], ident[hs, hs], gC[i][hs, 0:1], bf[hs, hs], ALU.mult, ALU.subtract)
                                P.cp(Dl[i][hs, :], bd[hs, h * 64:h * 64 + 64], eng="scalar")
                            bs = s.bank(); P.mm(bs[:, 0:64], PhiT[:], Sc[:])
                            P.tt(Sn[:], bs[:, 0:64], Dl[i][:], ALU.add)
                            Scur = 1 - Scur
                            if d == 0:
                                P.cp(yacc[:, c, :], by[:, 0:128], eng="scalar")
                                continue
                            if _os.environ.get('RW_NOEP'):
                                continue
                            ys = ep["ysum"]
                            P.tt(ys[:], by[:, 0:128], yacc[:, c, :], ALU.add)
                            P.red(mean[:], ys[:].re("p (h v) -> p h v", h=2), ALU.add)
                            P.ts(mean[:], mean[:], 1.0 / 64, ALU.mult)
                            for h in range(2):
                                hs = slice(h * 64, h * 64 + 64)
                                P.ts(ep["cen"][:, hs], ys[:, hs], mean[:, h:h + 1], ALU.subtract)
                                P.act(ep["junk"][:, hs], ep["cen"][:, hs], AF.Square, accum=var[:, h:h + 1])
                            P.ts(var[:], var[:], 1.0 / 64, ALU.mult, 64e-5, ALU.add)
                            P.act(var[:], var[:], AF.Sqrt)
                            P.recip(var[:], var[:])
                            for h in range(2):
                                hs = slice(h * 64, h * 64 + 64)
                                P.ts(ep["cen"][:, hs], ep["cen"][:, hs], var[:, h:h + 1], ALU.mult)
                            be = s.bank()
                            P.tr(be[:, 0:128], ep["cen"][:], ident[:])
                            P.act(ep["yaff"][:], be[:, 0:128], AF.Identity, bias=col(120 + hp), scale=col(112 + hp))
                            P.tt(ep["rk"][:], r_[:, tok], k_[:, tok], ALU.mult, eng="gpsimd")
                            bb = s.bank()
                            P.mm(bb[:, 0:128], RKb[:], ep["rk"][:])
                            P.mm(bb[:, 128:256], g2c[:, 0, :], sxg[:, 0, tok], start=True, stop=False)
                            P.mm(bb[:, 128:256], g2c[:, 1, :], sxg[:, 1, tok], start=False, stop=True)
                            P.tt(ep["bon"][:], bb[:, 0:128], v_[:, tok], ALU.mult)
                            P.tt(ep["yaff"][:], ep["yaff"][:], ep["bon"][:], ALU.add)
                            P.tt(yo[:, tok], ep["yaff"][:], bb[:, 128:256], ALU.mult)
                P.dma(s.yaT[hc, :], yo[:, :])
        P.barrier()

    def stage_gla(s, l):
        P = s.P
        C = s.C
        ident = C["ident"]
        with contextlib.ExitStack() as st:
            sb = lambda n, shp, dt=F32: P.sb(n, shp, dt, stack=st)
            packC = sb("packC", [128, 128]); colC = sb("colC", [128, 128])
            P.memset(packC[:], 0.0)
            P.dma(packC[0:48, :], s.inp["gla_conv"][l].re("j (c p) -> (j c) p", p=128))
            P.dma(packC[48:56, :], s.inp["gla_alpha_b"][l].re("d (c p) -> (d c) p", p=128))
            P.dma(packC[56:58, :], s.inp["gla_norm_g"][l].re("(c p) -> c p", p=128))
            s.transpose_pack(packC, 64, colC)
            col = lambda i: colC[:, i:i + 1]

            def conv_rows(dst, X, blk):
                P.ts(dst, X, col(16 + blk), ALU.mult)
                for (a, b) in ((0, 256), (256, T)):
                    P.stt(dst[:, a + 1:b], X[:, a:b - 1], col(blk), dst[:, a + 1:b], ALU.mult, ALU.add)
                    P.stt(dst[:, a:b - 1], X[:, a + 1:b], col(32 + blk), dst[:, a:b - 1], ALU.mult, ALU.add)
                P.act(dst, dst, AF.Silu)
            xb = [sb(f"gxb{i}", [128, T]) for i in range(2)]
            xbi = [0]

            def load_rows(row0, n):
                X = xb[xbi[0] % 2]
                xbi[0] += 1
                P.dma(X[0:n, :], s.pT[row0:row0 + n, :])
                return X
            adS = sb("adS", [16, T])
            P.dma(adS[:], s.pT[GLA_OFF + 2048:GLA_OFF + 2064, :])
            q_ = sb("q", [128, T]); k_ = sb("gk", [128, T]); v_ = sb("gv", [128, 2, T]); gt = sb("gt", [128, 2, T])
            vT = sb("gvT", [128, NT, 256]); oaccD = [sb(f"oacc{d}", [128, NT, 256]) for d in range(2)]; yo = sb("gyo", [128, 2, T], BF16)
            aw = sb("aw", [16, 2, 128])
            SbD = [[sb(f"gS{d}{i}", [128, 256]) for i in range(2)] for d in range(2)]
            NS = 4
            tn = ["g", "cs", "gc", "E1", "E2", "E4", "qg", "kg", "kend", "attT", "kendT"]
            tmp = [{n: sb(f"g{n}{i}", [128, 128]) for n in tn} for i in range(NS)]
            gCe = [sb(f"gCe{i}", [128, 1]) for i in range(NS)]
            osumE = [sb(f"osum{i}", [128, 256]) for i in range(2)]; junk = sb("gjunk", [128, 256]); ssqE = [sb(f"ssq{i}", [128, 1]) for i in range(2)]
            for hd in range(4):
                P.dma(aw[:], s.inp["gla_alpha_w2"][l].re("d k n -> k d n")[:, :, hd * 128:(hd + 1) * 128])
                X = load_rows(GLA_OFF + hd * 128, 128); conv_rows(q_[:, :], X[:, :], hd)
                X = load_rows(GLA_OFF + 512 + hd * 128, 128); conv_rows(k_[:, :], X[:, :], 4 + hd)
                for vc in range(2):
                    X = load_rows(GLA_OFF + 1024 + hd * 256 + vc * 128, 128)
                    conv_rows(v_[:, vc, :], X[:, :], 8 + hd * 2 + vc)
                    P.dma(gt[:, vc, :], s.pT[GLA_GATE_OFF + hd * 256 + vc * 128:GLA_GATE_OFF + hd * 256 + (vc + 1) * 128, :])
                    P.act(gt[:, vc, :], gt[:, vc, :], AF.Silu)
                for c in range(NT):
                    b = s.bank()
                    for vc in range(2):
                        P.tr(b[:, vc * 128:(vc + 1) * 128], v_[:, vc, c * 128:(c + 1) * 128], ident[:])
                    P.cp(vT[:, c, :], b[:, 0:256], eng="scalar")
                orders = {0: list(range(NT)), 1: [1, 0] + list(range(17, 1, -1))}
                Scur = {0: 0, 1: 0}
                for d in range(2):
                    P.memset(SbD[d][0][:], 0.0)
                for step in range(NT):
                    for d in range(2):
                        mI = C["m_ple"] if d == 0 else C["m_pge"]
                        c = orders[d][step]
                        i = d * 2 + step % 2
                        t = tmp[i]
                        tok = slice(c * 128, (c + 1) * 128)
                        zb = s.bank()
                        P.mm(zb[:, 0:128], aw[0:16, d, :], adS[0:16, tok])
                        P.act(t["g"][:], zb[:, 0:128], AF.Sigmoid, bias=col(48 + 4 * d + hd))
                        P.act(t["g"][:], t["g"][:], AF.Ln)
                        P.ts(t["g"][:], t["g"][:], 1.0 / 16, ALU.mult)
                        P.scan(t["cs"][:], C["ones"][:], t["g"][:], 0.0, ALU.mult, ALU.add)
                        tot = t["cs"][:, 127:128]
                        if d == 0:
                            gc = t["cs"]
                        else:
                            gc = t["gc"]
                            P.ts(gc[:], t["cs"][:], -1.0, ALU.mult, tot, ALU.add)
                            P.tt(gc[:], gc[:], t["g"][:], ALU.add)
                        P.act(t["E1"][:], gc[:], AF.Exp)
                        P.stt(t["qg"][:], q_[:, tok], 128 ** -0.5, t["E1"][:], ALU.mult, ALU.mult)
                        P.act(t["E2"][:], gc[:], AF.Exp, scale=-1.0)
                        P.tt(t["kg"][:], k_[:, tok], t["E2"][:], ALU.mult, eng="gpsimd")
                        P.act(t["E4"][:], gc[:], AF.Exp, bias=tot, scale=-1.0)
                        P.tt(t["kend"][:], k_[:, tok], t["E4"][:], ALU.mult, eng="gpsimd")
                        P.act(gCe[i][:], tot, AF.Exp)
                        ba = s.bank(); P.mm(ba[:, 0:128], t["kg"][:], t["qg"][:])
                        P.tt(t["attT"][:], ba[:, 0:128], mI[:], ALU.mult)
                        bt = s.bank(); P.tr(bt[:, 0:128], t["kend"][:], ident[:])
                        P.cp(t["kendT"][:], bt[:, 0:128], eng="scalar")
                        Sc = SbD[d][Scur[d]]; Sn = SbD[d][1 - Scur[d]]
                        bo = s.bank()
                        P.mm(bo[:, 0:256], t["attT"][:], vT[:, c, :], start=True, stop=False)
                        P.mm(bo[:, 0:256], t["qg"][:], Sc[:], start=False, stop=True)
                        bs = s.bank(); P.mm(bs[:, 0:256], t["kendT"][:], vT[:, c, :])
                        P.stt(Sn[:], Sc[:], gCe[i][:, 0:1], bs[:, 0:256], ALU.mult, ALU.add)
                        Scur[d] = 1 - Scur[d]
                        P.cp(oaccD[d][:, c, :], bo[:, 0:256], eng="scalar")
                for c in range(NT):
                    tok = slice(c * 128, (c + 1) * 128)
                    e_ = c % 2
                    P.tt(osumE[e_][:], oaccD[0][:, c, :], oaccD[1][:, c, :], ALU.add, eng="gpsimd")
                    P.act(junk[:], osumE[e_][:], AF.Square, accum=ssqE[e_][:, 0:1])
                    P.ts(ssqE[e_][:], ssqE[e_][:], 1.0 / 256, ALU.mult, EPS, ALU.add)
                    P.act(ssqE[e_][:], ssqE[e_][:], AF.Sqrt)
                    P.recip(ssqE[e_][:], ssqE[e_][:])
                    P.ts(osumE[e_][:], osumE[e_][:], ssqE[e_][:, 0:1], ALU.mult)
                    be = s.bank()
                    for vc in range(2):
                        P.tr(be[:, vc * 128:(vc + 1) * 128], osumE[e_][:, vc * 128:(vc + 1) * 128], ident[:])
                    for vc in range(2):
                        P.stt(yo[:, vc, tok], be[:, vc * 128:(vc + 1) * 128], col(56 + vc), gt[:, vc, tok], ALU.mult, ALU.mult)
                for vc in range(2):
                    P.dma(s.ybT[hd * 256 + vc * 128:hd * 256 + (vc + 1) * 128, :], yo[:, vc, :])
        P.barrier()


    def stage_merge(s, l, last):
        P = s.P
        with contextlib.ExitStack() as st:
            sb = lambda n, shp, dt=F32: P.sb(n, shp, dt, stack=st)
            yaS = sb("yaS", [128, 8, T], BF16); ybS = sb("ybS", [128, 8, T], BF16)
            for kc in range(8):
                P.dma(yaS[:, kc, :], s.yaT[kc * 128:(kc + 1) * 128, :])
                P.dma(ybS[:, kc, :], s.ybT[kc * 128:(kc + 1) * 128, :])
            gbc = sb("gbc", [128, 2, D])
            for w_ in range(2):
                P.dma(gbc[:, w_, :], s.modrow[w_, 2 * D:3 * D].pb(128))
            wa_st = sb("wa_st", [128, 8, 128]); wb_st = sb("wb_st", [128, 8, 128])
            wa = sb("wa", [128, 8, 128], BF16); wb = sb("wb", [128, 8, 128], BF16)
            gaS = sb("gaS", [128, 512]); gbS = sb("gbS", [128, 512])
            mix = sb("mix", [128, 16, 512], BF16)
            m1 = sb("mm1", [128, 512]); m2 = sb("mm2", [128, 512])
            wo_st = sb("wo_st", [128, 16, 512]); wo = sb("wo", [128, 16, 512], BF16)
            xt = sb("mxt", [128, 512]); yt = sb("myt", [128, 512])
            blocks = [(256 + 512 * j, 512, 0, j) for j in range(4)]
            if not last:
                blocks = [(0, 256, 1, -1)] + blocks
            wav = s.inp["w_branch_a"][l].re("(kc p) n -> p kc n", p=128)
            wbv = s.inp["w_branch_b"][l].re("(kc p) n -> p kc n", p=128)
            wov = s.inp["w_out"][l].re("(kc p) n -> p kc n", p=128)
            for (t0, n, w_, j) in blocks:
                for dc in range(16):
                    dcs = slice(dc * 128, (dc + 1) * 128)
                    P.dma(wa_st[:], wav[:, :, dcs]); P.cp(wa[:], wa_st[:], eng="gpsimd")
                    P.dma(wb_st[:], wbv[:, :, dcs]); P.cp(wb[:], wb_st[:], eng="gpsimd")
                    P.dma(gaS[:, 0:n], s.pT[BR_GATE_OFF + dc * 128:BR_GATE_OFF + (dc + 1) * 128, t0:t0 + n])
                    P.dma(gbS[:, 0:n], s.pT[BR_GATE_OFF + D + dc * 128:BR_GATE_OFF + D + (dc + 1) * 128, t0:t0 + n])
                    bA = s.bank()
                    for kc in range(8):
                        P.mm(bA[:, 0:n], wa[:, kc, :], yaS[:, kc, t0:t0 + n], start=(kc == 0), stop=(kc == 7))
                    bB = s.bank()
                    for kc in range(8):
                        if j < 0:
                            rhs = ybS[:, kc, 0:256]
                        else:
                            rhs = ybS[:, kc, 256:T].re("p (c r) -> p r c", r=32)[:, 8 * j:8 * j + 8, :]
                        P.mm(bB[:, 0:n], wb[:, kc, :], rhs, start=(kc == 0), stop=(kc == 7))
                    P.act(gaS[:, 0:n], gaS[:, 0:n], AF.Sigmoid)
                    P.act(gbS[:, 0:n], gbS[:, 0:n], AF.Sigmoid)
                    P.tt(m1[:, 0:n], bA[:, 0:n], gaS[:, 0:n], ALU.mult)
                    P.tt(m2[:, 0:n], bB[:, 0:n], gbS[:, 0:n], ALU.mult)
                    P.tt(mix[:, dc, 0:n], m1[:, 0:n], m2[:, 0:n], ALU.add, eng="gpsimd")
                for db in range(4):
                    dbs = slice(db * 512, (db + 1) * 512)
                    P.dma(wo_st[:], wov[:, :, dbs]); P.cp(wo[:], wo_st[:], eng="gpsimd")
                    for ti in range(n // 128):
                        tile = t0 // 128 + ti
                        rows = slice(tile * 128, (tile + 1) * 128)
                        bo = s.bank()
                        for kc in range(16):
                            P.mm(bo[:, 0:512], mix[:, kc, ti * 128:(ti + 1) * 128], wo[:, kc, :], start=(kc == 0), stop=(kc == 15))
                        P.dma(xt[:], s.x_src(l, tile)[:, dbs])
                        P.tt(yt[:], bo[:, 0:512], gbc[:, w_, dbs], ALU.mult)
                        P.tt(yt[:], yt[:], xt[:], ALU.add)
                        P.dma(s.xmid[rows, dbs], yt[:])
        P.barrier()

    def stage_ffn(s, l, last):
        P = s.P
        C = s.C
        tiles = list(range(2, NT)) if last else list(range(NT))
        groups = [tiles[0:6], tiles[6:12], tiles[12:18]] if not last else [tiles[0:6], tiles[6:11], tiles[11:16]]
        with contextlib.ExitStack() as st0:
            sb0 = lambda n, shp, dt=F32: P.sb(n, shp, dt, stack=st0)
            rw = sb0("rw", [128, 16, NE]); P.dma(rw[:], s.inp["router_w"][:].re("(kc p) e -> p kc e", p=128))
            rb = sb0("rb", [128, NE]); P.dma(rb[:], s.inp["router_b"][0].pb(128))
            s.sel16 = sb0("sel16s", [16, 16, 128])
            P.dma(s.sel16[:], s.inp["sel16"][:])
            hT2 = sb0("hT2", [128, 16, 768], BF16); acc = sb0("acc", [128, 6, D]); gatesT = sb0("gatesT", [16, 768])
            for grp in groups:
                ng = len(grp); ntok = ng * 128
                tbl = [(a, min(512, ntok - a)) for a in range(0, ntok, 512)]
                with contextlib.ExitStack() as stA:
                    sbA = lambda n, shp, dt=F32: P.sb(n, shp, dt, stack=stA)
                    xb1 = sbA("fx", [128, D])
                    small = (sbA("fjunk", [128, D]), sbA("fss", [128, 1]), sbA("frstd", [128, 1]))
                    hT32 = sbA("hT32", [128, 16, 128])
                    q = {n: sbA("r_" + n, [128, 16]) for n in ("sc", "sel", "msk", "eq1", "msk2", "eq2", "wts", "gates")}
                    g4 = {n: sbA("r4_" + n, [128, 4]) for n in ("gs", "t4", "gmask", "pen")}
                    r1 = {n: sbA("r1_" + n, [128, 1]) for n in ("gmax", "m1", "m2", "ssum")}
                    for ti, tile in enumerate(grp):
                        w_ = 1 if tile < 2 else 0
                        s.norm_to_hT(s.xmid[tile * 128:(tile + 1) * 128, :], hT2, ti, s.m2, s.mod, w_, [xb1, xb1], small,
                                     hT32=hT32, sh_off=48)
                        br = s.bank()
                        for kc in range(16):
                            P.mm(br[:, 0:NE], hT32[:, kc, :], rw[:, kc, :], start=(kc == 0), stop=(kc == 15))
                        P.act(q["sc"][:], br[:, 0:NE], AF.Sigmoid)
                        P.tt(q["sel"][:], q["sc"][:], rb[:], ALU.add)
                        s4 = q["sel"][:].re("p (g j) -> p g j", j=4)
                        first = True
                        for a_ in range(4):
                            for b_ in range(a_ + 1, 4):
                                if first:
                                    P.tt(g4["gs"][:], s4[:, :, a_], s4[:, :, b_], ALU.add)
                                    first = False
                                else:
                                    P.tt(g4["t4"][:], s4[:, :, a_], s4[:, :, b_], ALU.add)
                                    P.tt(g4["gs"][:], g4["gs"][:], g4["t4"][:], ALU.max)
                        P.red(r1["gmax"][:], g4["gs"][:], ALU.max)
                        P.ts(g4["gmask"][:], g4["gs"][:], r1["gmax"][:, 0:1], ALU.is_ge)
                        P.ts(g4["pen"][:], g4["gmask"][:], -1.0, ALU.add, 1e9, ALU.mult)
                        m4 = q["msk"][:].re("p (g j) -> p g j", j=4)
                        for j_ in range(4):
                            P.tt(m4[:, :, j_], s4[:, :, j_], g4["pen"][:], ALU.add)
                        P.red(r1["m1"][:], q["msk"][:], ALU.max)
                        P.ts(q["eq1"][:], q["msk"][:], r1["m1"][:, 0:1], ALU.is_equal)
                        P.stt(q["msk2"][:], q["eq1"][:], -1e9, q["msk"][:], ALU.mult, ALU.add)
                        P.red(r1["m2"][:], q["msk2"][:], ALU.max)
                        P.ts(q["eq2"][:], q["msk2"][:], r1["m2"][:, 0:1], ALU.is_equal)
                        P.tt(q["eq1"][:], q["eq1"][:], q["eq2"][:], ALU.add)
                        P.tt(q["wts"][:], q["sc"][:], q["eq1"][:], ALU.mult)
                        P.red(r1["ssum"][:], q["wts"][:], ALU.add)
                        P.recip(r1["ssum"][:], r1["ssum"][:])
                        P.ts(q["gates"][:], q["wts"][:], r1["ssum"][:, 0:1], ALU.mult)
                        bt = s.bank()
                        P.tr(bt[0:NE, 0:128], q["gates"][:, 0:NE], C["ident"][:])
                        P.cp(gatesT[0:NE, ti * 128:(ti + 1) * 128], bt[0:NE, 0:128])
                P.barrier()
                with contextlib.ExitStack() as stB:
                    sbB = lambda n, shp, dt=F32: P.sb(n, shp, dt, stack=stB)
                    wd = sbB("wd", [128, 11, D], BF16)
                    stg = [sbB(f"stg{i}", [128, D]) for i in range(3)]
                    wgb = sbB("wgb", [128, 16, 128], BF16); wub = sbB("wub", [128, 16, 128], BF16)
                    actE = sbB("actE", [128, 11, 768], BF16); gb_ = sbB("gb_", [128, 768])
                    sg = sbB("sg", [128, 512]); tq = sbB("tq", [128, 512])
                    for e in range(NE):
                        for (a, n) in tbl:
                            bk = s.bank()
                            P.mm(bk[:, 0:n], s.sel16[0:16, e, :], gatesT[0:16, a:a + n])
                            P.cp(gb_[:, a:a + n], bk[:, 0:n], eng="scalar")
                        wgv = s.inp["exp_w_gate"][l, e].re("(kc p) f -> p kc f", p=128)
                        wuv = s.inp["exp_w_up"][l, e].re("(kc p) f -> p kc f", p=128)
                        for fc in range(11):
                            fs = slice(fc * 128, (fc + 1) * 128)
                            s0 = stg[0][:].re("p (kc f) -> p kc f", f=128)
                            s1 = stg[1][:].re("p (kc f) -> p kc f", f=128)
                            P.dma(s0, wgv[:, :, fs]); P.cp(wgb[:], s0, eng="vector")
                            P.dma(s1, wuv[:, :, fs]); P.cp(wub[:], s1, eng="scalar")
                            P.dma(stg[2][:], s.inp["exp_w_down"][l, e, fs, :]); P.cp(wd[:, fc, :], stg[2][:], eng="gpsimd")
                            for (a, n) in tbl:
                                bg = s.bank()
                                for kc in range(16):
                                    P.mm(bg[:, 0:n], wgb[:, kc, :], hT2[:, kc, a:a + n], start=(kc == 0), stop=(kc == 15))
                                bu = s.bank()
                                for kc in range(16):
                                    P.mm(bu[:, 0:n], wub[:, kc, :], hT2[:, kc, a:a + n], start=(kc == 0), stop=(kc == 15))
                                P.act(sg[:, 0:n], bg[:, 0:n], AF.Silu)
                                P.tt(tq[:, 0:n], bu[:, 0:n], gb_[:, a:a + n], ALU.mult)
                                P.tt(actE[:, fc, a:a + n], sg[:, 0:n], tq[:, 0:n], ALU.mult, eng="gpsimd")
                        for ti in range(ng):
                            for db in range(4):
                                dbs = slice(db * 512, (db + 1) * 512)
                                bo = s.bank()
                                for fc in range(11):
                                    P.mm(bo[:, 0:512], actE[:, fc, ti * 128:(ti + 1) * 128], wd[:, fc, dbs], start=(fc == 0), stop=(fc == 10))
                                if e == 0:
                                    P.cp(acc[:, ti, dbs], bo[:, 0:512], eng="scalar")
                                else:
                                    P.tt(acc[:, ti, dbs], bo[:, 0:512], acc[:, ti, dbs], ALU.add)
                P.barrier()
                with contextlib.ExitStack() as stC:
                    sbC = lambda n, shp, dt=F32: P.sb(n, shp, dt, stack=stC)
                    gbc = sbC("fgbc", [128, 2, D])
                    for w_ in range(2):
                        P.dma(gbc[:, w_, :], s.modrow[w_, 5 * D:6 * D].pb(128))
                    fng = sbC("fng", [128, D])
                    if last:
                        P.dma(fng[:], s.inp["final_norm_g"][:].pb(128))
                    xt = sbC("cxt", [128, D]); yt = sbC("cyt", [128, D]); junk = sbC("cjunk", [128, D])
                    ss = sbC("css", [128, 1])
                    for ti, tile in enumerate(grp):
                        w_ = 1 if tile < 2 else 0
                        rows = slice(tile * 128, (tile + 1) * 128)
                        P.dma(xt[:], s.xmid[rows, :])
                        P.tt(yt[:], acc[:, ti, :], gbc[:, w_, :], ALU.mult)
                        P.tt(yt[:], yt[:], xt[:], ALU.add)
                        if not last:
                            P.dma(s.xcur[rows, :], yt[:])
                        else:
                            P.act(junk[:], yt[:], AF.Square, accum=ss[:, 0:1])
                            P.ts(ss[:], ss[:], 1.0 / D, ALU.mult, EPS, ALU.add)
                            P.act(ss[:], ss[:], AF.Sqrt)
                            P.recip(ss[:], ss[:])
                            P.stt(yt[:], yt[:], ss[:, 0:1], fng[:], ALU.mult, ALU.mult)
                            P.dma(s.out[(tile - 2) * 128:(tile - 1) * 128, :], yt[:])
                P.barrier()

    def build(s):
        P = s.P
        s.colA = P.sb("colA", [128, 128])
        s.mod = P.sb("mod", [128, 96, 2])
        s.m1 = P.sb("m1", [128, 16, 2])
        s.m2 = P.sb("m2", [128, 16, 2])
        s.stage_prologue()
        fin = []
        for l in range(DEPTH):
            s.stage_mod(l)
            if s.stop_after == ("mod", l):
                break
            s.stage_proj(l)
            if s.stop_after == ("proj", l):
                break
            if not s.skip_rwkv:
                s.stage_rwkv(l)
            if s.stop_after == ("rwkv", l):
                break
            s.stage_gla(l)
            if s.stop_after == ("gla", l):
                break
            last = (l == DEPTH - 1)
            s.stage_merge(l, last)
            if s.stop_after == ("merge", l):
                break
            s.stage_ffn(l, last)
            if s.stop_after == ("ffn", l):
                break
        P.barrier()
        P.final_tokens = [(sk, v) for sk, v in P.dma_tgt.items()]
        P.emit()
        P.st.close()


def build_nc(stop_after=None, dbg=False):
    nc = bass.Bass("TRN2", target_bir_lowering=False)
    k = K(nc, stop_after=stop_after, dbg=dbg)
    k.build()
    return nc


def make_in_maps(inputs, cores):
    consts = make_consts()
    shared = {}
    for k, shp in W_SPECS.items():
        shared[k] = np.ascontiguousarray(np.asarray(inputs[k], dtype=np.float32).reshape(shp))
    maps = []
    for b in cores:
        m = dict(shared)
        m["x"] = np.ascontiguousarray(inputs["x"][b])
        m["ctx"] = np.ascontiguousarray(inputs["ctx"][b])
        m["c"] = np.ascontiguousarray(inputs["c"][b:b + 1])
        m["c_ctx"] = np.ascontiguousarray(np.asarray(inputs["c_ctx"]).reshape(1, D))
        m["consts"] = consts
        sel = np.zeros((16, 16, 128), np.float32)
        for e in range(16):
            sel[e, e, :] = 1.0
        m["sel16"] = sel
        maps.append(m)
    return maps


def kernel(**inputs):
    nc = build_nc()
    maps = make_in_maps(inputs, list(range(8)))
    res = run_bass_kernel_spmd(nc, maps, core_ids=list(range(8)))
    return np.stack([r["out"] for r in res.results], 0).astype(np.float32)
```

```python
import contextlib
import numpy as np
import concourse.bass as bass
import concourse.mybir as mybir
from concourse.bass_utils import run_bass_kernel_spmd

F32 = mybir.dt.float32
BF16 = mybir.dt.bfloat16
AF = mybir.ActivationFunctionType
ALU = mybir.AluOpType
AX = mybir.AxisListType

ENGS = ("tensor", "vector", "scalar", "gpsimd", "sync")
N_DMA_SEMS = 12
import os as _os0
SEQ_REPLAY = bool(_os0.environ.get('SEQ_REPLAY'))
NO_SAME_ENGINE_SYNC = False

D = 2048
T = 2304
NT = 18
CTX = 256
DEPTH = 2
RWKV_IN = 3520
GLA_OFF = 3520
GLA_GATE_OFF = 5584
BR_GATE_OFF = 6608
IN_DIM = 10704
NE = 16
DE = 1408
EPS = 1e-6


class View:
    def __init__(s, buf, ap, keys):
        s.buf, s.ap, s.keys = buf, ap, keys

    def re(s, pat, **kw):
        return View(s.buf, s.ap.rearrange(pat, **kw), s.keys)

    def __getitem__(s, idx):
        return View(s.buf, s.ap[idx], s.keys)

    def pb(s, n):
        return View(s.buf, s.ap.partition_broadcast(n), s.keys)


class Buf:
    def __init__(s, name, h, nsub=None):
        s.name, s.h, s.nsub = name, h, nsub

    def allkeys(s):
        if s.nsub:
            return [(s.name, i) for i in range(s.nsub)]
        return [s.name]

    def __getitem__(s, idx):
        return View(s, s.h[idx], s.allkeys())

    def sub(s, i, idx):
        return View(s, s.h[idx], [(s.name, i)])


class Prog:
    def __init__(s, nc):
        s.nc = nc
        s.st = contextlib.ExitStack()
        s.ops = {e: [] for e in ENGS}
        s.cnt = {e: 0 for e in ENGS}
        s.seen = {e: {} for e in ENGS}
        s.last_w = {}
        s.readers = {}
        s.dma_rr = {e: 0 for e in ENGS}
        s.dma_tgt = {}
        s.final_tokens = []
        s.nbuf = 0
        s.defer = None

    def sb(s, name, shape, dt=F32, nsub=None, stack=None):
        s.nbuf += 1
        nm = f"{name}_{s.nbuf}"
        h = (stack or s.st).enter_context(s.nc.sbuf_tensor(nm, list(shape), dt))
        return Buf(nm, h, nsub)

    def ps(s, name, shape, dt=F32, stack=None):
        s.nbuf += 1
        nm = f"{name}_{s.nbuf}"
        h = (stack or s.st).enter_context(s.nc.psum_tensor(nm, list(shape), dt))
        return Buf(nm, h)

    def dram(s, name, shape, dt=F32, kind="Internal", nsub=None):
        h = s.nc.dram_tensor(name, list(shape), dt, kind=kind).ap()
        return Buf(name, h, nsub)

    def _need(s, eng, tok, waits):
        if tok is None:
            return
        sk, v = tok
        if sk == ("e", eng) and (eng == "tensor" or NO_SAME_ENGINE_SYNC):
            return
        if s.seen[eng].get(sk, 0) >= v:
            return
        s.seen[eng][sk] = v
        waits.append((sk, v))

    def _deps(s, eng, reads, writes):
        waits = []
        for k in reads:
            s._need(eng, s.last_w.get(k), waits)
        for k in writes:
            s._need(eng, s.last_w.get(k), waits)
            for t in s.readers.get(k, ()):
                s._need(eng, t, waits)
        return waits

    def _commit(s, tok, reads, writes):
        for k in reads:
            s.readers.setdefault(k, []).append(tok)
        for k in writes:
            s.last_w[k] = tok
            s.readers[k] = []

    def op(s, eng, fn, reads=(), writes=()):
        if s.defer is not None:
            s.defer.append(("op", eng, fn, list(reads), list(writes)))
            return None
        waits = s._deps(eng, reads, writes)
        s.cnt[eng] += 1
        tok = (("e", eng), s.cnt[eng])
        s.ops[eng].append((waits, fn, tok))
        s._commit(tok, reads, writes)
        return tok

    def _dma(s, eng, fn, reads=(), writes=()):
        if s.defer is not None:
            s.defer.append(("dma", eng, fn, list(reads), list(writes)))
            return None
        waits = s._deps(eng, reads, writes)
        slot = s.dma_rr[eng] % N_DMA_SEMS
        s.dma_rr[eng] += 1
        sk = ("d", eng, slot)
        prev = s.dma_tgt.get(sk, 0)
        if prev:
            s._need(eng, (sk, prev), waits)
        tgt = prev + 16
        s.dma_tgt[sk] = tgt
        tok = (sk, tgt)
        s.ops[eng].append((waits, fn, tok))
        s._commit(tok, reads, writes)
        return tok

    def replay(s, A, B=None):
        assert s.defer is None
        B = B or []
        ia = ib = 0
        na, nb = len(A), len(B)
        while ia < na or ib < nb:
            if ib >= nb or (ia < na and ia * nb <= ib * na):
                k, eng, fn, r, w = A[ia]; ia += 1
            else:
                k, eng, fn, r, w = B[ib]; ib += 1
            if k == "op":
                s.op(eng, fn, r, w)
            else:
                s._dma(eng, fn, r, w)

    def barrier(s):
        toks = [(("e", e), s.cnt[e]) for e in ENGS if s.cnt[e]]
        toks += [(sk, v) for sk, v in s.dma_tgt.items()]
        for e in ENGS:
            waits = []
            for t in toks:
                s._need(e, t, waits)
            if waits:
                s.ops[e].append((waits, None, None))
        s.last_w.clear()
        s.readers.clear()

    def emit(s):
        nc = s.nc
        with contextlib.ExitStack() as st:
            semh = {}
            for e in ENGS:
                semh[("e", e)] = st.enter_context(nc.semaphore(f"s_{e}"))
            for e in ENGS:
                for i in range(min(N_DMA_SEMS, s.dma_rr[e])):
                    semh[("d", e, i)] = st.enter_context(nc.semaphore(f"d_{e}_{i}"))
            block = st.enter_context(nc.Block())
            fin = []
            for t in s.final_tokens:
                s._need("sync", t, fin)
            for e in ENGS:
                ops = s.ops[e]
                extra = fin if e == "sync" else []
                if not ops and not extra:
                    continue

                def body(engine, ops=ops, extra=extra):
                    for waits, fn, tok in ops:
                        for sk, v in waits:
                            engine.wait_ge(semh[sk], v)
                        if fn is None:
                            continue
                        ins = fn(engine)
                        sk, v = tok
                        ins.then_inc(semh[sk], 16 if sk[0] == "d" else 1)
                    for sk, v in extra:
                        engine.wait_ge(semh[sk], v)
                getattr(block, e)(body)

    @staticmethod
    def _k(*vs):
        ks = []
        for v in vs:
            if isinstance(v, View):
                ks += v.keys
        return ks

    @staticmethod
    def _a(v):
        return v.ap if isinstance(v, View) else v

    def mm(s, out, lhsT, rhs, start=True, stop=True):
        return s.op("tensor", lambda e: e.matmul(out.ap, lhsT.ap, rhs.ap, start=start, stop=stop),
                    reads=s._k(lhsT, rhs), writes=s._k(out))

    def tr(s, out, in_, ident):
        return s.op("tensor", lambda e: e.transpose(out.ap, in_.ap, ident.ap),
                    reads=s._k(in_, ident), writes=s._k(out))

    def act(s, out, in_, func, bias=None, scale=None, accum=None):
        kw = {}
        if bias is not None:
            kw["bias"] = s._a(bias)
        if scale is not None:
            kw["scale"] = s._a(scale)
        if accum is not None:
            kw["accum_out"] = accum.ap
        return s.op("scalar", lambda e: e.activation(out.ap, in_.ap, func, **kw),
                    reads=s._k(in_, bias, scale), writes=s._k(out, accum))

    def tt(s, out, a, b, op, eng="vector"):
        return s.op(eng, lambda e: e.tensor_tensor(out.ap, a.ap, b.ap, op),
                    reads=s._k(a, b), writes=s._k(out))

    def ts(s, out, a, s1, op0, s2=None, op1=None, eng="vector", accum=None):
        kw = {}
        if op1 is not None:
            kw["op1"] = op1
        if accum is not None:
            kw["accum_out"] = accum.ap
        return s.op(eng, lambda e: e.tensor_scalar(out.ap, a.ap, s._a(s1), s._a(s2), op0, **kw),
                    reads=s._k(a, s1, s2), writes=s._k(out, accum))

    def stt(s, out, a, sc, b, op0, op1):
        return s.op("vector", lambda e: e.scalar_tensor_tensor(out.ap, a.ap, s._a(sc), b.ap, op0, op1),
                    reads=s._k(a, sc, b), writes=s._k(out))

    def cp(s, out, in_, eng="vector"):
        if eng == "scalar":
            return s.op("scalar", lambda e: e.copy(out.ap, in_.ap), reads=s._k(in_), writes=s._k(out))
        return s.op(eng, lambda e: e.tensor_copy(out.ap, in_.ap), reads=s._k(in_), writes=s._k(out))

    def memset(s, out, val, eng="vector"):
        return s.op(eng, lambda e: e.memset(out.ap, val), writes=s._k(out))

    def red(s, out, in_, op, axis=AX.X):
        return s.op("vector", lambda e: e.tensor_reduce(out.ap, in_.ap, axis, op),
                    reads=s._k(in_), writes=s._k(out))

    def recip(s, out, in_):
        return s.op("vector", lambda e: e.reciprocal(out.ap, in_.ap), reads=s._k(in_), writes=s._k(out))

    def scan(s, out, d0, d1, initial, op0, op1):
        return s.op("vector", lambda e: e.tensor_tensor_scan(out.ap, d0.ap, d1.ap, s._a(initial), op0, op1),
                    reads=s._k(d0, d1, initial), writes=s._k(out))

    def dma(s, out, in_, eng="sync"):
        return s._dma(eng, lambda e: e.dma_start(out=out.ap, in_=in_.ap), reads=s._k(in_), writes=s._k(out))


def make_consts():
    c = {}
    c["ident"] = np.eye(128, dtype=np.float32)
    p = np.arange(128)[:, None]
    f = np.arange(128)[None, :]
    c["m_plt"] = (p < f).astype(np.float32)
    c["m_ple"] = (p <= f).astype(np.float32)
    c["m_pgt"] = (p > f).astype(np.float32)
    c["m_pge"] = (p >= f).astype(np.float32)
    bo = np.zeros((128, 128), np.float32)
    bo[:64, :64] = 1
    bo[64:, 64:] = 1
    c["blockones"] = bo
    c["ones"] = np.ones((128, 128), np.float32)
    return np.stack([c[k] for k in ("ident", "m_plt", "m_ple", "m_pgt", "m_pge", "blockones", "ones")], 0)


CONST_NAMES = ("ident", "m_plt", "m_ple", "m_pgt", "m_pge", "blockones", "ones")

W_SPECS = {
    "w_ada": [DEPTH, D, 6 * D], "b_ada": [DEPTH, 6 * D], "norm_mix_g": [DEPTH, D], "norm_ffn_g": [DEPTH, D],
    "w_in": [DEPTH, D, IN_DIM], "rwkv_mu": [DEPTH, 2, RWKV_IN], "rwkv_w0": [DEPTH, 2, 1024],
    "rwkv_w2": [DEPTH, 2, 96, 1024], "rwkv_a0": [DEPTH, 2, 1024], "rwkv_a2": [DEPTH, 2, 96, 1024],
    "rwkv_g2": [DEPTH, 256, 1024], "rwkv_k_k": [DEPTH, 1024], "rwkv_k_a": [DEPTH, 1024],
    "rwkv_r_k": [DEPTH, 16, 64], "rwkv_ln_g": [DEPTH, 1024], "rwkv_ln_b": [DEPTH, 1024],
    "gla_conv": [DEPTH, 3, 2048], "gla_alpha_w2": [DEPTH, 2, 16, 512], "gla_alpha_b": [DEPTH, 2, 512],
    "gla_norm_g": [DEPTH, 256], "w_branch_a": [DEPTH, 1024, D], "w_branch_b": [DEPTH, 1024, D],
    "w_out": [DEPTH, D, D], "router_w": [D, NE], "router_b": [1, NE],
    "exp_w_gate": [DEPTH, NE, D, DE], "exp_w_up": [DEPTH, NE, D, DE], "exp_w_down": [DEPTH, NE, DE, D],
    "final_norm_g": [D],
}


class K:
    def __init__(s, nc, stop_after=None, dbg=False):
        s.nc = nc
        s.P = Prog(nc)
        s.stop_after = stop_after
        s.skip_rwkv = False
        s.dbg = dbg
        P = s.P
        s.inp = {}
        s.inp["x"] = P.dram("x", [2048, D], kind="ExternalInput")
        s.inp["ctx"] = P.dram("ctx", [CTX, D], kind="ExternalInput")
        s.inp["c"] = P.dram("c", [1, D], kind="ExternalInput")
        s.inp["c_ctx"] = P.dram("c_ctx", [1, D], kind="ExternalInput")
        s.inp["consts"] = P.dram("consts", [len(CONST_NAMES), 128, 128], kind="ExternalInput")
        for k, shp in W_SPECS.items():
            s.inp[k] = P.dram(k, shp, kind="ExternalInput")
        s.out = P.dram("out", [2048, D], kind="ExternalOutput")
        kd = "ExternalOutput" if dbg else "Internal"
        s.pT = P.dram("pT", [IN_DIM, T], kind=kd)
        s.xmid = P.dram("xmid", [T, D], kind=kd)
        s.xcur = P.dram("xcur", [T, D], kind=kd)
        s.yaT = P.dram("yaT", [1024, T], BF16, kind=kd)
        s.ybT = P.dram("ybT", [1024, T], BF16, kind=kd)
        s.moddbg = P.dram("moddbg", [128, 256], kind=kd)
        s.modrow = P.dram("modrow", [2, 6 * D], kind=kd)
        s.inp["sel16"] = P.dram("sel16", [16, 16, 128], kind="ExternalInput")

        s.C = {}
        for i, nm in enumerate(CONST_NAMES):
            b = P.sb("c_" + nm, [128, 128])
            P.dma(b[:], s.inp["consts"][i])
            s.C[nm] = b
        s.psb = [P.ps(f"bank{i}", [128, 512]) for i in range(8)]
        s.psi = 0
        s.pool = None

    def bank(s):
        if s.pool is not None:
            lst, st_ = s.pool
            b = s.psb[lst[st_[0] % len(lst)]]
            st_[0] += 1
            return b
        b = s.psb[s.psi % 8]
        s.psi += 1
        return b

    def transpose_pack(s, rows_buf, nrows, out_cols, stack=None):
        P = s.P
        b = s.bank()
        P.tr(b[:, 0:nrows], rows_buf[0:nrows, :], s.C["ident"][0:nrows, 0:nrows])
        P.cp(out_cols[:, 0:nrows], b[:, 0:nrows])

    def stage_prologue(s):
        P = s.P
        crow = P.sb("crow", [32, 128])
        P.dma(crow[0:16, :], s.inp["c"][0].re("(c p) -> c p", p=128))
        P.dma(crow[16:32, :], s.inp["c_ctx"][0].re("(c p) -> c p", p=128))
        P.act(crow[:], crow[:], AF.Silu)
        s.cT = P.sb("cT", [128, 32])
        s.transpose_pack(crow, 32, s.cT)

    def stage_mod(s, l):
        P = s.P
        with contextlib.ExitStack() as st:
            packA = P.sb("packA", [128, 128], stack=st)
            P.dma(packA[0:96, :], s.inp["b_ada"][l].re("(c p) -> c p", p=128))
            P.dma(packA[96:112, :], s.inp["norm_mix_g"][l].re("(c p) -> c p", p=128))
            P.dma(packA[112:128, :], s.inp["norm_ffn_g"][l].re("(c p) -> c p", p=128))
            colA = s.colA
            s.transpose_pack(packA, 128, colA)
            wst = [P.sb(f"wada{i}", [128, 16, 512], stack=st) for i in range(2)]
            acc = s.bank()
            for blk in range(24):
                w = wst[blk % 2]
                src = s.inp["w_ada"][l].re("(kc p) n -> p kc n", p=128)[:, :, blk * 512:(blk + 1) * 512]
                P.dma(w[:], src)
                for j in range(4):
                    dc = blk * 4 + j
                    for kc in range(16):
                        rhs = s.cT[:].re("p (w k) -> p w k", w=2)[:, :, kc]
                        P.mm(acc[:, dc * 2:dc * 2 + 2], w[:, kc, j * 128:(j + 1) * 128], rhs,
                             start=(kc == 0), stop=(kc == 15))
            mod = s.mod
            accv = acc[:, 0:192].re("p (c w) -> p c w", w=2)
            for w_ in range(2):
                P.tt(mod[:, :, w_], accv[:, :, w_], colA[:, 0:96], ALU.add)
            for w_ in range(2):
                P.stt(s.m1[:, :, w_], mod[:, 16:32, w_], 1.0, colA[:, 96:112], ALU.add, ALU.mult)
                P.stt(s.m2[:, :, w_], mod[:, 64:80, w_], 1.0, colA[:, 112:128], ALU.add, ALU.mult)
            if s.dbg:
                P.dma(s.moddbg[:, 0:192], mod[:].re("p c w -> p (c w)"))
            mrow = P.sb("mrow", [128, 128], stack=st)
            mtmp = P.sb("mtmp", [128, 96], stack=st)
            for w_ in range(2):
                P.cp(mtmp[:], mod[:, :, w_])
                bb = s.bank()
                P.tr(bb[0:96, 0:128], mtmp[:, 0:96], s.C["ident"][:])
                P.cp(mrow[0:96, :], bb[0:96, 0:128])
                P.dma(s.modrow[w_].re("(c p) -> c p", p=128), mrow[0:96, :])
        P.barrier()

    def x_src(s, l, tt_):
        if l == 0:
            if tt_ < 2:
                return s.inp["ctx"][tt_ * 128:(tt_ + 1) * 128, :]
            return s.inp["x"][(tt_ - 2) * 128:(tt_ - 1) * 128, :]
        return s.xcur[tt_ * 128:(tt_ + 1) * 128, :]

    def norm_to_hT(s, src_view, hT, tt_, mcol, shcol, w_, xbufs, st_small, hT32=None, sh_off=0):
        P = s.P
        xt = xbufs[tt_ % 2]
        P.dma(xt[:], src_view)
        junk, ss, rstd = st_small
        P.act(junk[:], xt[:], AF.Square, accum=ss[:, 0:1])
        P.ts(ss[:, 0:1], ss[:, 0:1], 1.0 / D, ALU.mult, EPS, ALU.add)
        P.act(ss[:, 0:1], ss[:, 0:1], AF.Sqrt)
        P.recip(rstd[:, 0:1], ss[:, 0:1])
        P.ts(xt[:], xt[:], rstd[:, 0:1], ALU.mult)
        for g in range(4):
            b = s.bank()
            for j in range(4):
                kc = g * 4 + j
                P.tr(b[:, j * 128:(j + 1) * 128], xt[:, kc * 128:(kc + 1) * 128], s.C["ident"][:])
            for j in range(4):
                kc = g * 4 + j
                P.act(hT[:, kc, tt_ * 128:(tt_ + 1) * 128], b[:, j * 128:(j + 1) * 128], AF.Identity,
                      bias=shcol[:, sh_off + kc, w_:w_ + 1], scale=mcol[:, kc, w_:w_ + 1])
                if hT32 is not None:
                    P.act(hT32[:, kc, :], b[:, j * 128:(j + 1) * 128], AF.Identity,
                          bias=shcol[:, sh_off + kc, w_:w_ + 1], scale=mcol[:, kc, w_:w_ + 1])

    def tok_blocks(s, cm):
        blks = [(0, 256, lambda v: v[:, 0:256])]
        for j in range(4):
            if not cm:
                blks.append((256 + 512 * j, 512, lambda v, j=j: v[:, 256 + 512 * j:256 + 512 * (j + 1)]))
            else:
                blks.append((256 + 512 * j, 512,
                             lambda v, j=j: v[:, 256:T].re("p (r c) -> p c r", c=64)[:, 16 * j:16 * (j + 1), :]))
        return blks

    def stage_proj(s, l):
        P = s.P
        with contextlib.ExitStack() as st:
            hT = P.sb("hT", [128, 16, T], BF16, stack=st)
            xbufs = [P.sb(f"xin{i}", [128, D], stack=st) for i in range(2)]
            small = (P.sb("junk", [128, D], stack=st), P.sb("ss", [128, 1], stack=st), P.sb("rstd", [128, 1], stack=st))
            for tt_ in range(NT):
                w_ = 1 if tt_ < 2 else 0
                s.norm_to_hT(s.x_src(l, tt_), hT, tt_, s.m1, s.mod, w_, xbufs, small)
            cbs = [(i * 128, 128) for i in range(24)] + [(3072, 96), (3168, 96), (3264, 128), (3392, 128)]
            c0 = GLA_OFF
            cbs += [(c0 + i * 128, 128) for i in range(16)] + [(c0 + 2048, 16)]
            cbs += [(GLA_GATE_OFF + i * 128, 128) for i in range(8)]
            cbs += [(BR_GATE_OFF + i * 128, 128) for i in range(32)]
            wst = [P.sb(f"wst{i}", [128, 16, 128], stack=st) for i in range(3)]
            wbf = [P.sb(f"wbf{i}", [128, 16, 128], BF16, stack=st) for i in range(3)]
            ob = [P.sb(f"ob{i}", [128, T], stack=st) for i in range(3)]
            win = s.inp["w_in"][l].re("(kc p) n -> p kc n", p=128)
            for bi, (c0, w) in enumerate(cbs):
                cm = GLA_OFF <= c0 < BR_GATE_OFF
                ws, wb, o = wst[bi % 3], wbf[bi % 3], ob[bi % 3]
                P.dma(ws[:, :, 0:w], win[:, :, c0:c0 + w])
                P.cp(wb[:, :, 0:w], ws[:, :, 0:w], eng="gpsimd")
                for ti, (t0, n, fn) in enumerate(s.tok_blocks(cm)):
                    b = s.bank()
                    for kc in range(16):
                        P.mm(b[0:w, 0:n], wb[:, kc, 0:w], fn(hT[:, kc, :]),
                             start=(kc == 0), stop=(kc == 15))
                    if ti % 2 == 0:
                        P.cp(o[0:w, t0:t0 + n], b[0:w, 0:n], eng="scalar")
                    else:
                        P.cp(o[0:w, t0:t0 + n], b[0:w, 0:n], eng="vector")
                P.dma(s.pT[c0:c0 + w, :], o[0:w, :])
        P.barrier()


    def stage_rwkv(s, l):
        P = s.P
        C = s.C
        with contextlib.ExitStack() as st:
            sb = lambda n, shp, dt=F32: P.sb(n, shp, dt, stack=st)
            packB = sb("packB", [128, 128]); colB = sb("colB", [128, 128])
            P.memset(packB[:], 0.0)
            mu = s.inp["rwkv_mu"][l]
            for i in range(2):
                o = 28 * i
                P.dma(packB[o:o + 24, :], mu[i, 0:3072].re("(c p) -> c p", p=128))
                P.dma(packB[o + 24:o + 25, 0:96], mu[i:i + 1, 3072:3168])
                P.dma(packB[o + 25:o + 26, 0:96], mu[i:i + 1, 3168:3264])
                P.dma(packB[o + 26:o + 28, :], mu[i, 3264:3520].re("(c p) -> c p", p=128))
            P.dma(packB[56:72, :], s.inp["rwkv_w0"][l].re("d (c p) -> (d c) p", p=128))
            P.dma(packB[72:88, :], s.inp["rwkv_a0"][l].re("d (c p) -> (d c) p", p=128))
            P.dma(packB[88:96, :], s.inp["rwkv_k_k"][l].re("(c p) -> c p", p=128))
            P.dma(packB[96:104, :], s.inp["rwkv_k_a"][l].re("(c p) -> c p", p=128))
            P.dma(packB[104:112, :], s.inp["rwkv_r_k"][l].re("(c two) k -> c (two k)", two=2))
            P.dma(packB[112:120, :], s.inp["rwkv_ln_g"][l].re("(c p) -> c p", p=128))
            P.dma(packB[120:128, :], s.inp["rwkv_ln_b"][l].re("(c p) -> c p", p=128))
            s.transpose_pack(packB, 128, colB)
            col = lambda i: colB[:, i:i + 1]
            cf = sb("cf", [128, 28])
            P.tt(cf[:], colB[:, 0:28], colB[:, 28:56], ALU.add)
            P.ts(cf[:], cf[:], -1.0, ALU.mult, 1.0, ALU.add)

            def shift_rows(dst, X, blk, n=128):
                P.ts(dst, X, cf[0:n, blk:blk + 1], ALU.mult)
                for (a, b) in ((0, 256), (256, T)):
                    P.stt(dst[:, a + 1:b], X[:, a:b - 1], colB[0:n, blk:blk + 1], dst[:, a + 1:b], ALU.mult, ALU.add)
                    P.stt(dst[:, a:b - 1], X[:, a + 1:b], colB[0:n, 28 + blk:29 + blk], dst[:, a:b - 1], ALU.mult, ALU.add)

            xb = [sb("xb0", [128, T])] * 2
            xbi = [0]

            def load_rows(row0, n):
                X = xb[xbi[0] % 2]
                xbi[0] += 1
                P.dma(X[0:n, :], s.pT[row0:row0 + n, :])
                return X
            txw = sb("txw", [128, T]); xaS = sb("xaS", [128, T]); sxg = sb("sxg", [128, 2, T], BF16)
            for j in range(2):
                X = load_rows(3264 + 128 * j, 128); shift_rows(txw[:, :], X[:, :], 26 + j)
                P.act(sxg[:, j, :], txw[:, :], AF.Sigmoid)
            X = load_rows(3072, 96); shift_rows(txw[0:96, :], X[0:96, :], 24, 96)
            P.act(txw[0:96, :], txw[0:96, :], AF.Tanh)
            X = load_rows(3168, 96); shift_rows(xaS[0:96, :], X[0:96, :], 25, 96)
            r_ = sb("r", [128, T]); k_ = sb("k", [128, T]); v_ = sb("v", [128, T]); kk_ = sb("kk", [128, T])
            vT = sb("vT", [128, NT, 128]); yacc = sb("yacc", [128, NT, 128]); yo = sb("yo", [128, T], BF16)
            w2c = sb("w2c", [96, 2, 128]); a2c = sb("a2c", [96, 2, 128]); g2c = sb("g2c", [128, 2, 128], BF16); g2f = sb("g2f", [128, 2, 128])
            RKb = sb("RKb", [128, 128])
            PhiT = sb("PhiT", [128, 128]); P.memset(PhiT[:], 0.0)
            Sb = [sb(f"S{i}", [128, 64]) for i in range(2)]
            G_ = 2
            NS = 4
            tn = ["lw", "a", "t1", "kd", "b", "cs", "EA", "EB"]
            tmp = [{n: sb(f"{n}{i}", [128, 128]) for n in tn} for i in range(NS)]
            tb = [{n: sb(f"{n}{i}", [128, 128]) for n in ("bh", "kh", "bend", "kend")} for i in range(NS)]
            KR = [sb(f"KR{i}", [128, 256]) for i in range(NS)]
            TMs = [sb(f"TMs{i}", [128, 384]) for i in range(NS)]
            gC = [sb(f"gC{i}", [128, 1]) for i in range(NS)]
            totb = [sb(f"totb{i}", [128, 1]) for i in range(NS)]
            Php = [sb(f"Php{i}", [128, 128]) for i in range(NS)]
            Wn = [sb(f"Wn{i}", [128, 128]) for i in range(NS)]
            QT = [sb(f"QT{i}", [128, 128]) for i in range(NS)]
            Dl = [sb(f"Dl{i}", [128, 64]) for i in range(NS)]
            hn = ["ArbT", "MkT", "ArkT", "TT0", "TT1", "G"]
            htmp = [[{n: sb(f"{n}{i}{h}", [128, 128] if n != "G" else [128, 64]) for n in hn} for h in range(2)] for i in range(NS)]
            XX = [[[sb(f"XX{i}{h}{j}", [128, 256]) for j in range(2)] for h in range(2)] for i in range(NS)]
            ep = {n: sb(n, [128, 128]) for n in ("ysum", "cen", "junk", "yaff", "rk", "bon")}
            mean = sb("mean", [128, 2]); var = sb("var", [128, 2])
            ident = C["ident"]
            pool14 = ([0, 1, 2, 3], [0]); pool5 = ([4, 5, 6, 7], [0])

            for hp in range(8):
                hc = slice(hp * 128, (hp + 1) * 128)
                P.dma(w2c[:], s.inp["rwkv_w2"][l].re("d k n -> k d n")[:, :, hc])
                P.dma(a2c[:], s.inp["rwkv_a2"][l].re("d k n -> k d n")[:, :, hc])
                P.dma(g2f[:], s.inp["rwkv_g2"][l].re("(kc p) n -> p kc n", p=128)[:, :, hc])
                P.cp(g2c[:], g2f[:], eng="gpsimd")
                X = load_rows(hp * 128, 128); shift_rows(r_[:, :], X[:, :], hp)
                X = load_rows(1024 + hp * 128, 128); shift_rows(k_[:, :], X[:, :], 8 + hp)
                X = load_rows(2048 + hp * 128, 128); shift_rows(v_[:, :], X[:, :], 16 + hp)
                kkr, sq = xb[0], kk_
                P.ts(kkr[:, :], k_[:, :], col(88 + hp), ALU.mult)
                P.tt(sq[:, :], kkr[:, :], kkr[:, :], ALU.mult)
                for t0 in range(0, T, 512):
                    n = min(512, T - t0)
                    b = s.bank()
                    P.mm(b[:, 0:n], C["blockones"][:], sq[:, t0:t0 + n])
                    P.act(sq[:, t0:t0 + n], b[:, 0:n], AF.Sqrt)
                P.ts(sq[:, :], sq[:, :], 1e-12, ALU.max)
                P.recip(sq[:, :], sq[:, :])
                P.tt(kk_[:, :], kkr[:, :], sq[:, :], ALU.mult)
                for c0 in range(0, NT, 4):
                    nn = min(4, NT - c0)
                    b = s.bank()
                    for j in range(nn):
                        P.tr(b[:, j * 128:(j + 1) * 128], v_[:, (c0 + j) * 128:(c0 + j + 1) * 128], ident[:])
                    P.cp(vT[:, c0:c0 + nn, :], b[:, 0:nn * 128].re("p (c f) -> p c f", f=128), eng="scalar")
                P.ts(RKb[:], C["blockones"][:], col(104 + hp), ALU.mult)
                ci = 0
                for d in range(2):
                    order = list(range(NT)) if d == 0 else [1, 0] + list(range(17, 1, -1))
                    mS, mI, mSt = (C["m_plt"], C["m_ple"], C["m_pgt"]) if d == 0 else (C["m_pgt"], C["m_pge"], C["m_plt"])
                    Scur = 0
                    P.memset(Sb[0][:], 0.0)
                    prev5 = None
                    for gi in range(0, NT, G_):
                        grp = [(c, (ci + j) % NS) for j, c in enumerate(order[gi:gi + G_])]
                        ci += len(grp)
                        L14 = []; P.defer = L14
                        s.pool = pool14
                        for (c, i) in grp:
                            t = tmp[i]; u = tb[i]; kr = KR[i]
                            tok = slice(c * 128, (c + 1) * 128)
                            zb = s.bank()
                            P.mm(zb[:, 0:128], w2c[0:96, d, :], txw[0:96, tok])
                            P.mm(zb[:, 128:256], a2c[0:96, d, :], xaS[0:96, tok])
                            P.act(t["lw"][:], zb[:, 0:128], AF.Sigmoid, bias=col(56 + 8 * d + hp))
                            P.ts(t["lw"][:], t["lw"][:], -0.6065306597, ALU.mult)
                            P.act(t["a"][:], zb[:, 128:256], AF.Sigmoid, bias=col(72 + 8 * d + hp))
                            P.ts(t["t1"][:], t["a"][:], -1.0, ALU.add, col(96 + hp), ALU.mult)
                            P.stt(t["kd"][:], t["t1"][:], 1.0, k_[:, tok], ALU.add, ALU.mult)
                            P.tt(t["b"][:], kk_[:, tok], t["a"][:], ALU.mult, eng="gpsimd")
                            P.scan(t["cs"][:], C["ones"][:], t["lw"][:], 0.0, ALU.mult, ALU.add)
                            P.cp(totb[i][:], t["cs"][:, 127:128], eng="gpsimd")
                            tot = totb[i][:, 0:1]
                            lg = t["cs"]
                            if d == 1:
                                P.ts(lg[:], t["cs"][:], -1.0, ALU.mult, tot, ALU.add)
                                P.tt(lg[:], lg[:], t["lw"][:], ALU.add)
                            P.act(t["EA"][:], lg[:], AF.Exp)
                            P.tt(kr[:, 128:256], r_[:, tok], t["EA"][:], ALU.mult)
                            P.tt(t["t1"][:], lg[:], t["lw"][:], ALU.subtract, eng="gpsimd")
                            P.act(t["EA"][:], t["t1"][:], AF.Exp)
                            P.tt(kr[:, 0:128], kk_[:, tok], t["EA"][:], ALU.mult)
                            P.act(t["EB"][:], lg[:], AF.Exp, scale=-1.0)
                            P.tt(u["bh"][:], t["b"][:], t["EB"][:], ALU.mult)
                            P.tt(u["kh"][:], t["kd"][:], t["EB"][:], ALU.mult, eng="gpsimd")
                            P.act(t["EB"][:], lg[:], AF.Exp, bias=tot, scale=-1.0)
                            P.tt(u["bend"][:], t["b"][:], t["EB"][:], ALU.mult, eng="gpsimd")
                            P.tt(u["kend"][:], t["kd"][:], t["EB"][:], ALU.mult)
                            P.act(gC[i][:], tot, AF.Exp)
                            bt = s.bank()
                            P.tr(bt[:, 0:128], kr[:, 0:128], ident[:])
                            P.tr(bt[:, 128:256], u["bend"][:], ident[:])
                            P.tr(bt[:, 256:384], u["kend"][:], ident[:])
                            P.cp(TMs[i][:], bt[:, 0:384], eng="scalar")
                        chains = [(c, i, h) for (c, i) in grp for h in range(2)]
                        for (c, i, h) in chains:
                            hs = slice(h * 64, h * 64 + 64)
                            u = tb[i]; kr = KR[i]; ht = htmp[i][h]; xx = XX[i][h]
                            b1 = s.bank(); P.mm(b1[:, 0:256], u["bh"][hs, :], kr[hs, :])
                            b2 = s.bank(); P.mm(b2[:, 0:256], u["kh"][hs, :], kr[hs, :])
                            P.mm(b2[:, 256:384], kr[hs, 0:128], u["bh"][hs, :])
                            P.tt(xx[0][:, 128:256], b1[:, 0:128], mS[:], ALU.mult)
                            P.tt(ht["ArbT"][:], b1[:, 128:256], mI[:], ALU.mult)
                            P.tt(ht["MkT"][:], b2[:, 0:128], mS[:], ALU.mult)
                            P.tt(ht["ArkT"][:], b2[:, 128:256], mI[:], ALU.mult)
                            P.tt(xx[0][:, 0:128], b2[:, 256:384], mSt[:], ALU.mult)
                            P.tt(ht["TT0"][:], ident[:], xx[0][:, 128:256], ALU.subtract, eng="gpsimd")
                        TTc = {(c, h): "TT0" for (c, i, h) in chains}
                        for lv in range(6):
                            bxs = {}
                            for (c, i, h) in chains:
                                xc = XX[i][h][lv % 2]
                                bx = s.bank(); bxs[(c, h)] = bx
                                P.mm(bx[:, 0:128], xc[:, 128:256], xc[:, 0:128])
                            for (c, i, h) in chains:
                                xn = XX[i][h][(lv + 1) % 2]
                                P.cp(xn[:, 0:128], bxs[(c, h)][:, 0:128], eng="scalar")
                            bqs = {}
                            for (c, i, h) in chains:
                                xn = XX[i][h][(lv + 1) % 2]
                                bq = s.bank(); bqs[(c, h)] = bq
                                P.mm(bq[:, 0:128], xn[:, 0:128], htmp[i][h][TTc[(c, h)]][:])
                                if lv < 5:
                                    P.tr(bq[:, 128:256], xn[:, 0:128], ident[:])
                            for (c, i, h) in chains:
                                cur = TTc[(c, h)]
                                nxt = "TT1" if cur == "TT0" else "TT0"
                                P.tt(htmp[i][h][nxt][:], bqs[(c, h)][:, 0:128], htmp[i][h][cur][:], ALU.add)
                                if lv < 5:
                                    P.cp(XX[i][h][(lv + 1) % 2][:, 128:256], bqs[(c, h)][:, 128:256])
                                TTc[(c, h)] = nxt
                        for (c, i, h) in chains:
                            hs = slice(h * 64, h * 64 + 64)
                            ht = htmp[i][h]
                            TTf = ht[TTc[(c, h)]]
                            bg = s.bank()
                            P.mm(bg[:, 0:64], ht["MkT"][:], vT[:, c, hs])
                            P.cp(ht["G"][:, 0:64], bg[:, 0:64], eng="scalar")
                            bp = s.bank()
                            P.mm(bp[:, 0:64], TTf[:], TMs[i][:, h * 64:h * 64 + 64])
                            P.mm(bp[:, 64:128], TTf[:], ht["G"][:, 0:64])
                            P.cp(Php[i][:, hs], bp[:, 0:64], eng="scalar")
                            P.act(Wn[i][:, hs], bp[:, 64:128], AF.Copy, scale=-1.0)
                        L5 = []; P.defer = L5
                        s.pool = pool5
                        for (c, i) in grp:
                            t = tmp[i]; tm = TMs[i]
                            tok = slice(c * 128, (c + 1) * 128)
                            BendT, KendT = tm[:, 128:256], tm[:, 256:384]
                            Sc = Sb[Scur]; Sn = Sb[1 - Scur]
                            for h in range(2):
                                hs = slice(h * 64, h * 64 + 64)
                                bq = s.bank()
                                P.mm(bq[:, 0:128], Php[i][:], htmp[i][h]["ArbT"][:])
                                P.tt(QT[i][hs, :], KR[i][hs, 128:256], bq[hs, 0:128], ALU.subtract)
                            by = s.bank()
                            for h in range(2):
                                hs = slice(h * 64, h * 64 + 64)
                                P.mm(by[:, hs], htmp[i][h]["ArkT"][:], vT[:, c, hs], start=True, stop=False)
                                P.mm(by[:, hs], htmp[i][h]["ArbT"][:], Wn[i][:, hs], start=False, stop=False)
                                P.mm(by[:, hs], QT[i][hs, :], Sc[hs, :], start=False, stop=True)
                            bf = s.bank(); P.mm(bf[:, 0:128], Php[i][:], BendT)
                            bd = s.bank()
                            P.mm(bd[:, 0:128], KendT, vT[:, c, :], start=True, stop=False)
                            P.mm(bd[:, 0:128], BendT, Wn[i][:], start=False, stop=True)
                            for h in range(2):
                                hs = slice(h * 64, h * 64 + 64)
                                P.stt(PhiT[hs, hs], ident[hs, hs], gC[i][hs, 0:1], bf[hs, hs], ALU.mult, ALU.subtract)
                                P.cp(Dl[i][hs, :], bd[hs, h * 64:h * 64 + 64], eng="scalar")
                            bs = s.bank(); P.mm(bs[:, 0:64], PhiT[:], Sc[:])
                            P.tt(Sn[:], bs[:, 0:64], Dl[i][:], ALU.add)
                            Scur = 1 - Scur
                            if d == 0:
                                P.cp(yacc[:, c, :], by[:, 0:128], eng="scalar")
                                continue
                            ys = ep["ysum"]
                            P.tt(ys[:], by[:, 0:128], yacc[:, c, :], ALU.add)
                            P.red(mean[:], ys[:].re("p (h v) -> p h v", h=2), ALU.add)
                            P.ts(mean[:], mean[:], 1.0 / 64, ALU.mult)
                            for h in range(2):
                                hs = slice(h * 64, h * 64 + 64)
                                P.ts(ep["cen"][:, hs], ys[:, hs], mean[:, h:h + 1], ALU.subtract)
                                P.act(ep["junk"][:, hs], ep["cen"][:, hs], AF.Square, accum=var[:, h:h + 1])
                            P.ts(var[:], var[:], 1.0 / 64, ALU.mult, 64e-5, ALU.add)
                            P.act(var[:], var[:], AF.Sqrt)
                            P.recip(var[:], var[:])
                            for h in range(2):
                                hs = slice(h * 64, h * 64 + 64)
                                P.ts(ep["cen"][:, hs], ep["cen"][:, hs], var[:, h:h + 1], ALU.mult)
                            be = s.bank()
                            P.tr(be[:, 0:128], ep["cen"][:], ident[:])
                            P.act(ep["yaff"][:], be[:, 0:128], AF.Identity, bias=col(120 + hp), scale=col(112 + hp))
                            P.tt(ep["rk"][:], r_[:, tok], k_[:, tok], ALU.mult, eng="gpsimd")
                            bb = s.bank()
                            P.mm(bb[:, 0:128], RKb[:], ep["rk"][:])
                            P.mm(bb[:, 128:256], g2c[:, 0, :], sxg[:, 0, tok], start=True, stop=False)
                            P.mm(bb[:, 128:256], g2c[:, 1, :], sxg[:, 1, tok], start=False, stop=True)
                            P.tt(ep["bon"][:], bb[:, 0:128], v_[:, tok], ALU.mult)
                            P.tt(ep["yaff"][:], ep["yaff"][:], ep["bon"][:], ALU.add)
                            P.tt(yo[:, tok], ep["yaff"][:], bb[:, 128:256], ALU.mult)
                        P.defer = None
                        s.pool = None
                        if prev5 is None:
                            P.replay(L14)
                        elif SEQ_REPLAY:
                            P.replay(prev5); P.replay(L14)
                        else:
                            P.replay(prev5, L14)
                        prev5 = L5
                    P.replay(prev5)
                P.dma(s.yaT[hc, :], yo[:, :])
        P.barrier()

    def stage_gla(s, l):
        P = s.P
        C = s.C
        ident = C["ident"]
        with contextlib.ExitStack() as st:
            sb = lambda n, shp, dt=F32: P.sb(n, shp, dt, stack=st)
            packC = sb("packC", [128, 128]); colC = sb("colC", [128, 128])
            P.memset(packC[:], 0.0)
            P.dma(packC[0:48, :], s.inp["gla_conv"][l].re("j (c p) -> (j c) p", p=128))
            P.dma(packC[48:56, :], s.inp["gla_alpha_b"][l].re("d (c p) -> (d c) p", p=128))
            P.dma(packC[56:58, :], s.inp["gla_norm_g"][l].re("(c p) -> c p", p=128))
            s.transpose_pack(packC, 64, colC)
            col = lambda i: colC[:, i:i + 1]

            def conv_rows(dst, X, blk):
                P.ts(dst, X, col(16 + blk), ALU.mult)
                for (a, b) in ((0, 256), (256, T)):
                    P.stt(dst[:, a + 1:b], X[:, a:b - 1], col(blk), dst[:, a + 1:b], ALU.mult, ALU.add)
                    P.stt(dst[:, a:b - 1], X[:, a + 1:b], col(32 + blk), dst[:, a:b - 1], ALU.mult, ALU.add)
                P.act(dst, dst, AF.Silu)
            xb = [sb(f"gxb{i}", [128, T]) for i in range(2)]
            xbi = [0]

            def load_rows(row0, n):
                X = xb[xbi[0] % 2]
                xbi[0] += 1
                P.dma(X[0:n, :], s.pT[row0:row0 + n, :])
                return X
            adS = sb("adS", [16, T])
            P.dma(adS[:], s.pT[GLA_OFF + 2048:GLA_OFF + 2064, :])
            q_ = sb("q", [128, T]); k_ = sb("gk", [128, T]); v_ = sb("gv", [128, 2, T]); gt = sb("gt", [128, 2, T])
            vT = sb("gvT", [128, NT, 256]); oaccD = [sb(f"oacc{d}", [128, NT, 256]) for d in range(2)]; yo = sb("gyo", [128, 2, T], BF16)
            aw = sb("aw", [16, 2, 128])
            SbD = [[sb(f"gS{d}{i}", [128, 256]) for i in range(2)] for d in range(2)]
            NS = 4
            tn = ["g", "cs", "gc", "E1", "E2", "E4", "qg", "kg", "kend", "attT", "kendT"]
            tmp = [{n: sb(f"g{n}{i}", [128, 128]) for n in tn} for i in range(NS)]
            gCe = [sb(f"gCe{i}", [128, 1]) for i in range(NS)]
            osumE = [sb(f"osum{i}", [128, 256]) for i in range(2)]; junk = sb("gjunk", [128, 256]); ssqE = [sb(f"ssq{i}", [128, 1]) for i in range(2)]
            for hd in range(4):
                P.dma(aw[:], s.inp["gla_alpha_w2"][l].re("d k n -> k d n")[:, :, hd * 128:(hd + 1) * 128])
                X = load_rows(GLA_OFF + hd * 128, 128); conv_rows(q_[:, :], X[:, :], hd)
                X = load_rows(GLA_OFF + 512 + hd * 128, 128); conv_rows(k_[:, :], X[:, :], 4 + hd)
                for vc in range(2):
                    X = load_rows(GLA_OFF + 1024 + hd * 256 + vc * 128, 128)
                    conv_rows(v_[:, vc, :], X[:, :], 8 + hd * 2 + vc)
                    P.dma(gt[:, vc, :], s.pT[GLA_GATE_OFF + hd * 256 + vc * 128:GLA_GATE_OFF + hd * 256 + (vc + 1) * 128, :])
                    P.act(gt[:, vc, :], gt[:, vc, :], AF.Silu)
                for c in range(NT):
                    b = s.bank()
                    for vc in range(2):
                        P.tr(b[:, vc * 128:(vc + 1) * 128], v_[:, vc, c * 128:(c + 1) * 128], ident[:])
                    P.cp(vT[:, c, :], b[:, 0:256], eng="scalar")
                orders = {0: list(range(NT)), 1: [1, 0] + list(range(17, 1, -1))}
                Scur = {0: 0, 1: 0}
                for d in range(2):
                    P.memset(SbD[d][0][:], 0.0)
                for step in range(NT):
                    for d in range(2):
                        mI = C["m_ple"] if d == 0 else C["m_pge"]
                        c = orders[d][step]
                        i = d * 2 + step % 2
                        t = tmp[i]
                        tok = slice(c * 128, (c + 1) * 128)
                        zb = s.bank()
                        P.mm(zb[:, 0:128], aw[0:16, d, :], adS[0:16, tok])
                        P.act(t["g"][:], zb[:, 0:128], AF.Sigmoid, bias=col(48 + 4 * d + hd))
                        P.act(t["g"][:], t["g"][:], AF.Ln)
                        P.ts(t["g"][:], t["g"][:], 1.0 / 16, ALU.mult)
                        P.scan(t["cs"][:], C["ones"][:], t["g"][:], 0.0, ALU.mult, ALU.add)
                        tot = t["cs"][:, 127:128]
                        if d == 0:
                            gc = t["cs"]
                        else:
                            gc = t["gc"]
                            P.ts(gc[:], t["cs"][:], -1.0, ALU.mult, tot, ALU.add)
                            P.tt(gc[:], gc[:], t["g"][:], ALU.add)
                        P.act(t["E1"][:], gc[:], AF.Exp)
                        P.stt(t["qg"][:], q_[:, tok], 128 ** -0.5, t["E1"][:], ALU.mult, ALU.mult)
                        P.act(t["E2"][:], gc[:], AF.Exp, scale=-1.0)
                        P.tt(t["kg"][:], k_[:, tok], t["E2"][:], ALU.mult, eng="gpsimd")
                        P.act(t["E4"][:], gc[:], AF.Exp, bias=tot, scale=-1.0)
                        P.tt(t["kend"][:], k_[:, tok], t["E4"][:], ALU.mult, eng="gpsimd")
                        P.act(gCe[i][:], tot, AF.Exp)
                        ba = s.bank(); P.mm(ba[:, 0:128], t["kg"][:], t["qg"][:])
                        P.tt(t["attT"][:], ba[:, 0:128], mI[:], ALU.mult)
                        bt = s.bank(); P.tr(bt[:, 0:128], t["kend"][:], ident[:])
                        P.cp(t["kendT"][:], bt[:, 0:128], eng="scalar")
                        Sc = SbD[d][Scur[d]]; Sn = SbD[d][1 - Scur[d]]
                        bo = s.bank()
                        P.mm(bo[:, 0:256], t["attT"][:], vT[:, c, :], start=True, stop=False)
                        P.mm(bo[:, 0:256], t["qg"][:], Sc[:], start=False, stop=True)
                        bs = s.bank(); P.mm(bs[:, 0:256], t["kendT"][:], vT[:, c, :])
                        P.stt(Sn[:], Sc[:], gCe[i][:, 0:1], bs[:, 0:256], ALU.mult, ALU.add)
                        Scur[d] = 1 - Scur[d]
                        P.cp(oaccD[d][:, c, :], bo[:, 0:256], eng="scalar")
                for c in range(NT):
                    tok = slice(c * 128, (c + 1) * 128)
                    e_ = c % 2
                    P.tt(osumE[e_][:], oaccD[0][:, c, :], oaccD[1][:, c, :], ALU.add, eng="gpsimd")
                    P.act(junk[:], osumE[e_][:], AF.Square, accum=ssqE[e_][:, 0:1])
                    P.ts(ssqE[e_][:], ssqE[e_][:], 1.0 / 256, ALU.mult, EPS, ALU.add)
                    P.act(ssqE[e_][:], ssqE[e_][:], AF.Sqrt)
                    P.recip(ssqE[e_][:], ssqE[e_][:])
                    P.ts(osumE[e_][:], osumE[e_][:], ssqE[e_][:, 0:1], ALU.mult)
                    be = s.bank()
                    for vc in range(2):
                        P.tr(be[:, vc * 128:(vc + 1) * 128], osumE[e_][:, vc * 128:(vc + 1) * 128], ident[:])
                    for vc in range(2):
                        P.stt(yo[:, vc, tok], be[:, vc * 128:(vc + 1) * 128], col(56 + vc), gt[:, vc, tok], ALU.mult, ALU.mult)
                for vc in range(2):
                    P.dma(s.ybT[hd * 256 + vc * 128:hd * 256 + (vc + 1) * 128, :], yo[:, vc, :])
        P.barrier()


    def stage_merge(s, l, last):
        P = s.P
        with contextlib.ExitStack() as st:
            sb = lambda n, shp, dt=F32: P.sb(n, shp, dt, stack=st)
            yaS = sb("yaS", [128, 8, T], BF16); ybS = sb("ybS", [128, 8, T], BF16)
            for kc in range(8):
                P.dma(yaS[:, kc, :], s.yaT[kc * 128:(kc + 1) * 128, :])
                P.dma(ybS[:, kc, :], s.ybT[kc * 128:(kc + 1) * 128, :])
            gbc = sb("gbc", [128, 2, D])
            for w_ in range(2):
                P.dma(gbc[:, w_, :], s.modrow[w_, 2 * D:3 * D].pb(128))
            wa_st = sb("wa_st", [128, 8, 128]); wb_st = sb("wb_st", [128, 8, 128])
            wa = sb("wa", [128, 8, 128], BF16); wb = sb("wb", [128, 8, 128], BF16)
            gaS = sb("gaS", [128, 512]); gbS = sb("gbS", [128, 512])
            mix = sb("mix", [128, 16, 512], BF16)
            m1 = sb("mm1", [128, 512]); m2 = sb("mm2", [128, 512])
            wo_st = sb("wo_st", [128, 16, 512]); wo = sb("wo", [128, 16, 512], BF16)
            xt = sb("mxt", [128, 512]); yt = sb("myt", [128, 512])
            blocks = [(256 + 512 * j, 512, 0, j) for j in range(4)]
            if not last:
                blocks = [(0, 256, 1, -1)] + blocks
            wav = s.inp["w_branch_a"][l].re("(kc p) n -> p kc n", p=128)
            wbv = s.inp["w_branch_b"][l].re("(kc p) n -> p kc n", p=128)
            wov = s.inp["w_out"][l].re("(kc p) n -> p kc n", p=128)
            for (t0, n, w_, j) in blocks:
                for dc in range(16):
                    dcs = slice(dc * 128, (dc + 1) * 128)
                    P.dma(wa_st[:], wav[:, :, dcs]); P.cp(wa[:], wa_st[:], eng="gpsimd")
                    P.dma(wb_st[:], wbv[:, :, dcs]); P.cp(wb[:], wb_st[:], eng="gpsimd")
                    P.dma(gaS[:, 0:n], s.pT[BR_GATE_OFF + dc * 128:BR_GATE_OFF + (dc + 1) * 128, t0:t0 + n])
                    P.dma(gbS[:, 0:n], s.pT[BR_GATE_OFF + D + dc * 128:BR_GATE_OFF + D + (dc + 1) * 128, t0:t0 + n])
                    bA = s.bank()
                    for kc in range(8):
                        P.mm(bA[:, 0:n], wa[:, kc, :], yaS[:, kc, t0:t0 + n], start=(kc == 0), stop=(kc == 7))
                    bB = s.bank()
                    for kc in range(8):
                        if j < 0:
                            rhs = ybS[:, kc, 0:256]
                        else:
                            rhs = ybS[:, kc, 256:T].re("p (c r) -> p r c", r=32)[:, 8 * j:8 * j + 8, :]
                        P.mm(bB[:, 0:n], wb[:, kc, :], rhs, start=(kc == 0), stop=(kc == 7))
                    P.act(gaS[:, 0:n], gaS[:, 0:n], AF.Sigmoid)
                    P.act(gbS[:, 0:n], gbS[:, 0:n], AF.Sigmoid)
                    P.tt(m1[:, 0:n], bA[:, 0:n], gaS[:, 0:n], ALU.mult)
                    P.tt(m2[:, 0:n], bB[:, 0:n], gbS[:, 0:n], ALU.mult)
                    P.tt(mix[:, dc, 0:n], m1[:, 0:n], m2[:, 0:n], ALU.add, eng="gpsimd")
                for db in range(4):
                    dbs = slice(db * 512, (db + 1) * 512)
                    P.dma(wo_st[:], wov[:, :, dbs]); P.cp(wo[:], wo_st[:], eng="gpsimd")
                    for ti in range(n // 128):
                        tile = t0 // 128 + ti
                        rows = slice(tile * 128, (tile + 1) * 128)
                        bo = s.bank()
                        for kc in range(16):
                            P.mm(bo[:, 0:512], mix[:, kc, ti * 128:(ti + 1) * 128], wo[:, kc, :], start=(kc == 0), stop=(kc == 15))
                        P.dma(xt[:], s.x_src(l, tile)[:, dbs])
                        P.tt(yt[:], bo[:, 0:512], gbc[:, w_, dbs], ALU.mult)
                        P.tt(yt[:], yt[:], xt[:], ALU.add)
                        P.dma(s.xmid[rows, dbs], yt[:])
        P.barrier()

    def stage_ffn(s, l, last):
        P = s.P
        C = s.C
        tiles = list(range(2, NT)) if last else list(range(NT))
        groups = [tiles[0:6], tiles[6:12], tiles[12:18]] if not last else [tiles[0:6], tiles[6:11], tiles[11:16]]
        with contextlib.ExitStack() as st0:
            sb0 = lambda n, shp, dt=F32: P.sb(n, shp, dt, stack=st0)
            rw = sb0("rw", [128, 16, NE]); P.dma(rw[:], s.inp["router_w"][:].re("(kc p) e -> p kc e", p=128))
            rb = sb0("rb", [128, NE]); P.dma(rb[:], s.inp["router_b"][0].pb(128))
            s.sel16 = sb0("sel16s", [16, 16, 128])
            P.dma(s.sel16[:], s.inp["sel16"][:])
            hT2 = sb0("hT2", [128, 16, 768], BF16); acc = sb0("acc", [128, 6, D]); gatesT = sb0("gatesT", [16, 768])
            for grp in groups:
                ng = len(grp); ntok = ng * 128
                tbl = [(a, min(512, ntok - a)) for a in range(0, ntok, 512)]
                with contextlib.ExitStack() as stA:
                    sbA = lambda n, shp, dt=F32: P.sb(n, shp, dt, stack=stA)
                    xb1 = sbA("fx", [128, D])
                    small = (sbA("fjunk", [128, D]), sbA("fss", [128, 1]), sbA("frstd", [128, 1]))
                    hT32 = sbA("hT32", [128, 16, 128])
                    q = {n: sbA("r_" + n, [128, 16]) for n in ("sc", "sel", "msk", "eq1", "msk2", "eq2", "wts", "gates")}
                    g4 = {n: sbA("r4_" + n, [128, 4]) for n in ("gs", "t4", "gmask", "pen")}
                    r1 = {n: sbA("r1_" + n, [128, 1]) for n in ("gmax", "m1", "m2", "ssum")}
                    for ti, tile in enumerate(grp):
                        w_ = 1 if tile < 2 else 0
                        s.norm_to_hT(s.xmid[tile * 128:(tile + 1) * 128, :], hT2, ti, s.m2, s.mod, w_, [xb1, xb1], small,
                                     hT32=hT32, sh_off=48)
                        br = s.bank()
                        for kc in range(16):
                            P.mm(br[:, 0:NE], hT32[:, kc, :], rw[:, kc, :], start=(kc == 0), stop=(kc == 15))
                        P.act(q["sc"][:], br[:, 0:NE], AF.Sigmoid)
                        P.tt(q["sel"][:], q["sc"][:], rb[:], ALU.add)
                        s4 = q["sel"][:].re("p (g j) -> p g j", j=4)
                        first = True
                        for a_ in range(4):
                            for b_ in range(a_ + 1, 4):
                                if first:
                                    P.tt(g4["gs"][:], s4[:, :, a_], s4[:, :, b_], ALU.add)
                                    first = False
                                else:
                                    P.tt(g4["t4"][:], s4[:, :, a_], s4[:, :, b_], ALU.add)
                                    P.tt(g4["gs"][:], g4["gs"][:], g4["t4"][:], ALU.max)
                        P.red(r1["gmax"][:], g4["gs"][:], ALU.max)
                        P.ts(g4["gmask"][:], g4["gs"][:], r1["gmax"][:, 0:1], ALU.is_ge)
                        P.ts(g4["pen"][:], g4["gmask"][:], -1.0, ALU.add, 1e9, ALU.mult)
                        m4 = q["msk"][:].re("p (g j) -> p g j", j=4)
                        for j_ in range(4):
                            P.tt(m4[:, :, j_], s4[:, :, j_], g4["pen"][:], ALU.add)
                        P.red(r1["m1"][:], q["msk"][:], ALU.max)
                        P.ts(q["eq1"][:], q["msk"][:], r1["m1"][:, 0:1], ALU.is_equal)
                        P.stt(q["msk2"][:], q["eq1"][:], -1e9, q["msk"][:], ALU.mult, ALU.add)
                        P.red(r1["m2"][:], q["msk2"][:], ALU.max)
                        P.ts(q["eq2"][:], q["msk2"][:], r1["m2"][:, 0:1], ALU.is_equal)
                        P.tt(q["eq1"][:], q["eq1"][:], q["eq2"][:], ALU.add)
                        P.tt(q["wts"][:], q["sc"][:], q["eq1"][:], ALU.mult)
                        P.red(r1["ssum"][:], q["wts"][:], ALU.add)
                        P.recip(r1["ssum"][:], r1["ssum"][:])
                        P.ts(q["gates"][:], q["wts"][:], r1["ssum"][:, 0:1], ALU.mult)
                        bt = s.bank()
                        P.tr(bt[0:NE, 0:128], q["gates"][:, 0:NE], C["ident"][:])
                        P.cp(gatesT[0:NE, ti * 128:(ti + 1) * 128], bt[0:NE, 0:128])
                P.barrier()
                with contextlib.ExitStack() as stB:
                    sbB = lambda n, shp, dt=F32: P.sb(n, shp, dt, stack=stB)
                    wd = sbB("wd", [128, 11, D], BF16)
                    stg = [sbB(f"stg{i}", [128, D]) for i in range(3)]
                    wgb = sbB("wgb", [128, 16, 128], BF16); wub = sbB("wub", [128, 16, 128], BF16)
                    actE = sbB("actE", [128, 11, 768], BF16); gb_ = sbB("gb_", [128, 768])
                    sg = sbB("sg", [128, 512]); tq = sbB("tq", [128, 512])
                    for e in range(NE):
                        for (a, n) in tbl:
                            bk = s.bank()
                            P.mm(bk[:, 0:n], s.sel16[0:16, e, :], gatesT[0:16, a:a + n])
                            P.cp(gb_[:, a:a + n], bk[:, 0:n], eng="scalar")
                        wgv = s.inp["exp_w_gate"][l, e].re("(kc p) f -> p kc f", p=128)
                        wuv = s.inp["exp_w_up"][l, e].re("(kc p) f -> p kc f", p=128)
                        for fc in range(11):
                            fs = slice(fc * 128, (fc + 1) * 128)
                            s0 = stg[0][:].re("p (kc f) -> p kc f", f=128)
                            s1 = stg[1][:].re("p (kc f) -> p kc f", f=128)
                            P.dma(s0, wgv[:, :, fs]); P.cp(wgb[:], s0, eng="vector")
                            P.dma(s1, wuv[:, :, fs]); P.cp(wub[:], s1, eng="scalar")
                            P.dma(stg[2][:], s.inp["exp_w_down"][l, e, fs, :]); P.cp(wd[:, fc, :], stg[2][:], eng="gpsimd")
                            for (a, n) in tbl:
                                bg = s.bank()
                                for kc in range(16):
                                    P.mm(bg[:, 0:n], wgb[:, kc, :], hT2[:, kc, a:a + n], start=(kc == 0), stop=(kc == 15))
                                bu = s.bank()
                                for kc in range(16):
                                    P.mm(bu[:, 0:n], wub[:, kc, :], hT2[:, kc, a:a + n], start=(kc == 0), stop=(kc == 15))
                                P.act(sg[:, 0:n], bg[:, 0:n], AF.Silu)
                                P.tt(tq[:, 0:n], bu[:, 0:n], gb_[:, a:a + n], ALU.mult)
                                P.tt(actE[:, fc, a:a + n], sg[:, 0:n], tq[:, 0:n], ALU.mult, eng="gpsimd")
                        for ti in range(ng):
                            for db in range(4):
                                dbs = slice(db * 512, (db + 1) * 512)
                                bo = s.bank()
                                for fc in range(11):
                                    P.mm(bo[:, 0:512], actE[:, fc, ti * 128:(ti + 1) * 128], wd[:, fc, dbs], start=(fc == 0), stop=(fc == 10))
                                if e == 0:
                                    P.cp(acc[:, ti, dbs], bo[:, 0:512], eng="scalar")
                                else:
                                    P.tt(acc[:, ti, dbs], bo[:, 0:512], acc[:, ti, dbs], ALU.add)
                P.barrier()
                with contextlib.ExitStack() as stC:
                    sbC = lambda n, shp, dt=F32: P.sb(n, shp, dt, stack=stC)
                    gbc = sbC("fgbc", [128, 2, D])
                    for w_ in range(2):
                        P.dma(gbc[:, w_, :], s.modrow[w_, 5 * D:6 * D].pb(128))
                    fng = sbC("fng", [128, D])
                    if last:
                        P.dma(fng[:], s.inp["final_norm_g"][:].pb(128))
                    xt = sbC("cxt", [128, D]); yt = sbC("cyt", [128, D]); junk = sbC("cjunk", [128, D])
                    ss = sbC("css", [128, 1])
                    for ti, tile in enumerate(grp):
                        w_ = 1 if tile < 2 else 0
                        rows = slice(tile * 128, (tile + 1) * 128)
                        P.dma(xt[:], s.xmid[rows, :])
                        P.tt(yt[:], acc[:, ti, :], gbc[:, w_, :], ALU.mult)
                        P.tt(yt[:], yt[:], xt[:], ALU.add)
                        if not last:
                            P.dma(s.xcur[rows, :], yt[:])
                        else:
                            P.act(junk[:], yt[:], AF.Square, accum=ss[:, 0:1])
                            P.ts(ss[:], ss[:], 1.0 / D, ALU.mult, EPS, ALU.add)
                            P.act(ss[:], ss[:], AF.Sqrt)
                            P.recip(ss[:], ss[:])
                            P.stt(yt[:], yt[:], ss[:, 0:1], fng[:], ALU.mult, ALU.mult)
                            P.dma(s.out[(tile - 2) * 128:(tile - 1) * 128, :], yt[:])
                P.barrier()

    def build(s):
        P = s.P
        s.colA = P.sb("colA", [128, 128])
        s.mod = P.sb("mod", [128, 96, 2])
        s.m1 = P.sb("m1", [128, 16, 2])
        s.m2 = P.sb("m2", [128, 16, 2])
        s.stage_prologue()
        fin = []
        for l in range(DEPTH):
            s.stage_mod(l)
            if s.stop_after == ("mod", l):
                break
            s.stage_proj(l)
            if s.stop_after == ("proj", l):
                break
            if not s.skip_rwkv:
                s.stage_rwkv(l)
            if s.stop_after == ("rwkv", l):
                break
            s.stage_gla(l)
            if s.stop_after == ("gla", l):
                break
            last = (l == DEPTH - 1)
            s.stage_merge(l, last)
            if s.stop_after == ("merge", l):
                break
            s.stage_ffn(l, last)
            if s.stop_after == ("ffn", l):
                break
        P.barrier()
        P.final_tokens = [(sk, v) for sk, v in P.dma_tgt.items()]
        P.emit()
        P.st.close()


def build_nc(stop_after=None, dbg=False):
    nc = bass.Bass("TRN2", target_bir_lowering=False)
    k = K(nc, stop_after=stop_after, dbg=dbg)
    k.build()
    return nc


def make_in_maps(inputs, cores):
    consts = make_consts()
    shared = {}
    for k, shp in W_SPECS.items():
        shared[k] = np.ascontiguousarray(np.asarray(inputs[k], dtype=np.float32).reshape(shp))
    maps = []
    for b in cores:
        m = dict(shared)
        m["x"] = np.ascontiguousarray(inputs["x"][b])
        m["ctx"] = np.ascontiguousarray(inputs["ctx"][b])
        m["c"] = np.ascontiguousarray(inputs["c"][b:b + 1])
        m["c_ctx"] = np.ascontiguousarray(np.asarray(inputs["c_ctx"]).reshape(1, D))
        m["consts"] = consts
        sel = np.zeros((16, 16, 128), np.float32)
        for e in range(16):
            sel[e, e, :] = 1.0
        m["sel16"] = sel
        maps.append(m)
    return maps


def kernel(**inputs):
    nc = build_nc()
    maps = make_in_maps(inputs, list(range(8)))
    res = run_bass_kernel_spmd(nc, maps, core_ids=list(range(8)))
    return np.stack([r["out"] for r in res.results], 0).astype(np.float32)
```
